# Optimizing a Trainium2 kernel written in Bass

```python
import math
import jax, jax.numpy as jnp
from jax import lax
import numpy as np

D_MODEL = 1024
BATCH = 32
SEQ = 2048
DEPTH = 1

D_MIX = D_MODEL
ATTN_HEADS = 8
ATTN_HD = 64
ATTN_W = ATTN_HEADS * ATTN_HD
MOBA_BLOCK = 256
MOBA_TOPK = 3
Q_BLOCK = 128
REC_HEADS = 4
REC_DK = 128
REC_DV = 128
REC_W = REC_HEADS * REC_DK
REC_VW = REC_HEADS * REC_DV
REC_CHUNK = 64
PEER_HEADS = 8
PEER_NKEYS = 128
PEER_N = PEER_NKEYS * PEER_NKEYS
PEER_DKEY = 256
PEER_TOPK = 16
PEER_TOKEN_BLOCK = 128
NORM_EPS = 1e-6
IN_WIDTHS = [ATTN_W, ATTN_W, ATTN_W, REC_W, REC_W, REC_VW, REC_VW]
IN_COLS = sum(IN_WIDTHS)
IN_SPLITS = [int(c) for c in np.cumsum(IN_WIDTHS)[:-1]]

kernel_name = 'hymba_moba_hgrn2_peer_block'


def rmsnorm(x, g):
    xf = x.astype(jnp.float32)
    y = xf * lax.rsqrt(jnp.mean(xf * xf, axis=-1, keepdims=True) + NORM_EPS)
    return (y * g.astype(jnp.float32)).astype(x.dtype)


def alibi_slopes(n):
    return 2.0 ** (-8.0 * jnp.arange(1, n + 1, dtype=jnp.float32) / n)


def moba_attention(q, k, v):
    B, S, H, hd = q.shape
    nb = -(-S // MOBA_BLOCK)
    pad = nb * MOBA_BLOCK - S
    kp = jnp.pad(k, ((0, 0), (0, pad), (0, 0), (0, 0)))
    vp = jnp.pad(v, ((0, 0), (0, pad), (0, 0), (0, 0)))
    kb = kp.reshape(B, nb, MOBA_BLOCK, H, hd).transpose(0, 3, 1, 2, 4)
    vb = vp.reshape(B, nb, MOBA_BLOCK, H, hd).transpose(0, 3, 1, 2, 4)
    kmean = jnp.mean(kb.astype(jnp.float32), axis=3)
    gate = jnp.einsum('bshd,bhnd->bhsn', q.astype(jnp.float32), kmean)
    pos = jnp.arange(S)
    past = jnp.arange(nb)[None, :] < (pos // MOBA_BLOCK)[:, None]
    gate = jnp.where(past[None, None], gate, -jnp.inf)
    ksel = min(MOBA_TOPK, nb)
    _, sel = lax.top_k(gate, ksel)
    nq = S // Q_BLOCK
    qq = q.reshape(B, nq, Q_BLOCK, H, hd).transpose(0, 1, 3, 2, 4).reshape(B * nq, H, Q_BLOCK, hd)
    ss = sel.reshape(B, H, nq, Q_BLOCK, ksel).transpose(0, 2, 1, 3, 4).reshape(B * nq, H, Q_BLOCK, ksel)
    b_idx = jnp.repeat(jnp.arange(B, dtype=jnp.int32), nq)
    qb_idx = jnp.tile(jnp.arange(nq, dtype=jnp.int32), B)
    slopes = alibi_slopes(H)
    scale = hd ** -0.5
    off = jnp.arange(MOBA_BLOCK)
    gather = jax.vmap(lambda kk, s: kk[s])

    def one_block(args):
        qblk, sblk, b, qi = args
        kh = kb[b]
        vh = vb[b]
        tpos = qi * Q_BLOCK + jnp.arange(Q_BLOCK)
        own = (qi * Q_BLOCK) // MOBA_BLOCK
        k_s = gather(kh, sblk).astype(jnp.float32)
        v_s = gather(vh, sblk).astype(jnp.float32)
        k_o = lax.dynamic_index_in_dim(kh, own, axis=1, keepdims=False).astype(jnp.float32)
        v_o = lax.dynamic_index_in_dim(vh, own, axis=1, keepdims=False).astype(jnp.float32)
        qf = qblk.astype(jnp.float32) * scale
        s_sel = jnp.einsum('hqd,hqjkd->hqjk', qf, k_s)
        kpos_sel = sblk[..., None] * MOBA_BLOCK + off
        dist_sel = (tpos[None, :, None, None] - kpos_sel).astype(jnp.float32)
        s_sel = s_sel - slopes[:, None, None, None] * dist_sel
        s_sel = jnp.where((sblk < own)[..., None], s_sel, -jnp.inf)
        s_own = jnp.einsum('hqd,hkd->hqk', qf, k_o)
        kpos_own = own * MOBA_BLOCK + off
        dist_own = (tpos[:, None] - kpos_own[None, :]).astype(jnp.float32)
        s_own = s_own - slopes[:, None, None] * dist_own[None]
        s_own = jnp.where((kpos_own[None, :] <= tpos[:, None])[None], s_own, -jnp.inf)
        logits = jnp.concatenate([s_sel.reshape(H, Q_BLOCK, ksel * MOBA_BLOCK), s_own], axis=-1)
        p = jax.nn.softmax(logits, axis=-1)
        p_sel = p[..., :ksel * MOBA_BLOCK].reshape(H, Q_BLOCK, ksel, MOBA_BLOCK)
        p_own = p[..., ksel * MOBA_BLOCK:]
        o = jnp.einsum('hqjk,hqjkd->qhd', p_sel, v_s) + jnp.einsum('hqk,hkd->qhd', p_own, v_o)
        return o.astype(q.dtype)

    out = lax.map(one_block, (qq, ss, b_idx, qb_idx))
    return out.reshape(B, S, H * hd)


def hgrn2(q, f_pre, i, g, lb, norm_g):
    B, S, _ = q.shape
    nc = S // REC_CHUNK
    f = lb + (1.0 - lb) * jax.nn.sigmoid(f_pre.astype(jnp.float32))
    logf = jnp.log(f)
    kk = 1.0 - f
    qf = jax.nn.silu(q.astype(jnp.float32))
    vf = i.astype(jnp.float32)

    def heads(t, d):
        return t.reshape(B, nc, REC_CHUNK, REC_HEADS, d).transpose(1, 0, 3, 2, 4)

    causal = jnp.tril(jnp.ones((REC_CHUNK, REC_CHUNK), dtype=bool))

    def step(state, inp):
        qc, lfc, kc, vc = inp
        cum = jnp.cumsum(lfc, axis=2)
        diff = cum[:, :, :, None, :] - cum[:, :, None, :, :]
        decay = jnp.where(causal[None, None, :, :, None], jnp.exp(jnp.minimum(diff, 0.0)), 0.0)
        scores = jnp.einsum('bhtd,bhtsd,bhsd->bhts', qc, decay, kc)
        o = jnp.einsum('bhts,bhsv->bhtv', scores, vc) + jnp.einsum('bhtd,bhdv->bhtv', qc * jnp.exp(cum), state)
        last = cum[:, :, -1:, :]
        state = jnp.exp(last[:, :, 0, :])[..., None] * state + jnp.einsum('bhsd,bhsv->bhdv', kc * jnp.exp(last - cum), vc)
        return state, o

    state0 = jnp.zeros((B, REC_HEADS, REC_DK, REC_DV), jnp.float32)
    _, o = lax.scan(step, state0, (heads(qf, REC_DK), heads(logf, REC_DK), heads(kk, REC_DK), heads(vf, REC_DV)))
    o = o.transpose(1, 0, 3, 2, 4).reshape(B, S, REC_HEADS, REC_DV)
    o = rmsnorm(o, norm_g.reshape(REC_HEADS, REC_DV))
    o = o * jax.nn.silu(g.astype(jnp.float32)).reshape(B, S, REC_HEADS, REC_DV)
    return o.reshape(B, S, REC_VW).astype(q.dtype)


def peer(x, wq, subkeys, u, v):
    B, S, D = x.shape
    T = B * S
    xt = x.reshape(T // PEER_TOKEN_BLOCK, PEER_TOKEN_BLOCK, D)

    def one(xb):
        qp = (xb @ wq).reshape(PEER_TOKEN_BLOCK, PEER_HEADS, 2, PEER_DKEY // 2)
        s = jnp.einsum('thcd,hcnd->thcn', qp.astype(jnp.float32), subkeys.astype(jnp.float32))
        v1, i1 = lax.top_k(s[:, :, 0], PEER_TOPK)
        v2, i2 = lax.top_k(s[:, :, 1], PEER_TOPK)
        cand = (v1[..., :, None] + v2[..., None, :]).reshape(PEER_TOKEN_BLOCK, PEER_HEADS, PEER_TOPK * PEER_TOPK)
        cidx = (i1[..., :, None] * PEER_NKEYS + i2[..., None, :]).reshape(PEER_TOKEN_BLOCK, PEER_HEADS, PEER_TOPK * PEER_TOPK)
        sc, p = lax.top_k(cand, PEER_TOPK)
        eidx = jnp.take_along_axis(cidx, p, axis=-1)
        gates = jax.nn.softmax(sc, axis=-1)
        ue = u[eidx]
        ve = v[eidx]
        act = jax.nn.gelu(jnp.einsum('td,thkd->thk', xb, ue).astype(jnp.float32), approximate=False)
        return jnp.einsum('thk,thkd->td', (gates * act).astype(x.dtype), ve)

    return lax.map(one, xt).reshape(B, S, D)


def setup_inputs(seed: int = 0) -> dict:
    key = jax.random.key(seed)
    ks = jax.random.split(key, 12)
    nrm = jax.random.normal
    x = nrm(ks[0], (BATCH, SEQ, D_MODEL), jnp.float32)
    norm1_g = 1.0 + 0.05 * nrm(ks[1], (DEPTH, D_MODEL), jnp.float32)
    w_in = nrm(ks[2], (DEPTH, D_MODEL, IN_COLS), jnp.float32) * D_MODEL ** -0.5
    rec_lb_logits = 0.5 * nrm(ks[3], (DEPTH + 1, REC_W), jnp.float32)
    rec_norm_g = 1.0 + 0.05 * nrm(ks[4], (DEPTH, REC_VW), jnp.float32)
    w_out = nrm(ks[5], (DEPTH, D_MIX, D_MODEL), jnp.float32) * D_MIX ** -0.5
    norm2_g = 1.0 + 0.05 * nrm(ks[6], (DEPTH, D_MODEL), jnp.float32)
    peer_wq = nrm(ks[7], (DEPTH, D_MODEL, PEER_HEADS * PEER_DKEY), jnp.float32) * D_MODEL ** -0.5
    peer_subkeys = nrm(ks[8], (DEPTH, PEER_HEADS, 2, PEER_NKEYS, PEER_DKEY // 2), jnp.float32) * (PEER_DKEY // 2) ** -0.5
    peer_u = nrm(ks[9], (DEPTH, PEER_N, D_MODEL), jnp.float32) * D_MODEL ** -0.5
    peer_v = 0.1 * nrm(ks[10], (DEPTH, PEER_N, D_MODEL), jnp.float32)
    normf_g = 1.0 + 0.05 * nrm(ks[11], (D_MODEL,), jnp.float32)
    return {'x': x, 'norm1_g': norm1_g, 'w_in': w_in, 'rec_lb_logits': rec_lb_logits,
            'rec_norm_g': rec_norm_g, 'w_out': w_out, 'norm2_g': norm2_g, 'peer_wq': peer_wq,
            'peer_subkeys': peer_subkeys, 'peer_u': peer_u, 'peer_v': peer_v, 'normf_g': normf_g}


def reference(x, norm1_g, w_in, rec_lb_logits, rec_norm_g, w_out, norm2_g, peer_wq, peer_subkeys, peer_u, peer_v, normf_g):
    B, S, _ = x.shape
    lb_all = jnp.cumsum(jax.nn.softmax(rec_lb_logits.astype(jnp.float32), axis=0), axis=0)
    h = x
    for layer in range(DEPTH):
        xn = rmsnorm(h, norm1_g[layer])
        proj = xn @ w_in[layer]
        q_a, k_a, v_a, q_r, f_r, i_r, g_r = jnp.split(proj, IN_SPLITS, axis=-1)
        attn = moba_attention(q_a.reshape(B, S, ATTN_HEADS, ATTN_HD),
                              k_a.reshape(B, S, ATTN_HEADS, ATTN_HD),
                              v_a.reshape(B, S, ATTN_HEADS, ATTN_HD))
        rec = hgrn2(q_r, f_r, i_r, g_r, lb_all[layer], rec_norm_g[layer])
        h = h + jnp.concatenate([attn, rec], axis=-1) @ w_out[layer]
        hn = rmsnorm(h, norm2_g[layer])
        h = h + peer(hn, peer_wq[layer], peer_subkeys[layer], peer_u[layer], peer_v[layer])
    return rmsnorm(h, normf_g)
```

```python
import numpy as np
from contextlib import ExitStack
import concourse.bass as bass
import concourse.mybir as mybir
from concourse.bass_utils import run_bass_kernel_spmd

F32 = mybir.dt.float32
BF16 = mybir.dt.bfloat16
U32 = mybir.dt.uint32
I32 = mybir.dt.int32
AF = mybir.ActivationFunctionType
ALU = mybir.AluOpType
AX = mybir.AxisListType

SEM_LIMIT = 30000
NEG = -1.0e30
MASKV = 32768.0


class Prog:
    ENG = ('pe', 'dve', 'act', 'pool', 'sp')

    def __init__(self, nc, stack):
        self.nc = nc
        self.stack = stack
        self.eng = {'pe': nc.tensor, 'dve': nc.vector, 'act': nc.scalar, 'pool': nc.gpsimd, 'sp': nc.sync}
        self.semcnt = {}
        self.sems = {}
        self.seen = {e: {} for e in self.ENG}
        self.res = {}
        self.ninst = 0

    def _sem(self, key):
        if key not in self.sems:
            self.sems[key] = self.stack.enter_context(self.nc.semaphore(f"s{len(self.sems)}"))
        return self.sems[key]

    def _bump(self, name, inc):
        ep, val = self.semcnt.get(name, (0, 0))
        if val + inc > SEM_LIMIT:
            ep += 1
            val = 0
        val += inc
        self.semcnt[name] = (ep, val)
        return ((name, ep), val)

    @staticmethod
    def _sibling(semname, w):
        return isinstance(semname, tuple) and len(semname) >= 2 and semname[0] == 'd' and semname[1] == w

    def _deps(self, reads, writes, myname, group, nowaw=False):
        d = {}

        def add(k, v):
            if d.get(k, 0) < v:
                d[k] = v
        for r in reads:
            st = self.res.get(r)
            if st:
                for (k, v) in st[0].values():
                    add(k, v)
        self._saved = {}
        for w in writes:
            st = self.res.get(w)
            sv = {}
            if st:
                sib = group and len(st[0]) > 0 and all(self._sibling(n, w) for n in st[0]) and self._sibling(myname, w)
                if sib or nowaw:
                    sv = dict(st[2])
                else:
                    for (k, v) in st[0].values():
                        sv[k] = max(sv.get(k, 0), v)
                for k, v in st[1].items():
                    sv[k] = max(sv.get(k, 0), v)
                for k, v in sv.items():
                    add(k, v)
            self._saved[w] = sv
        return d

    def _waits(self, eng, d):
        e = self.eng[eng]
        for k, v in d.items():
            if eng == 'pe' and k[0] == 'pe':
                continue
            if self.seen[eng].get(k, 0) >= v:
                continue
            self.seen[eng][k] = v
            e.wait_ge(self._sem(k), v)

    def _update(self, reads, writes, myname, mykey, myval, group, nowaw=False):
        for r in reads:
            st = self.res.setdefault(r, [{}, {}, {}])
            st[1][mykey] = myval
        for w in writes:
            st = self.res.get(w)
            if st and (nowaw or (group and len(st[0]) > 0 and all(self._sibling(n, w) for n in st[0]) and self._sibling(myname, w))):
                wr = dict(st[0])
            else:
                wr = {}
            wr[myname] = (mykey, myval)
            self.res[w] = [wr, {}, self._saved.get(w, {})]

    def op(self, eng, fn, reads=(), writes=()):
        mykey, myval = self._bump(eng, 1)
        d = self._deps(reads, writes, eng, False)
        self._waits(eng, d)
        ins = fn(self.eng[eng])
        ins.then_inc(self._sem(mykey), 1)
        self._update(reads, writes, eng, mykey, myval, False)
        self.ninst += 1
        return ins

    def dma(self, qeng, out, in_, reads=(), writes=(), sem=None, lane=0, group=True, fn=None, nowaw=False, **kw):
        semname = ('d', sem if sem is not None else writes[0], lane)
        prev = self.semcnt.get(semname)
        mykey, myval = self._bump(semname, 16)
        d = self._deps(reads, writes, semname, group, nowaw)
        if prev is not None and prev[1] > 0:
            pk = (semname, prev[0])
            d[pk] = max(d.get(pk, 0), prev[1])
        self._waits(qeng, d)
        e = self.eng[qeng]
        if fn is not None:
            ins = fn(e)
        else:
            ins = e.dma_start(out=out, in_=in_, **kw)
        ins.then_inc(self._sem(mykey), 16)
        self._update(reads, writes, semname, mykey, myval, group, nowaw)
        self.ninst += 1
        return ins

    def barrier(self):
        snap = {(name, ep): val for name, (ep, val) in self.semcnt.items()}
        for eng in self.ENG:
            self._waits(eng, snap)

    def fence(self, eng, reads=(), writes=()):
        d = self._deps(reads, writes, None, False)
        self._waits(eng, d)


class Tiles:
    def __init__(self, p, stack, name, shape, dtype, bufs=1, psum=False):
        alloc = p.nc.psum_tensor if psum else p.nc.sbuf_tensor
        self.t = [stack.enter_context(alloc(f"{name}_{i}", shape, dtype)) for i in range(bufs)]
        self.name = name
        self.bufs = bufs
        self.i = -1

    def next(self):
        self.i += 1
        return self.cur()

    def cur(self):
        b = self.i % self.bufs
        return self.t[b], (self.name, b)


class Views:
    def __init__(self, items):
        self.items = items
        self.i = -1

    def next(self):
        self.i += 1
        return self.items[self.i % len(self.items)]


def make_consts():
    c = {}
    c["c_ident"] = np.eye(128, dtype=np.float32)
    kq = np.arange(128)
    c["c_tri"] = np.where(kq[:, None] > kq[None, :], -MASKV, 0.0).astype(np.float32)
    pos = np.arange(2048)
    blk = pos // 256
    r = pos % 256
    kaug = np.zeros((8, 12, 2048), np.float32)
    qaug = np.zeros((8, 4, 2048), np.float32)
    for h in range(8):
        sl = 2.0 ** (-(h + 1))
        for n in range(8):
            kaug[h, n] = MASKV * (blk == n)
        kaug[h, 8] = sl * r
        kaug[h, 9] = 1.0
        kaug[h, 10] = sl * 256.0 * blk
        kaug[h, 11] = 1.0
        qaug[h, 0] = 1.0
        qaug[h, 1] = -sl * r
        qaug[h, 2] = 1.0
        qaug[h, 3] = -sl * 256.0 * blk
    c["c_kaug"] = kaug
    c["c_qaug"] = qaug
    el = np.zeros((3, 16, 8), np.float32)
    for qt in range(16):
        j = qt // 2
        for n in range(8):
            el[0, qt, n] = 0.0 if n < j else NEG
            el[1, qt, n] = 1.0 if n < j else 0.0
            el[2, qt, n] = 1.0 if n == j else 0.0
    c["c_elig"] = np.broadcast_to(el.reshape(1, 3 * 16 * 8), (128, 384)).copy()
    s = np.arange(128)
    ch = s // 64
    mid = ch * 64 + 31
    same = ch[:, None] == ch[None, :]
    amat = (same & (s[:, None] <= s[None, :])).astype(np.float32) - (same & (s[:, None] <= mid[None, :])).astype(np.float32)
    c["c_amat"] = amat.astype(np.float32)
    bsel = np.zeros((128, 4), np.float32)
    bsel[:, 0] = (s <= 31)
    bsel[:, 1] = (s <= 63)
    bsel[:, 2] = (s >= 64) & (s <= 95)
    bsel[:, 3] = (s >= 64)
    c["c_bsel"] = bsel
    c["c_cmask"] = (same & (s[:, None] <= s[None, :])).astype(np.int32)
    c["c_iota16"] = np.broadcast_to(np.arange(16, dtype=np.float32)[None, :], (128, 16)).copy()
    return c


CONST_SHAPES = {"c_ident": ([128, 128], F32), "c_tri": ([128, 128], F32), "c_kaug": ([8, 12, 2048], F32),
                "c_qaug": ([8, 4, 2048], F32), "c_elig": ([128, 384], F32), "c_amat": ([128, 128], F32),
                "c_bsel": ([128, 4], F32), "c_cmask": ([128, 128], I32), "c_iota16": ([128, 16], F32)}


def build(nseq=4, dbg=False, stages="0ABCD"):
    T = nseq * 2048
    NT = T // 128
    NG = T // 512
    nc = bass.Bass("TRN2", target_bir_lowering=False)

    def din(name, shape, dt=F32):
        return nc.dram_tensor(name, shape, dt, kind="ExternalInput").ap()

    x = din("x", [T, 1024])
    norm1_g = din("norm1_g", [1, 1024])
    w_in = din("w_in", [1024, 3584])
    rec_lb = din("rec_lb_logits", [2, 512])
    rec_norm_g = din("rec_norm_g", [1, 512])
    w_out = din("w_out", [1024, 1024])
    norm2_g = din("norm2_g", [1, 1024])
    peer_wq = din("peer_wq", [1024, 2048])
    peer_sk = din("peer_subkeys", [16, 128, 128])
    peer_u = din("peer_u", [16384, 1024])
    peer_v = din("peer_v", [16384, 1024])
    normf_g = din("normf_g", [1, 1024])
    cst = {k: din(k, sh, dt) for k, (sh, dt) in CONST_SHAPES.items()}
    out = nc.dram_tensor("out", [T, 1024], F32, kind="ExternalOutput").ap()
    skind = "ExternalOutput" if dbg else "Internal"
    projT = nc.dram_tensor("projT", [T, 2560], F32, kind=skind).ap()
    qkT = nc.dram_tensor("qkT", [16, 64, T], BF16, kind=skind).ap()
    mix = nc.dram_tensor("mix", [T, 1024], BF16, kind=skind).ap()
    uvtab = nc.dram_tensor("uvtab", [16384, 2048], BF16, kind="Internal").ap()

    with ExitStack() as st:
        p = Prog(nc, st)
        identf = Tiles(p, st, "identf", [128, 128], F32)
        identb = Tiles(p, st, "identb", [128, 128], BF16)
        idf_t, idf_k = identf.next()
        idb_t, idb_k = identb.next()
        p.dma('sp', idf_t[:], cst["c_ident"], writes=[idf_k])
        p.op('dve', lambda e: e.tensor_copy(idb_t[:], idf_t[:]), reads=[idf_k], writes=[idb_k])
        ksum = Tiles(p, st, "ksum", [64, 8, NG * 2], F32)
        ksum_t, ksum_k = ksum.next()

        p.barrier()
        if "A" in stages:
            with ExitStack() as ph:
                win = Tiles(p, ph, "win", [128, 8, 3584], BF16)
                win_t, win_k = win.next()
                for k in range(8):
                    for hf in range(2):
                        p.dma('pool', win_t[:, k, hf * 1792:(hf + 1) * 1792],
                              w_in[k * 128:(k + 1) * 128, hf * 1792:(hf + 1) * 1792], writes=[win_k], lane=(2 * k + hf) % 4)
                if "0" in stages:
                    for blk in range(4):
                        rows = slice(blk * 4096, (blk + 1) * 4096)
                        p.dma('pool', uvtab[rows, 0:1024], peer_u[rows, :], writes=['uvtab'], sem=('uvt', blk), lane=0, nowaw=True)
                        p.dma('pool', uvtab[rows, 1024:2048], peer_v[rows, :], writes=['uvtab'], sem=('uvt', blk), lane=1, nowaw=True)
                g1b = Tiles(p, ph, "g1b", [128, 1024], F32)
                g1_t, g1_k = g1b.next()
                p.dma('sp', g1_t[:], norm1_g.partition_broadcast(128), writes=[g1_k])
                xt = Tiles(p, ph, "xt", [128, 1024], F32, bufs=3)
                junk = Tiles(p, ph, "junkA", [128, 1024], BF16, bufs=2)
                ssA = Tiles(p, ph, "ssA", [128, 4], F32, bufs=4)
                xn = Tiles(p, ph, "xn", [128, 1024], BF16, bufs=2)
                xnT = Tiles(p, ph, "xnT", [128, 8, 512], BF16, bufs=2)
                tok = Tiles(p, ph, "tok", [128, 2560], F32, bufs=2)
                qkst = Tiles(p, ph, "qkst", [64, 16, 512], BF16, bufs=2)
                psT = Tiles(p, ph, "psT", [128, 8, 128], BF16, bufs=1, psum=True)
                psA = Tiles(p, ph, "psA", [128, 512], F32, bufs=6, psum=True)
                ev = 0
                def prep_a(gi, ctx):
                    xnT_t, xnT_k = xnT.next()
                    for ti in range(4):
                        r0 = gi * 512 + ti * 128
                        xt_t, xt_k = xt.next()
                        p.dma('sp', xt_t[:], x[r0:r0 + 128, :], writes=[xt_k])
                        j_t, j_k = junk.next()
                        ss_t, ss_k = ssA.next()
                        p.op('act', lambda e: e.activation(j_t[:], xt_t[:], AF.Square, accum_out=ss_t[:, 0:1]),
                             reads=[xt_k], writes=[j_k, ss_k])
                        p.op('dve', lambda e: e.tensor_scalar(ss_t[:, 1:2], ss_t[:, 0:1], 1.0 / 1024, 1e-6, ALU.mult, ALU.add),
                             reads=[ss_k], writes=[ss_k])
                        p.op('act', lambda e: e.activation(ss_t[:, 2:3], ss_t[:, 1:2], AF.Sqrt), reads=[ss_k], writes=[ss_k])
                        p.op('dve', lambda e: e.reciprocal(ss_t[:, 3:4], ss_t[:, 2:3]), reads=[ss_k], writes=[ss_k])
                        xn_t, xn_k = xn.next()
                        p.op('dve', lambda e: e.scalar_tensor_tensor(xn_t[:], xt_t[:], ss_t[:, 3:4], g1_t[:], ALU.mult, ALU.mult),
                             reads=[xt_k, ss_k, g1_k], writes=[xn_k])
                        yield
                        pst_t, pst_k = psT.next()

                        def tr(e):
                            for k in range(8):
                                ins = e.transpose(pst_t[:, k, :], xn_t[:, k * 128:(k + 1) * 128], idb_t[:])
                            return ins
                        p.op('pe', tr, reads=[xn_k, idb_k], writes=[pst_k])
                        p.op('act', lambda e: e.copy(xnT_t[:, :, ti * 128:(ti + 1) * 128], pst_t[:]),
                             reads=[pst_k], writes=[xnT_k])
                        yield
                    ctx["xnT"] = (xnT_t, xnT_k)
                    yield

                def adv_a(gen, n):
                    if gen is None:
                        return
                    for _ in range(n):
                        try:
                            next(gen)
                        except StopIteration:
                            return
                NGA = NG
                ctxa = [dict() for _ in range(NGA + 1)]
                gen_a = prep_a(0, ctxa[0])
                adv_a(gen_a, 100)
                for gi in range(NGA):
                    gen_a = prep_a(gi + 1, ctxa[gi + 1]) if gi + 1 < NGA else None
                    xnT_t, xnT_k = ctxa[gi]["xnT"]
                    for ti in range(4):
                        r0 = gi * 512 + ti * 128
                        tok_t, tok_k = tok.next()
                        for cg in range(5):
                            ps_t, ps_k = psA.next()

                            def mm(e):
                                for k in range(8):
                                    ins = e.matmul(ps_t[:], xnT_t[:, k, ti * 128:(ti + 1) * 128],
                                                   win_t[:, k, 1024 + cg * 512:1024 + (cg + 1) * 512],
                                                   start=(k == 0), stop=(k == 7))
                                return ins
                            p.op('pe', mm, reads=[xnT_k, win_k], writes=[ps_k])
                            ev += 1
                            if ev % 2:
                                p.op('act', lambda e: e.copy(tok_t[:, cg * 512:(cg + 1) * 512], ps_t[:]),
                                     reads=[ps_k], writes=[tok_k])
                            else:
                                p.op('dve', lambda e: e.tensor_copy(tok_t[:, cg * 512:(cg + 1) * 512], ps_t[:]),
                                     reads=[ps_k], writes=[tok_k])
                        p.dma('pool', projT[r0:r0 + 128, :], tok_t[:], reads=[tok_k], writes=['projT'], sem=tok_k, lane='st', nowaw=True)
                        adv_a(gen_a, 1)
                    qk_t, qk_k = qkst.next()
                    for i in range(16):
                        if i % 3 == 0:
                            adv_a(gen_a, 1)
                        ps_t, ps_k = psA.next()

                        def mm2(e):
                            for k in range(8):
                                ins = e.matmul(ps_t[0:64, :], win_t[:, k, i * 64:(i + 1) * 64], xnT_t[:, k, :],
                                               start=(k == 0), stop=(k == 7))
                            return ins
                        p.op('pe', mm2, reads=[xnT_k, win_k], writes=[ps_k])
                        if i < 8:
                            p.op('act', lambda e: e.mul(qk_t[:, i, :], ps_t[0:64, :], 0.125), reads=[ps_k], writes=[qk_k])
                        else:
                            h = i - 8
                            for b2 in range(2):
                                p.op('act', lambda e: e.activation(qk_t[:, i, b2 * 256:(b2 + 1) * 256], ps_t[0:64, b2 * 256:(b2 + 1) * 256],
                                                                   AF.Copy, accum_out=ksum_t[:, h, gi * 2 + b2:gi * 2 + b2 + 1]),
                                     reads=[ps_k], writes=[qk_k, ksum_k])
                    p.dma('pool', qkT[:, :, gi * 512:(gi + 1) * 512].rearrange("i d t -> d i t"), qk_t[:],
                          reads=[qk_k], writes=['qkT'], sem=qk_k, lane='st', nowaw=True)
                    adv_a(gen_a, 100)

        p.barrier()
        if "B" in stages:
            with ExitStack() as ph:
                qTa = Tiles(p, ph, "qTa", [76, 2048], BF16, bufs=2)
                kTa = Tiles(p, ph, "kTa", [76, 2048], BF16, bufs=2)
                vaug = Tiles(p, ph, "vaug", [128, 16, 65], BF16, bufs=2)
                kmb = Tiles(p, ph, "kmb", [64, 8], BF16, bufs=2)
                elig = Tiles(p, ph, "elig", [128, 3, 16, 8], F32)
                tri = Tiles(p, ph, "tri", [128, 128], BF16)
                gm = Tiles(p, ph, "gm", [128, 16, 8], F32, bufs=2)
                m8 = Tiles(p, ph, "m8", [128, 16, 8], F32, bufs=2)
                selt = Tiles(p, ph, "selt", [128, 16, 8], F32, bufs=2)
                biast = Tiles(p, ph, "biast", [128, 16, 8], BF16, bufs=2)
                bT = Tiles(p, ph, "bT", [8, 2048], BF16, bufs=2)
                pT = Tiles(p, ph, "pT", [128, 16, 512], BF16, bufs=2)
                osb = Tiles(p, ph, "osb", [128, 4, 64], BF16, bufs=3)
                rc = Tiles(p, ph, "rc", [128, 4], F32, bufs=3)
                psS = Tiles(p, ph, "psS", [128, 512], F32, bufs=3, psum=True)
                psO = Tiles(p, ph, "psO", [128, 512], F32, bufs=2, psum=True)
                psG = Tiles(p, ph, "psG", [128, 16, 8], F32, bufs=1, psum=True)
                psB = Tiles(p, ph, "psB", [128, 2048], BF16, bufs=1, psum=True)
                el_t, el_k = elig.next()
                p.dma('sp', el_t[:].rearrange("p a b c -> p (a b c)"), cst["c_elig"], writes=[el_k])
                tri_t, tri_k = tri.next()
                p.dma('pool', tri_t[:], cst["c_tri"], writes=[tri_k])
                for b in range(2):
                    va_t, va_k = vaug.next()
                    p.op('pool', lambda e: e.memset(va_t[:, :, 64:65], 1.0), writes=[va_k])
                def advance_b(gen, n):
                    if gen is None:
                        return
                    for _ in range(n):
                        try:
                            next(gen)
                        except StopIteration:
                            return

                def prologue(s, h, ctx):
                    t0 = s * 2048
                    if True:
                        q_t, q_k = qTa.next()
                        k_t, k_k = kTa.next()
                        va_t, va_k = vaug.next()
                        p.dma('sp', q_t[0:64, :], qkT[h, :, t0:t0 + 2048], reads=['qkT'], writes=[q_k])
                        p.dma('sp', k_t[0:64, :], qkT[8 + h, :, t0:t0 + 2048], reads=['qkT'], writes=[k_k])
                        p.dma('pool', k_t[64:76, :], cst["c_kaug"][h], writes=[k_k], lane=1)
                        p.dma('pool', q_t[72:76, :], cst["c_qaug"][h], writes=[q_k], lane=1)
                        p.dma('pool', va_t[:, :, 0:64],
                              projT[t0:t0 + 2048, h * 64:(h + 1) * 64].rearrange("(kt p) c -> p kt c", p=128),
                              reads=['projT'], writes=[va_k])
                        yield
                        km_t, km_k = kmb.next()
                        p.op('dve', lambda e: e.tensor_scalar(km_t[:], ksum_t[:, h, s * 8:(s + 1) * 8], 1.0 / 256, None, ALU.mult),
                             reads=[ksum_k], writes=[km_k])
                        pg_t, pg_k = psG.next()

                        def gmm(e):
                            for qt in range(16):
                                ins = e.matmul(pg_t[:, qt, :], q_t[0:64, qt * 128:(qt + 1) * 128], km_t[:], start=True, stop=True)
                            return ins
                        p.op('pe', gmm, reads=[q_k, km_k], writes=[pg_k])
                        yield
                        gm_t, gm_k = gm.next()
                        m8_t, m8_k = m8.next()
                        se_t, se_k = selt.next()
                        bi_t, bi_k = biast.next()
                        p.op('dve', lambda e: e.tensor_tensor(gm_t[:], pg_t[:], el_t[:, 0], ALU.add), reads=[pg_k, el_k], writes=[gm_k])
                        for qt in range(16):
                            p.op('dve', lambda e: e.max(m8_t[:, qt, :], gm_t[:, qt, :]), reads=[gm_k], writes=[m8_k])
                        yield
                        p.op('dve', lambda e: e.tensor_tensor(se_t[:], gm_t[:], m8_t[:, :, 2:3].to_broadcast([128, 16, 8]), ALU.is_ge),
                             reads=[gm_k, m8_k], writes=[se_k])
                        p.op('dve', lambda e: e.tensor_tensor(se_t[:], se_t[:], el_t[:, 1], ALU.mult), reads=[se_k, el_k], writes=[se_k])
                        p.op('dve', lambda e: e.scalar_tensor_tensor(bi_t[:], se_t[:], -1.0, el_t[:, 2], ALU.add, ALU.add),
                             reads=[se_k, el_k], writes=[bi_k])
                        yield
                        pb_t, pb_k = psB.next()

                        def btr(e):
                            for qt in range(16):
                                ins = e.transpose(pb_t[0:8, qt * 128:(qt + 1) * 128], bi_t[:, qt, :], idb_t[:])
                            return ins
                        p.op('pe', btr, reads=[bi_k, idb_k], writes=[pb_k])
                        bT_t, bT_k = bT.next()
                        p.op('act', lambda e: e.copy(bT_t[:], pb_t[0:8, :]), reads=[pb_k], writes=[bT_k])
                        p.dma('sp', q_t[64:72, :], bT_t[:], reads=[bT_k], writes=[q_k], lane=2)
                        ctx.update(q=(q_t, q_k), k=(k_t, k_k), va=(va_t, va_k), t0=t0, h=h)
                    yield

                def attention(ctx, gen):
                    q_t, q_k = ctx["q"]; k_t, k_k = ctx["k"]; va_t, va_k = ctx["va"]; t0 = ctx["t0"]; h = ctx["h"]
                    if True:
                        for g in range(4):
                            pT_t, pT_k = pT.next()
                            for kt in range(4 * g + 4):
                                c0 = max(0, kt * 128 - g * 512)
                                ps_t, ps_k = psS.next()
                                diag = kt >= 4 * g

                                def smm(e):
                                    ins = e.matmul(ps_t[:, c0:512], k_t[0:76, kt * 128:(kt + 1) * 128],
                                                   q_t[0:76, g * 512 + c0:(g + 1) * 512], start=True, stop=not diag)
                                    if diag:
                                        ins = e.matmul(ps_t[:, c0:c0 + 128], idb_t[:], tri_t[:], start=False, stop=True)
                                    return ins
                                p.op('pe', smm, reads=[k_k, q_k, idb_k, tri_k], writes=[ps_k])
                                p.op('act', lambda e: e.activation(pT_t[:, kt, c0:512], ps_t[:, c0:512], AF.Exp),
                                     reads=[ps_k], writes=[pT_k])
                                if kt % 4 == 3:
                                    advance_b(gen, 1)
                            po_t, po_k = psO.next()
                            po_v = po_t[:, 0:260].rearrange("p (a b) -> p a b", b=65)

                            def pv(e):
                                for qs in range(4):
                                    qt = 4 * g + qs
                                    for kt in range(qt + 1):
                                        ins = e.matmul(po_v[:, qs, :], pT_t[:, kt, qs * 128:(qs + 1) * 128], va_t[:, kt, :],
                                                       start=(kt == 0), stop=(kt == qt))
                                return ins
                            p.op('pe', pv, reads=[pT_k, va_k], writes=[po_k])
                            rc_t, rc_k = rc.next()
                            os_t, os_k = osb.next()
                            p.op('dve', lambda e: e.reciprocal(rc_t[:], po_v[:, :, 64]), reads=[po_k], writes=[rc_k])
                            p.op('dve', lambda e: e.tensor_tensor(os_t[:], po_v[:, :, 0:64],
                                                                  rc_t[:].unsqueeze(2).to_broadcast([128, 4, 64]), ALU.mult),
                                 reads=[po_k, rc_k], writes=[os_k])
                            r0 = t0 + g * 512
                            p.dma('sp', mix[r0:r0 + 512, h * 64:(h + 1) * 64].rearrange("(qs p) c -> p qs c", p=128), os_t[:],
                                  reads=[os_k], writes=['mix'], sem=os_k, lane='st', nowaw=True)

                units = [(s_, h_) for s_ in range(nseq) for h_ in range(8)]
                ctxb = [dict() for _ in units]
                g0 = prologue(units[0][0], units[0][1], ctxb[0])
                advance_b(g0, 100)
                for ui in range(len(units)):
                    gen = prologue(units[ui + 1][0], units[ui + 1][1], ctxb[ui + 1]) if ui + 1 < len(units) else None
                    attention(ctxb[ui], gen)
                    advance_b(gen, 100)

        p.barrier()
        if "C" in stages:
            with ExitStack() as ph:
                amat = Tiles(p, ph, "amat", [128, 128], F32)
                bsel = Tiles(p, ph, "bsel", [128, 4], F32)
                cmask = Tiles(p, ph, "cmask", [128, 128], I32)
                lbb = Tiles(p, ph, "lbb", [128, 4, 512], F32)
                am_t, am_k = amat.next()
                bs_t, bs_k = bsel.next()
                cm_t, cm_k = cmask.next()
                lb_t, lb_k = lbb.next()
                p.dma('sp', am_t[:], cst["c_amat"], writes=[am_k])
                p.dma('sp', bs_t[:], cst["c_bsel"], writes=[bs_k])
                p.dma('sp', cm_t[:], cst["c_cmask"], writes=[cm_k])
                p.dma('sp', lb_t[:, 0, :], rec_lb[0:1, :].partition_broadcast(128), writes=[lb_k])
                p.dma('sp', lb_t[:, 1, :], rec_lb[1:2, :].partition_broadcast(128), writes=[lb_k], lane=1)
                p.dma('sp', lb_t[:, 3, :], rec_norm_g.partition_broadcast(128), writes=[lb_k], lane=2)
                p.op('dve', lambda e: e.tensor_tensor(lb_t[:, 0, :], lb_t[:, 0, :], lb_t[:, 1, :], ALU.subtract), reads=[lb_k], writes=[lb_k])
                p.op('act', lambda e: e.activation(lb_t[:, 1, :], lb_t[:, 0, :], AF.Sigmoid), reads=[lb_k], writes=[lb_k])
                p.op('dve', lambda e: e.tensor_scalar(lb_t[:, 2, :], lb_t[:, 1, :], -1.0, 1.0, ALU.mult, ALU.add), reads=[lb_k], writes=[lb_k])
                rec = Tiles(p, ph, "rec", [128, 2048], F32, bufs=2)
                fT = Tiles(p, ph, "fT", [128, 512], F32, bufs=2)
                lf = Tiles(p, ph, "lf", [128, 512], F32, bufs=2)
                kkT = Tiles(p, ph, "kkT", [128, 512], F32, bufs=2)
                qfT = Tiles(p, ph, "qfT", [128, 512], F32, bufs=2)
                eD = Tiles(p, ph, "eD", [128, 2, 512], F32, bufs=2)
                qtl = Tiles(p, ph, "qtl", [128, 512], BF16, bufs=2)
                ktl = Tiles(p, ph, "ktl", [128, 512], BF16, bufs=2)
                vbf = Tiles(p, ph, "vbf", [128, 512], BF16, bufs=2)
                qtT = Tiles(p, ph, "qtT", [128, 4, 128], BF16, bufs=2)
                ktT = Tiles(p, ph, "ktT", [128, 4, 128], BF16, bufs=2)
                ecs = Tiles(p, ph, "ecs", [128, 4, 4], F32, bufs=2)
                dl = Tiles(p, ph, "dl", [128, 4, 2], F32, bufs=2)
                pcs = Tiles(p, ph, "pcs", [128, 4, 4], F32, bufs=2)
                edl = Tiles(p, ph, "edl", [128, 4, 2], F32, bufs=2)
                AT = Tiles(p, ph, "AT", [128, 4, 128], BF16, bufs=2)
                Sst = Tiles(p, ph, "Sst", [128, 4, 128], F32, bufs=nseq)
                Sb = Tiles(p, ph, "Sb", [128, 128], BF16, bufs=4)
                tmpS = Tiles(p, ph, "tmpS", [128, 128], F32, bufs=3)
                ssC = Tiles(p, ph, "ssC", [128, 4, 4], F32, bufs=2)
                junkC = Tiles(p, ph, "junkC", [128, 128], BF16, bufs=2)
                sg = Tiles(p, ph, "sg", [128, 512], F32, bufs=2)
                o1 = Tiles(p, ph, "o1", [128, 4, 128], F32, bufs=2)
                recb = Tiles(p, ph, "recb", [128, 512], BF16, bufs=2)
                psD = Tiles(p, ph, "psD", [128, 512], F32, bufs=2, psum=True)
                psOo = Tiles(p, ph, "psOo", [128, 4, 128], F32, bufs=2, psum=True)
                psTq = Tiles(p, ph, "psTq", [128, 4, 128], BF16, bufs=1, psum=True)
                psC = Tiles(p, ph, "psC", [128, 4, 4], F32, bufs=1, psum=True)
                psSc = Tiles(p, ph, "psSc", [128, 128], F32, bufs=1, psum=True)
                psKV = Tiles(p, ph, "psKV", [128, 128], F32, bufs=1, psum=True)
                for b in range(2):
                    at_t, at_k = AT.next()
                    p.op('pool', lambda e: e.memset(at_t[:], 0.0), writes=[at_k])
                for s in range(nseq):
                    S_t, S_k = Sst.next()
                    p.op('pool', lambda e: e.memset(S_t[:], 0.0), writes=[S_k])
                def tile_c(s, ti):
                    if True:
                        S_t, S_k = Sst.t[s], ("Sst", s)
                        r0 = s * 2048 + ti * 128
                        rec_t, rec_k = rec.next()
                        p.dma('sp', rec_t[:], projT[r0:r0 + 128, 512:2560], reads=['projT'], writes=[rec_k])
                        f_t, f_k = fT.next()
                        lf_t, lf_k = lf.next()
                        kk_t, kk_k = kkT.next()
                        qf_t, qf_k = qfT.next()
                        p.op('act', lambda e: e.activation(f_t[:], rec_t[:, 512:1024], AF.Sigmoid), reads=[rec_k], writes=[f_k])
                        yield
                        p.op('act', lambda e: e.activation(qf_t[:], rec_t[:, 0:512], AF.Silu), reads=[rec_k], writes=[qf_k])
                        sg_t, sg_k = sg.next()
                        p.op('act', lambda e: e.activation(sg_t[:], rec_t[:, 1536:2048], AF.Silu), reads=[rec_k], writes=[sg_k])
                        p.op('dve', lambda e: e.tensor_tensor(f_t[:], f_t[:], lb_t[:, 2, :], ALU.mult), reads=[f_k, lb_k], writes=[f_k])
                        p.op('dve', lambda e: e.tensor_tensor(f_t[:], f_t[:], lb_t[:, 1, :], ALU.add), reads=[f_k, lb_k], writes=[f_k])
                        yield
                        p.op('act', lambda e: e.activation(lf_t[:], f_t[:], AF.Ln), reads=[f_k], writes=[lf_k])
                        p.op('dve', lambda e: e.tensor_scalar(kk_t[:], f_t[:], -1.0, 1.0, ALU.mult, ALU.add), reads=[f_k], writes=[kk_k])
                        yield
                        pd_t, pd_k = psD.next()
                        p.op('pe', lambda e: e.matmul(pd_t[:], am_t[:], lf_t[:], start=True, stop=True), reads=[am_k, lf_k], writes=[pd_k])
                        pc_t, pc_k = psC.next()

                        def cmm(e):
                            for h in range(4):
                                ins = e.matmul(pc_t[:, h, :], lf_t[:, h * 128:(h + 1) * 128], bs_t[:], start=True, stop=True)
                            return ins
                        p.op('pe', cmm, reads=[lf_k, bs_k], writes=[pc_k])
                        pcs_t, pcs_k = pcs.next()
                        p.op('act', lambda e: e.copy(pcs_t[:], pc_t[:]), reads=[pc_k], writes=[pcs_k])
                        yield
                        eD_t, eD_k = eD.next()
                        p.op('act', lambda e: e.activation(eD_t[:, 0, :], pd_t[:], AF.Exp), reads=[pd_k], writes=[eD_k])
                        p.op('act', lambda e: e.activation(eD_t[:, 1, :], pd_t[:], AF.Exp, scale=-1.0), reads=[pd_k], writes=[eD_k])
                        ec_t, ec_k = ecs.next()
                        dl_t, dl_k = dl.next()
                        ed_t, ed_k = edl.next()
                        pc_v = pcs_t[:].rearrange("p h (c two) -> p h c two", two=2)
                        p.op('dve', lambda e: e.tensor_tensor(dl_t[:], pc_v[:, :, :, 1], pc_v[:, :, :, 0], ALU.subtract), reads=[pcs_k], writes=[dl_k])
                        p.op('act', lambda e: e.activation(ec_t[:], pcs_t[:], AF.Exp), reads=[pcs_k], writes=[ec_k])
                        p.op('act', lambda e: e.activation(ed_t[:], dl_t[:], AF.Exp), reads=[dl_k], writes=[ed_k])
                        yield
                        qt_t, qt_k = qtl.next()
                        kt_t, kt_k = ktl.next()
                        vb_t, vb_k = vbf.next()
                        p.op('dve', lambda e: e.tensor_tensor(qt_t[:], qf_t[:], eD_t[:, 0, :], ALU.mult), reads=[qf_k, eD_k], writes=[qt_k])
                        p.op('dve', lambda e: e.tensor_tensor(kt_t[:], kk_t[:], eD_t[:, 1, :], ALU.mult), reads=[kk_k, eD_k], writes=[kt_k])
                        p.op('act', lambda e: e.copy(vb_t[:], rec_t[:, 1024:1536]), reads=[rec_k], writes=[vb_k])
                        yield
                        pq_t, pq_k = psTq.next()

                        def trq(e):
                            for h in range(4):
                                ins = e.transpose(pq_t[:, h, :], qt_t[:, h * 128:(h + 1) * 128], idb_t[:])
                            return ins
                        p.op('pe', trq, reads=[qt_k, idb_k], writes=[pq_k])
                        qT_t, qT_k = qtT.next()
                        p.op('act', lambda e: e.copy(qT_t[:], pq_t[:]), reads=[pq_k], writes=[qT_k])
                        yield
                        pk_t, pk_k = psTq.next()

                        def trk(e):
                            for h in range(4):
                                ins = e.transpose(pk_t[:, h, :], kt_t[:, h * 128:(h + 1) * 128], idb_t[:])
                            return ins
                        p.op('pe', trk, reads=[kt_k, idb_k], writes=[pk_k])
                        kT_t, kT_k = ktT.next()
                        p.op('dve', lambda e: e.tensor_copy(kT_t[:], pk_t[:]), reads=[pk_k], writes=[kT_k])
                        yield
                        at_t, at_k = AT.next()
                        po_t, po_k = psOo.next()
                        for h in range(4):
                            yield
                            hs = slice(h * 128, (h + 1) * 128)
                            psc_t, psc_k = psSc.next()
                            p.op('pe', lambda e: e.matmul(psc_t[:], kT_t[:, h, :], qT_t[:, h, :], start=True, stop=True),
                                 reads=[kT_k, qT_k], writes=[psc_k])
                            p.op('dve', lambda e: e.copy_predicated(at_t[:, h, :], cm_t[:], psc_t[:]), reads=[psc_k, cm_k], writes=[at_k])
                            sbs = []
                            for c in range(2):
                                yield
                                sb_t, sb_k = Sb.next()
                                p.op('dve', lambda e: e.tensor_scalar(sb_t[:], S_t[:, h, :], ec_t[:, h, 2 * c:2 * c + 1], None, ALU.mult),
                                     reads=[S_k, ec_k], writes=[sb_k])
                                sbs.append((sb_t, sb_k))
                                pkv_t, pkv_k = psKV.next()
                                cs = slice(c * 64, (c + 1) * 64)
                                p.op('pe', lambda e: e.matmul(pkv_t[:], kt_t[cs, hs], vb_t[cs, hs], start=True, stop=True),
                                     reads=[kt_k, vb_k], writes=[pkv_k])
                                tm_t, tm_k = tmpS.next()
                                p.op('dve', lambda e: e.tensor_scalar(tm_t[:], pkv_t[:], ed_t[:, h, c:c + 1], None, ALU.mult),
                                     reads=[pkv_k, ed_k], writes=[tm_k])
                                p.op('dve', lambda e: e.scalar_tensor_tensor(S_t[:, h, :], S_t[:, h, :], ec_t[:, h, 2 * c + 1:2 * c + 2],
                                                                             tm_t[:], ALU.mult, ALU.add),
                                     reads=[S_k, ec_k, tm_k], writes=[S_k])

                            def omm(e):
                                e.matmul(po_t[:, h, :], at_t[:, h, :], vb_t[:, hs], start=True, stop=False)
                                e.matmul(po_t[0:64, h, :], qT_t[:, h, 0:64], sbs[0][0][:], start=False, stop=True)
                                ins = e.matmul(po_t[64:128, h, :], qT_t[:, h, 64:128], sbs[1][0][:], start=False, stop=True)
                                return ins
                            p.op('pe', omm, reads=[at_k, vb_k, qT_k, sbs[0][1], sbs[1][1]], writes=[po_k])
                        yield
                        ss_t, ss_k = ssC.next()
                        for h in range(4):
                            j_t, j_k = junkC.next()
                            p.op('act', lambda e: e.activation(j_t[:], po_t[:, h, :], AF.Square, accum_out=ss_t[:, 0, h:h + 1]),
                                 reads=[po_k], writes=[j_k, ss_k])
                        p.op('dve', lambda e: e.tensor_scalar(ss_t[:, 1, :], ss_t[:, 0, :], 1.0 / 128, 1e-6, ALU.mult, ALU.add), reads=[ss_k], writes=[ss_k])
                        p.op('act', lambda e: e.activation(ss_t[:, 2, :], ss_t[:, 1, :], AF.Sqrt), reads=[ss_k], writes=[ss_k])
                        p.op('dve', lambda e: e.reciprocal(ss_t[:, 3, :], ss_t[:, 2, :]), reads=[ss_k], writes=[ss_k])
                        yield
                        p.op('dve', lambda e: e.tensor_tensor(sg_t[:], sg_t[:], lb_t[:, 3, :], ALU.mult), reads=[sg_k, lb_k], writes=[sg_k])
                        yield
                        o1_t, o1_k = o1.next()
                        p.op('dve', lambda e: e.tensor_tensor(o1_t[:], po_t[:], ss_t[:, 3, :].unsqueeze(2).to_broadcast([128, 4, 128]), ALU.mult),
                             reads=[po_k, ss_k], writes=[o1_k])
                        rb_t, rb_k = recb.next()
                        p.op('dve', lambda e: e.tensor_tensor(rb_t[:], o1_t[:].rearrange("p a b -> p (a b)"), sg_t[:], ALU.mult),
                             reads=[o1_k, sg_k], writes=[rb_k])
                        p.dma('pool', mix[r0:r0 + 128, 512:1024], rb_t[:], reads=[rb_k], writes=['mix'], sem=rb_k, lane='st', nowaw=True)

                def run_lockstep(gens):
                    live = list(gens)
                    while live:
                        nxt = []
                        for g_ in live:
                            try:
                                next(g_)
                                nxt.append(g_)
                            except StopIteration:
                                pass
                        live = nxt
                for ti in range(16):
                    for s0 in range(0, nseq, 2):
                        run_lockstep([tile_c(s_, ti) for s_ in range(s0, min(s0 + 2, nseq))])

        p.barrier()
        if "D" in stages:
            build_phase_d(nc, p, st, locals())
        p.barrier()
        p.fence('sp', reads=['out', 'mix', 'projT', 'qkT'])
        print("instructions:", p.ninst, "semaphores:", len(p.sems))
    return nc


def build_phase_d(nc, p, st, env):
    x = env["x"]; out = env["out"]; mix = env["mix"]; uvtab = env["uvtab"]; cst = env["cst"]
    w_out = env["w_out"]; norm2_g = env["norm2_g"]; normf_g = env["normf_g"]
    peer_wq = env["peer_wq"]; peer_sk = env["peer_sk"]
    idb_t, idb_k = env["idb_t"], env["idb_k"]
    NT = env["NT"]
    SG = 2
    NBUF = 10
    with ExitStack() as ph:
        wo = Tiles(p, ph, "wo", [128, 8, 1024], BF16)
        wq = Tiles(p, ph, "wq", [128, 8, 2048], BF16)
        skT = Tiles(p, ph, "skT", [128, 16, 128], BF16)
        g2b = Tiles(p, ph, "g2b", [128, 2, 1024], F32)
        io16 = Tiles(p, ph, "io16", [128, 16], F32)
        wo_t, wo_k = wo.next()
        wq_t, wq_k = wq.next()
        skT_t, skT_k = skT.next()
        g2_t, g2_k = g2b.next()
        io_t, io_k = io16.next()
        for k in range(8):
            p.dma('pool', wo_t[:, k, :], w_out[k * 128:(k + 1) * 128, :], writes=[wo_k], lane=k % 4)
            p.dma('pool', wq_t[:, k, :], peer_wq[k * 128:(k + 1) * 128, :], writes=[wq_k], lane=k % 4)
        p.dma('sp', g2_t[:, 0, :], norm2_g.partition_broadcast(128), writes=[g2_k])
        p.dma('sp', g2_t[:, 1, :], normf_g.partition_broadcast(128), writes=[g2_k], lane=1)
        p.dma('sp', io_t[:], cst["c_iota16"], writes=[io_k])
        psT = Tiles(p, ph, "psTd", [128, 8, 128], BF16, bufs=1, psum=True)
        psY = Tiles(p, ph, "psY", [128, 512], F32, bufs=1, psum=True)
        psQ = Tiles(p, ph, "psQ", [128, 512], F32, bufs=2, psum=True)
        psP = Tiles(p, ph, "psP", [128, 1024], F32, bufs=2, psum=True)
        scr = Tiles(p, ph, "scr", [128, 2048], F32, bufs=1)
        scr_t, scr_k = scr.next()
        skn_v = scr_t[:, 0:1024].bitcast(BF16).rearrange("p (g n) -> p g n", n=128)
        p.dma('pool', skn_v, peer_sk.rearrange("g n d -> n g d"), writes=[scr_k])
        for half in range(2):
            pt_t, pt_k = psT.next()

            def trs(e):
                for j in range(8):
                    ins = e.transpose(pt_t[:, j, :], skn_v[:, half * 8 + j, :], idb_t[:])
                return ins
            p.op('pe', trs, reads=[scr_k, idb_k], writes=[pt_k])
            p.op('act', lambda e: e.copy(skT_t[:, half * 8:(half + 1) * 8, :], pt_t[:]), reads=[pt_k], writes=[skT_k])

        mx = Tiles(p, ph, "mx", [128, 1024], BF16, bufs=2)
        mxT = Tiles(p, ph, "mxT", [128, 8, 128], BF16, bufs=1)
        xt = Tiles(p, ph, "xtd", [128, 1024], F32, bufs=3)
        junk = Tiles(p, ph, "junkD", [128, 1024], BF16, bufs=2)
        junk2 = Tiles(p, ph, "junkD2", [128, 1024], BF16, bufs=1)
        ssD = Tiles(p, ph, "ssD", [128, 8], F32, bufs=3)
        mhalf = Tiles(p, ph, "mhalf", [128, 1], F32)
        mh_t, mh_k = mhalf.next()
        p.op('pool', lambda e: e.memset(mh_t[:], -0.5), writes=[mh_k])
        hnb = Tiles(p, ph, "hnb", [128, 1024], BF16, bufs=2)
        hnT = Tiles(p, ph, "hnT", [128, 8, 128], BF16, bufs=1)
        qpT = Tiles(p, ph, "qpT", [128, 16, 128], BF16, bufs=1)
        ss2 = Tiles(p, ph, "ss2", [128, 128], F32, bufs=4)
        v12 = Tiles(p, ph, "v12", [128, 16, 16], F32, bufs=1)
        i12 = Tiles(p, ph, "i12", [128, 16, 16], U32, bufs=1)
        i12f = Tiles(p, ph, "i12f", [128, 16, 16], F32, bufs=1)
        cand2 = Tiles(p, ph, "cand2", [128, 256], F32, bufs=4)
        sc = Tiles(p, ph, "sc", [128, 8, 16], F32, bufs=1)
        pidx = Tiles(p, ph, "pidx", [128, 8, 16], U32, bufs=1)
        pij = Tiles(p, ph, "pij", [128, 2, 128], U32, bufs=1)
        pijf = Tiles(p, ph, "pijf", [128, 2, 128], F32, bufs=1)
        abf = Tiles(p, ph, "abf", [128, 2, 128], F32, bufs=1)
        eidf = Tiles(p, ph, "eidf", [128, 128], F32, bufs=1)
        eid = Tiles(p, ph, "eid", [128, 128], U32, bufs=2)
        gts = Tiles(p, ph, "gts", [128, 8, 16], F32, bufs=2)
        zz = Tiles(p, ph, "zz", [128, 2, 8], F32, bufs=1)
        uvb = Tiles(p, ph, "uvb", [128, SG, 2048], BF16, bufs=NBUF)
        apre = Tiles(p, ph, "apre", [128, SG], F32, bufs=NBUF)
        wgt = Tiles(p, ph, "wgt", [128, SG], F32, bufs=NBUF)
        dg = Tiles(p, ph, "dg", [128, 128], BF16, bufs=6)
        ot = Tiles(p, ph, "ot", [128, 1024], F32, bufs=1)
        OFFLOAD = False
        prod = Tiles(p, ph, "prod", [128, 1024], F32, bufs=1) if OFFLOAD else None

        def stage1(ti, ctx):
            r0 = ti * 128
            mx_t, mx_k = mx.next()
            p.dma('sp', mx_t[:], mix[r0:r0 + 128, :], reads=['mix'], writes=[mx_k])
            xt_t, xt_k = xt.next()
            p.dma('sp', xt_t[:], x[r0:r0 + 128, :], writes=[xt_k])
            pt_t, pt_k = psT.next()

            def tr1(e):
                for k in range(8):
                    ins = e.transpose(pt_t[:, k, :], mx_t[:, k * 128:(k + 1) * 128], idb_t[:])
                return ins
            p.op('pe', tr1, reads=[mx_k, idb_k], writes=[pt_k])
            mxT_t, mxT_k = mxT.next()
            p.op('act', lambda e: e.copy(mxT_t[:], pt_t[:]), reads=[pt_k], writes=[mxT_k])
            yield
            h_t, h_k = xt_t, xt_k
            for hf in range(2):
                py_t, py_k = psY.next()

                def mmy(e):
                    for k in range(8):
                        ins = e.matmul(py_t[:], mxT_t[:, k, :], wo_t[:, k, hf * 512:(hf + 1) * 512], start=(k == 0), stop=(k == 7))
                    return ins
                p.op('pe', mmy, reads=[mxT_k, wo_k], writes=[py_k])
                yield
                p.op('dve', lambda e: e.tensor_tensor(h_t[:, hf * 512:(hf + 1) * 512], py_t[:], xt_t[:, hf * 512:(hf + 1) * 512], ALU.add),
                     reads=[py_k, xt_k], writes=[h_k])
            j_t, j_k = junk2.next()
            ss_t, ss_k = ssD.next()
            yield
            p.op('act', lambda e: e.activation(j_t[:], h_t[:], AF.Square, accum_out=ss_t[:, 0:1]), reads=[h_k], writes=[j_k, ss_k])
            yield
            p.op('dve', lambda e: e.tensor_scalar(ss_t[:, 1:2], ss_t[:, 0:1], 1.0 / 1024, 1e-6, ALU.mult, ALU.add), reads=[ss_k], writes=[ss_k])
            p.op('act', lambda e: e.activation(ss_t[:, 2:3], ss_t[:, 1:2], AF.Sqrt), reads=[ss_k], writes=[ss_k])
            yield
            p.op('dve', lambda e: e.reciprocal(ss_t[:, 3:4], ss_t[:, 2:3]), reads=[ss_k], writes=[ss_k])
            hn_t, hn_k = hnb.next()
            p.op('dve', lambda e: e.scalar_tensor_tensor(hn_t[:], h_t[:], ss_t[:, 3:4], g2_t[:, 0, :], ALU.mult, ALU.mult),
                 reads=[h_k, ss_k, g2_k], writes=[hn_k])
            yield
            pt_t, pt_k = psT.next()

            def tr2(e):
                for k in range(8):
                    ins = e.transpose(pt_t[:, k, :], hn_t[:, k * 128:(k + 1) * 128], idb_t[:])
                return ins
            p.op('pe', tr2, reads=[hn_k, idb_k], writes=[pt_k])
            hnT_t, hnT_k = hnT.next()
            p.op('act', lambda e: e.copy(hnT_t[:], pt_t[:]), reads=[pt_k], writes=[hnT_k])
            yield
            qp_t, qp_k = qpT.next()
            for gq in range(4):
                pq_t, pq_k = psQ.next()

                def mmq(e):
                    for gg in range(4):
                        g = gq * 4 + gg
                        for k in range(8):
                            ins = e.matmul(pq_t[:, gg * 128:(gg + 1) * 128], wq_t[:, k, g * 128:(g + 1) * 128], hnT_t[:, k, :],
                                           start=(k == 0), stop=(k == 7))
                    return ins
                p.op('pe', mmq, reads=[wq_k, hnT_k], writes=[pq_k])
                p.op('act', lambda e: e.copy(qp_t[:, gq * 4:(gq + 1) * 4, :], pq_t[:].rearrange("p (a b) -> p a b", b=128)),
                     reads=[pq_k], writes=[qp_k])
            yield
            s_v = scr_t[:].rearrange("p (g n) -> p g n", n=128)
            for gq in range(4):
                pq_t, pq_k = psQ.next()

                def mms(e):
                    for gg in range(4):
                        g = gq * 4 + gg
                        ins = e.matmul(pq_t[:, gg * 128:(gg + 1) * 128], qp_t[:, g, :], skT_t[:, g, :], start=True, stop=True)
                    return ins
                p.op('pe', mms, reads=[qp_k, skT_k], writes=[pq_k])
                p.op('act', lambda e: e.copy(s_v[:, gq * 4:(gq + 1) * 4, :], pq_t[:].rearrange("p (a b) -> p a b", b=128)),
                     reads=[pq_k], writes=[scr_k])
            yield
            yield
            v_t, v_k = v12.next()
            i_t, i_k = i12.next()
            for g0 in range(0, 16, 4):
                gs = [g0, g0 + 1, g0 + 2, g0 + 3]
                s2s = [ss2.next() for _ in gs]
                for g in gs:
                    p.op('dve', lambda e: e.max(v_t[:, g, 0:8], s_v[:, g, :]), reads=[scr_k], writes=[(v_k, g)])
                for g in gs:
                    p.op('dve', lambda e: e.max_index(i_t[:, g, 0:8], v_t[:, g, 0:8], s_v[:, g, :]), reads=[scr_k, (v_k, g)], writes=[(i_k, g)])
                for g, (s2_t, s2_k) in zip(gs, s2s):
                    p.op('dve', lambda e: e.match_replace(s2_t[:], v_t[:, g, 0:8], s_v[:, g, :], NEG), reads=[scr_k, (v_k, g)], writes=[s2_k])
                for g, (s2_t, s2_k) in zip(gs, s2s):
                    p.op('dve', lambda e: e.max(v_t[:, g, 8:16], s2_t[:]), reads=[s2_k], writes=[(v_k, g, 1)])
                for g, (s2_t, s2_k) in zip(gs, s2s):
                    p.op('dve', lambda e: e.max_index(i_t[:, g, 8:16], v_t[:, g, 8:16], s2_t[:]), reads=[s2_k, (v_k, g, 1)], writes=[(i_k, g, 1)])
                yield
            vall = [(v_k, g) for g in range(16)] + [(v_k, g, 1) for g in range(16)]
            iall = [(i_k, g) for g in range(16)] + [(i_k, g, 1) for g in range(16)]
            if_t, if_k = i12f.next()
            p.op('dve', lambda e: e.tensor_copy(if_t[:], i_t[:]), reads=iall, writes=[if_k])
            ca_v = scr_t[:].rearrange("p (h c) -> p h c", c=256)
            v_v = v_t[:].rearrange("p (h c) k -> p h c k", c=2)
            p.op('dve', lambda e: e.tensor_tensor(ca_v.rearrange("p h (i j) -> p h i j", j=16),
                                                  v_v[:, :, 0, :].unsqueeze(3).to_broadcast([128, 8, 16, 16]),
                                                  v_v[:, :, 1, :].unsqueeze(2).to_broadcast([128, 8, 16, 16]), ALU.add),
                 reads=vall, writes=[scr_k])
            yield
            sc_t, sc_k = sc.next()
            pi_t, pi_k = pidx.next()
            for h0 in range(0, 8, 4):
                hs = [h0, h0 + 1, h0 + 2, h0 + 3]
                c2s = [cand2.next() for _ in hs]
                for h in hs:
                    p.op('dve', lambda e: e.max(sc_t[:, h, 0:8], ca_v[:, h, :]), reads=[scr_k], writes=[(sc_k, h)])
                for h in hs:
                    p.op('dve', lambda e: e.max_index(pi_t[:, h, 0:8], sc_t[:, h, 0:8], ca_v[:, h, :]), reads=[scr_k, (sc_k, h)], writes=[(pi_k, h)])
                for h, (c2_t, c2_k) in zip(hs, c2s):
                    p.op('dve', lambda e: e.match_replace(c2_t[:], sc_t[:, h, 0:8], ca_v[:, h, :], NEG), reads=[scr_k, (sc_k, h)], writes=[c2_k])
                for h, (c2_t, c2_k) in zip(hs, c2s):
                    p.op('dve', lambda e: e.max(sc_t[:, h, 8:16], c2_t[:]), reads=[c2_k], writes=[(sc_k, h, 1)])
                for h, (c2_t, c2_k) in zip(hs, c2s):
                    p.op('dve', lambda e: e.max_index(pi_t[:, h, 8:16], sc_t[:, h, 8:16], c2_t[:]), reads=[c2_k, (sc_k, h, 1)], writes=[(pi_k, h, 1)])
                yield
            scall = [(sc_k, h) for h in range(8)] + [(sc_k, h, 1) for h in range(8)]
            piall = [(pi_k, h) for h in range(8)] + [(pi_k, h, 1) for h in range(8)]
            g_t, g_k = gts.next()
            z_t, z_k = zz.next()
            p.op('dve', lambda e: e.tensor_tensor(g_t[:], sc_t[:], sc_t[:, :, 0:1].to_broadcast([128, 8, 16]), ALU.subtract),
                 reads=scall, writes=[g_k])
            yield
            for h in range(8):
                p.op('act', lambda e: e.activation(g_t[:, h, :], g_t[:, h, :], AF.Exp, accum_out=z_t[:, 0, h:h + 1]),
                     reads=[g_k], writes=[g_k, z_k])
            yield
            GATES_TAIL = True
            pij_t, pij_k = pij.next()
            pf_t, pf_k = pijf.next()
            pi_flat = pi_t[:].rearrange("p h k -> p (h k)")
            p.op('dve', lambda e: e.tensor_single_scalar(pij_t[:, 0, :], pi_flat, 4, ALU.logical_shift_right), reads=piall, writes=[pij_k])
            p.op('dve', lambda e: e.tensor_single_scalar(pij_t[:, 1, :], pi_flat, 15, ALU.bitwise_and), reads=piall, writes=[pij_k])
            p.op('dve', lambda e: e.tensor_copy(pf_t[:], pij_t[:]), reads=[pij_k], writes=[pf_k])
            ab_t, ab_k = abf.next()
            if_v = if_t[:].rearrange("p (h c) k -> p h c k", c=2)
            oh_v3 = scr_t[:].rearrange("p (s i) -> p s i", i=16)
            for c in range(2):
                p.op('dve', lambda e: e.tensor_tensor(oh_v3, io_t[:].unsqueeze(1).to_broadcast([128, 128, 16]),
                                                      pf_t[:, c, :].unsqueeze(2).to_broadcast([128, 128, 16]), ALU.is_equal),
                     reads=[io_k, pf_k], writes=[scr_k])
                oh_v = oh_v3.rearrange("p (h k) i -> p h k i", h=8)
                p.op('dve', lambda e: e.tensor_tensor(oh_v, oh_v, if_v[:, :, c, :].unsqueeze(2).to_broadcast([128, 8, 16, 16]), ALU.mult),
                     reads=[scr_k, if_k], writes=[scr_k])
                yield
                p.op('dve', lambda e: e.tensor_tensor(oh_v3[:, :, 0:8], oh_v3[:, :, 0:8], oh_v3[:, :, 8:16], ALU.add), reads=[scr_k], writes=[scr_k])
                p.op('dve', lambda e: e.tensor_tensor(oh_v3[:, :, 0:4], oh_v3[:, :, 0:4], oh_v3[:, :, 4:8], ALU.add), reads=[scr_k], writes=[scr_k])
                p.op('dve', lambda e: e.tensor_tensor(oh_v3[:, :, 0:2], oh_v3[:, :, 0:2], oh_v3[:, :, 2:4], ALU.add), reads=[scr_k], writes=[scr_k])
                p.op('dve', lambda e: e.tensor_tensor(ab_t[:, c, :], oh_v3[:, :, 0], oh_v3[:, :, 1], ALU.add), reads=[scr_k], writes=[ab_k])
                yield
            p.op('dve', lambda e: e.reciprocal(z_t[:, 1, :], z_t[:, 0, :]), reads=[z_k], writes=[z_k])
            p.op('dve', lambda e: e.tensor_tensor(g_t[:], g_t[:], z_t[:, 1, :].unsqueeze(2).to_broadcast([128, 8, 16]), ALU.mult),
                 reads=[g_k, z_k], writes=[g_k])
            ef_t, ef_k = eidf.next()
            p.op('dve', lambda e: e.scalar_tensor_tensor(ef_t[:], ab_t[:, 0, :], 128.0, ab_t[:, 1, :], ALU.mult, ALU.add),
                 reads=[ab_k], writes=[ef_k])
            ei_t, ei_k = eid.next()
            p.op('dve', lambda e: e.tensor_copy(ei_t[:], ef_t[:]), reads=[ef_k], writes=[ei_k])
            ctx.update(h=(h_t, h_k), hn=(hn_t, hn_k), ss=(ss_t, ss_k), ei=(ei_t, ei_k), g=(g_t, g_k), r0=r0)

        def advance(gen, n):
            if gen is None:
                return
            for _ in range(n):
                try:
                    next(gen)
                except StopIteration:
                    return

        def stage2(ctx, gen, prev_epi):
            h_t, h_k = ctx["h"]; hn_t, hn_k = ctx["hn"]; ss_t, ss_k = ctx["ss"]
            ei_t, ei_k = ctx["ei"]; g_t, g_k = ctx["g"]; r0 = ctx["r0"]
            pp_t, pp_k = psP.next()
            g_flat = g_t[:].rearrange("p h k -> p (h k)")
            NGRP = 128 // SG
            issued = {}

            def issue(grp):
                uv_t, uv_k = uvb.next()
                for sl in range(SG):
                    slot = grp * SG + sl
                    p.dma('pool', None, None, reads=[ei_k, 'uvtab'], writes=[uv_k], lane=sl,
                          fn=lambda e: e.indirect_dma_start(out=uv_t[:, sl, :], out_offset=None, in_=uvtab,
                                                            in_offset=bass.IndirectOffsetOnAxis(ap=ei_t[:, slot:slot + 1], axis=0)))
                issued[grp] = (uv_t, uv_k)
            PD = NBUF - 2
            for grp in range(min(PD, NGRP)):
                issue(grp)

            def post_b(grp, uv_t, uv_k, w_t, w_k):
                p.op('dve', lambda e: e.tensor_tensor(w_t[:], w_t[:], g_flat[:, grp * SG:(grp + 1) * SG], ALU.mult), reads=[w_k, g_k], writes=[w_k])
                for sl in range(SG):
                    slot = grp * SG + sl
                    dg_t, dg_k = dg.next()
                    p.op('act', lambda e: e.activation(dg_t[:], idb_t[:], AF.Copy, scale=w_t[:, sl:sl + 1]), reads=[idb_k, w_k], writes=[dg_k])

                    def mmv(e):
                        e.matmul(pp_t[:, 0:512], dg_t[:], uv_t[:, sl, 1024:1536], start=(slot == 0), stop=(slot == 127))
                        return e.matmul(pp_t[:, 512:1024], dg_t[:], uv_t[:, sl, 1536:2048], start=(slot == 0), stop=(slot == 127))
                    p.op('pe', mmv, reads=[dg_k, uv_k], writes=[pp_k])
            pend = None
            for grp in range(NGRP):
                if grp + PD < NGRP:
                    issue(grp + PD)
                uv_t, uv_k = issued.pop(grp)
                ap_t, ap_k = apre.next()
                w_t, w_k = wgt.next()
                for sl in range(SG):
                    if sl == SG - 1 and OFFLOAD:
                        pr_t, pr_k = prod.next()
                        p.op('pool', lambda e: e.tensor_tensor(pr_t[:], uv_t[:, sl, 0:1024], hn_t[:], ALU.mult), reads=[uv_k, hn_k], writes=[pr_k])
                        jj_t, jj_k = junk2.next()
                        p.op('act', lambda e: e.activation(jj_t[:], pr_t[:], AF.Copy, accum_out=ap_t[:, sl:sl + 1]),
                             reads=[pr_k], writes=[jj_k, (ap_k, 1)])
                        continue
                    jj_t, jj_k = junk.next()
                    p.op('dve', lambda e: e.scalar_tensor_tensor(jj_t[:], uv_t[:, sl, 0:1024], 1.0, hn_t[:], ALU.mult, ALU.mult,
                                                                 accum_out=ap_t[:, sl:sl + 1]),
                         reads=[uv_k, hn_k], writes=[(ap_k, 'c', sl)])
                p.op('act', lambda e: e.activation(w_t[:], ap_t[:], AF.Gelu), reads=[(ap_k, 'c', sl_) for sl_ in range(SG)], writes=[w_k])
                if pend is not None:
                    post_b(*pend)
                pend = (grp, uv_t, uv_k, w_t, w_k)
                if grp == 3 and prev_epi is not None:
                    prev_epi()
                advance(gen, 1)
            post_b(*pend)

            def epilogue():
                p.op('dve', lambda e: e.tensor_tensor(h_t[:], pp_t[:], h_t[:], ALU.add), reads=[pp_k, h_k], writes=[h_k])
                j_t, j_k = junk2.next()
                p.op('act', lambda e: e.activation(j_t[:], h_t[:], AF.Square, accum_out=ss_t[:, 4:5]), reads=[h_k], writes=[j_k, ss_k])
                p.op('dve', lambda e: e.tensor_scalar(ss_t[:, 5:6], ss_t[:, 4:5], 1.0 / 1024, 1e-6, ALU.mult, ALU.add), reads=[ss_k], writes=[ss_k])
                p.op('act', lambda e: e.activation(ss_t[:, 6:7], ss_t[:, 5:6], AF.Sqrt), reads=[ss_k], writes=[ss_k])
                p.op('dve', lambda e: e.reciprocal(ss_t[:, 7:8], ss_t[:, 6:7]), reads=[ss_k], writes=[ss_k])
                o_t, o_k = ot.next()
                p.op('dve', lambda e: e.scalar_tensor_tensor(o_t[:], h_t[:], ss_t[:, 7:8], g2_t[:, 1, :], ALU.mult, ALU.mult),
                     reads=[h_k, ss_k, g2_k], writes=[o_k])
                p.dma('sp', out[r0:r0 + 128, :], o_t[:], reads=[o_k], writes=['out'], sem=o_k, lane='st', nowaw=True)
            return epilogue

        ctxs = [dict() for _ in range(NT)]
        g0 = stage1(0, ctxs[0])
        advance(g0, 10 ** 6)
        epi = None
        for ti in range(NT):
            gen = stage1(ti + 1, ctxs[ti + 1]) if ti + 1 < NT else None
            epi = stage2(ctxs[ti], gen, epi)
            advance(gen, 10 ** 6)
        epi()


_CONSTS = None


def kernel(x, norm1_g, w_in, rec_lb_logits, rec_norm_g, w_out, norm2_g, peer_wq, peer_subkeys, peer_u, peer_v, normf_g):
    global _CONSTS
    if _CONSTS is None:
        _CONSTS = make_consts()
    n = 8
    x = np.asarray(x, np.float32)
    B = x.shape[0]
    per = B // n
    shared = {
        "norm1_g": np.ascontiguousarray(np.asarray(norm1_g, np.float32).reshape(1, 1024)),
        "w_in": np.ascontiguousarray(np.asarray(w_in, np.float32).reshape(1024, 3584)),
        "rec_lb_logits": np.ascontiguousarray(np.asarray(rec_lb_logits, np.float32).reshape(2, 512)),
        "rec_norm_g": np.ascontiguousarray(np.asarray(rec_norm_g, np.float32).reshape(1, 512)),
        "w_out": np.ascontiguousarray(np.asarray(w_out, np.float32).reshape(1024, 1024)),
        "norm2_g": np.ascontiguousarray(np.asarray(norm2_g, np.float32).reshape(1, 1024)),
        "peer_wq": np.ascontiguousarray(np.asarray(peer_wq, np.float32).reshape(1024, 2048)),
        "peer_subkeys": np.ascontiguousarray(np.asarray(peer_subkeys, np.float32).reshape(16, 128, 128)),
        "peer_u": np.ascontiguousarray(np.asarray(peer_u, np.float32).reshape(16384, 1024)),
        "peer_v": np.ascontiguousarray(np.asarray(peer_v, np.float32).reshape(16384, 1024)),
        "normf_g": np.ascontiguousarray(np.asarray(normf_g, np.float32).reshape(1, 1024)),
    }
    shared.update(_CONSTS)
    nc = build(nseq=per)
    in_maps = []
    for i in range(n):
        m = dict(shared)
        m["x"] = np.ascontiguousarray(x[i * per:(i + 1) * per].reshape(per * 2048, 1024))
        in_maps.append(m)
    res = run_bass_kernel_spmd(nc, in_maps, core_ids=list(range(n)))
    outs = [np.asarray(r["out"], np.float32).reshape(per, 2048, 1024) for r in res.results]
    return np.concatenate(outs, axis=0)
```

```python
import numpy as np
from contextlib import ExitStack
import concourse.bass as bass
import concourse.mybir as mybir
from concourse.bass_utils import run_bass_kernel_spmd

F32 = mybir.dt.float32
BF16 = mybir.dt.bfloat16
U32 = mybir.dt.uint32
I32 = mybir.dt.int32
AF = mybir.ActivationFunctionType
ALU = mybir.AluOpType
AX = mybir.AxisListType

SEM_LIMIT = 30000
NEG = -1.0e30
MASKV = 32768.0


class Prog:
    ENG = ('pe', 'dve', 'act', 'pool', 'sp')

    def __init__(self, nc, stack):
        self.nc = nc
        self.stack = stack
        self.eng = {'pe': nc.tensor, 'dve': nc.vector, 'act': nc.scalar, 'pool': nc.gpsimd, 'sp': nc.sync}
        self.semcnt = {}
        self.sems = {}
        self.seen = {e: {} for e in self.ENG}
        self.res = {}
        self.ninst = 0

    def _sem(self, key):
        if key not in self.sems:
            self.sems[key] = self.stack.enter_context(self.nc.semaphore(f"s{len(self.sems)}"))
        return self.sems[key]

    def _bump(self, name, inc):
        ep, val = self.semcnt.get(name, (0, 0))
        if val + inc > SEM_LIMIT:
            ep += 1
            val = 0
        val += inc
        self.semcnt[name] = (ep, val)
        return ((name, ep), val)

    @staticmethod
    def _sibling(semname, w):
        return isinstance(semname, tuple) and len(semname) >= 2 and semname[0] == 'd' and semname[1] == w

    def _deps(self, reads, writes, myname, group, nowaw=False):
        d = {}

        def add(k, v):
            if d.get(k, 0) < v:
                d[k] = v
        for r in reads:
            st = self.res.get(r)
            if st:
                for (k, v) in st[0].values():
                    add(k, v)
        self._saved = {}
        for w in writes:
            st = self.res.get(w)
            sv = {}
            if st:
                sib = group and len(st[0]) > 0 and all(self._sibling(n, w) for n in st[0]) and self._sibling(myname, w)
                if sib or nowaw:
                    sv = dict(st[2])
                else:
                    for (k, v) in st[0].values():
                        sv[k] = max(sv.get(k, 0), v)
                for k, v in st[1].items():
                    sv[k] = max(sv.get(k, 0), v)
                for k, v in sv.items():
                    add(k, v)
            self._saved[w] = sv
        return d

    def _waits(self, eng, d):
        e = self.eng[eng]
        for k, v in d.items():
            if eng == 'pe' and k[0] == 'pe':
                continue
            if self.seen[eng].get(k, 0) >= v:
                continue
            self.seen[eng][k] = v
            e.wait_ge(self._sem(k), v)

    def _update(self, reads, writes, myname, mykey, myval, group, nowaw=False):
        for r in reads:
            st = self.res.setdefault(r, [{}, {}, {}])
            st[1][mykey] = myval
        for w in writes:
            st = self.res.get(w)
            if st and (nowaw or (group and len(st[0]) > 0 and all(self._sibling(n, w) for n in st[0]) and self._sibling(myname, w))):
                wr = dict(st[0])
            else:
                wr = {}
            wr[myname] = (mykey, myval)
            self.res[w] = [wr, {}, self._saved.get(w, {})]

    def op(self, eng, fn, reads=(), writes=()):
        mykey, myval = self._bump(eng, 1)
        d = self._deps(reads, writes, eng, False)
        self._waits(eng, d)
        ins = fn(self.eng[eng])
        ins.then_inc(self._sem(mykey), 1)
        self._update(reads, writes, eng, mykey, myval, False)
        self.ninst += 1
        return ins

    def dma(self, qeng, out, in_, reads=(), writes=(), sem=None, lane=0, group=True, fn=None, nowaw=False, **kw):
        semname = ('d', sem if sem is not None else writes[0], lane)
        prev = self.semcnt.get(semname)
        mykey, myval = self._bump(semname, 16)
        d = self._deps(reads, writes, semname, group, nowaw)
        if prev is not None and prev[1] > 0:
            pk = (semname, prev[0])
            d[pk] = max(d.get(pk, 0), prev[1])
        self._waits(qeng, d)
        e = self.eng[qeng]
        if fn is not None:
            ins = fn(e)
        else:
            ins = e.dma_start(out=out, in_=in_, **kw)
        ins.then_inc(self._sem(mykey), 16)
        self._update(reads, writes, semname, mykey, myval, group, nowaw)
        self.ninst += 1
        return ins

    def barrier(self):
        snap = {(name, ep): val for name, (ep, val) in self.semcnt.items()}
        for eng in self.ENG:
            self._waits(eng, snap)

    def fence(self, eng, reads=(), writes=()):
        d = self._deps(reads, writes, None, False)
        self._waits(eng, d)


class Tiles:
    def __init__(self, p, stack, name, shape, dtype, bufs=1, psum=False):
        alloc = p.nc.psum_tensor if psum else p.nc.sbuf_tensor
        self.t = [stack.enter_context(alloc(f"{name}_{i}", shape, dtype)) for i in range(bufs)]
        self.name = name
        self.bufs = bufs
        self.i = -1

    def next(self):
        self.i += 1
        return self.cur()

    def cur(self):
        b = self.i % self.bufs
        return self.t[b], (self.name, b)


class Views:
    def __init__(self, items):
        self.items = items
        self.i = -1

    def next(self):
        self.i += 1
        return self.items[self.i % len(self.items)]


def make_consts():
    c = {}
    c["c_ident"] = np.eye(128, dtype=np.float32)
    kq = np.arange(128)
    c["c_tri"] = np.where(kq[:, None] > kq[None, :], -MASKV, 0.0).astype(np.float32)
    pos = np.arange(2048)
    blk = pos // 256
    r = pos % 256
    kaug = np.zeros((8, 12, 2048), np.float32)
    qaug = np.zeros((8, 4, 2048), np.float32)
    for h in range(8):
        sl = 2.0 ** (-(h + 1))
        for n in range(8):
            kaug[h, n] = MASKV * (blk == n)
        kaug[h, 8] = sl * r
        kaug[h, 9] = 1.0
        kaug[h, 10] = sl * 256.0 * blk
        kaug[h, 11] = 1.0
        qaug[h, 0] = 1.0
        qaug[h, 1] = -sl * r
        qaug[h, 2] = 1.0
        qaug[h, 3] = -sl * 256.0 * blk
    c["c_kaug"] = kaug
    c["c_qaug"] = qaug
    el = np.zeros((3, 16, 8), np.float32)
    for qt in range(16):
        j = qt // 2
        for n in range(8):
            el[0, qt, n] = 0.0 if n < j else NEG
            el[1, qt, n] = 1.0 if n < j else 0.0
            el[2, qt, n] = 1.0 if n == j else 0.0
    c["c_elig"] = np.broadcast_to(el.reshape(1, 3 * 16 * 8), (128, 384)).copy()
    s = np.arange(128)
    ch = s // 64
    mid = ch * 64 + 31
    same = ch[:, None] == ch[None, :]
    amat = (same & (s[:, None] <= s[None, :])).astype(np.float32) - (same & (s[:, None] <= mid[None, :])).astype(np.float32)
    c["c_amat"] = amat.astype(np.float32)
    bsel = np.zeros((128, 4), np.float32)
    bsel[:, 0] = (s <= 31)
    bsel[:, 1] = (s <= 63)
    bsel[:, 2] = (s >= 64) & (s <= 95)
    bsel[:, 3] = (s >= 64)
    c["c_bsel"] = bsel
    c["c_cmask"] = (same & (s[:, None] <= s[None, :])).astype(np.int32)
    c["c_iota16"] = np.broadcast_to(np.arange(16, dtype=np.float32)[None, :], (128, 16)).copy()
    return c


CONST_SHAPES = {"c_ident": ([128, 128], F32), "c_tri": ([128, 128], F32), "c_kaug": ([8, 12, 2048], F32),
                "c_qaug": ([8, 4, 2048], F32), "c_elig": ([128, 384], F32), "c_amat": ([128, 128], F32),
                "c_bsel": ([128, 4], F32), "c_cmask": ([128, 128], I32), "c_iota16": ([128, 16], F32)}


def build(nseq=4, dbg=False, stages="0ABCD"):
    T = nseq * 2048
    NT = T // 128
    NG = T // 512
    nc = bass.Bass("TRN2", target_bir_lowering=False)

    def din(name, shape, dt=F32):
        return nc.dram_tensor(name, shape, dt, kind="ExternalInput").ap()

    x = din("x", [T, 1024])
    norm1_g = din("norm1_g", [1, 1024])
    w_in = din("w_in", [1024, 3584])
    rec_lb = din("rec_lb_logits", [2, 512])
    rec_norm_g = din("rec_norm_g", [1, 512])
    w_out = din("w_out", [1024, 1024])
    norm2_g = din("norm2_g", [1, 1024])
    peer_wq = din("peer_wq", [1024, 2048])
    peer_sk = din("peer_subkeys", [16, 128, 128])
    peer_u = din("peer_u", [16384, 1024])
    peer_v = din("peer_v", [16384, 1024])
    normf_g = din("normf_g", [1, 1024])
    cst = {k: din(k, sh, dt) for k, (sh, dt) in CONST_SHAPES.items()}
    out = nc.dram_tensor("out", [T, 1024], F32, kind="ExternalOutput").ap()
    skind = "ExternalOutput" if dbg else "Internal"
    projT = nc.dram_tensor("projT", [T, 2560], F32, kind=skind).ap()
    qkT = nc.dram_tensor("qkT", [16, 64, T], BF16, kind=skind).ap()
    mix = nc.dram_tensor("mix", [T, 1024], BF16, kind=skind).ap()
    uvtab = nc.dram_tensor("uvtab", [16384, 2048], BF16, kind="Internal").ap()

    with ExitStack() as st:
        p = Prog(nc, st)
        identf = Tiles(p, st, "identf", [128, 128], F32)
        identb = Tiles(p, st, "identb", [128, 128], BF16)
        idf_t, idf_k = identf.next()
        idb_t, idb_k = identb.next()
        p.dma('sp', idf_t[:], cst["c_ident"], writes=[idf_k])
        p.op('dve', lambda e: e.tensor_copy(idb_t[:], idf_t[:]), reads=[idf_k], writes=[idb_k])
        ksum = Tiles(p, st, "ksum", [64, 8, NG * 2], F32)
        ksum_t, ksum_k = ksum.next()

        p.barrier()
        if "A" in stages:
            with ExitStack() as ph:
                win = Tiles(p, ph, "win", [128, 8, 3584], BF16)
                win_t, win_k = win.next()
                for k in range(8):
                    for hf in range(2):
                        p.dma('pool', win_t[:, k, hf * 1792:(hf + 1) * 1792],
                              w_in[k * 128:(k + 1) * 128, hf * 1792:(hf + 1) * 1792], writes=[win_k], lane=(2 * k + hf) % 4)
                g1b = Tiles(p, ph, "g1b", [128, 1024], F32)
                g1_t, g1_k = g1b.next()
                p.dma('sp', g1_t[:], norm1_g.partition_broadcast(128), writes=[g1_k])
                xt = Tiles(p, ph, "xt", [128, 1024], F32, bufs=3)
                junk = Tiles(p, ph, "junkA", [128, 1024], BF16, bufs=2)
                ssA = Tiles(p, ph, "ssA", [128, 4], F32, bufs=4)
                xn = Tiles(p, ph, "xn", [128, 1024], BF16, bufs=2)
                xnT = Tiles(p, ph, "xnT", [128, 8, 512], BF16, bufs=2)
                tok = Tiles(p, ph, "tok", [128, 2560], F32, bufs=2)
                qkst = Tiles(p, ph, "qkst", [128, 8, 512], BF16, bufs=2)
                psT = Tiles(p, ph, "psT", [128, 8, 128], BF16, bufs=1, psum=True)
                psA = Tiles(p, ph, "psA", [128, 512], F32, bufs=6, psum=True)
                ev = 0
                def prep_a(gi, ctx):
                    xnT_t, xnT_k = xnT.next()
                    for ti in range(4):
                        r0 = gi * 512 + ti * 128
                        xt_t, xt_k = xt.next()
                        p.dma('sp', xt_t[:], x[r0:r0 + 128, :], writes=[xt_k])
                        j_t, j_k = junk.next()
                        ss_t, ss_k = ssA.next()
                        p.op('act', lambda e: e.activation(j_t[:], xt_t[:], AF.Square, accum_out=ss_t[:, 0:1]),
                             reads=[xt_k], writes=[j_k, ss_k])
                        p.op('dve', lambda e: e.tensor_scalar(ss_t[:, 1:2], ss_t[:, 0:1], 1.0 / 1024, 1e-6, ALU.mult, ALU.add),
                             reads=[ss_k], writes=[ss_k])
                        p.op('act', lambda e: e.activation(ss_t[:, 2:3], ss_t[:, 1:2], AF.Sqrt), reads=[ss_k], writes=[ss_k])
                        p.op('dve', lambda e: e.reciprocal(ss_t[:, 3:4], ss_t[:, 2:3]), reads=[ss_k], writes=[ss_k])
                        xn_t, xn_k = xn.next()
                        p.op('dve', lambda e: e.scalar_tensor_tensor(xn_t[:], xt_t[:], ss_t[:, 3:4], g1_t[:], ALU.mult, ALU.mult),
                             reads=[xt_k, ss_k, g1_k], writes=[xn_k])
                        yield
                        pst_t, pst_k = psT.next()

                        def tr(e):
                            for k in range(8):
                                ins = e.transpose(pst_t[:, k, :], xn_t[:, k * 128:(k + 1) * 128], idb_t[:])
                            return ins
                        p.op('pe', tr, reads=[xn_k, idb_k], writes=[pst_k])
                        p.op('act', lambda e: e.copy(xnT_t[:, :, ti * 128:(ti + 1) * 128], pst_t[:]),
                             reads=[pst_k], writes=[xnT_k])
                        yield
                    ctx["xnT"] = (xnT_t, xnT_k)
                    yield

                def adv_a(gen, n):
                    if gen is None:
                        return
                    for _ in range(n):
                        try:
                            next(gen)
                        except StopIteration:
                            return
                NGA = NG
                ctxa = [dict() for _ in range(NGA + 1)]
                gen_a = prep_a(0, ctxa[0])
                adv_a(gen_a, 100)
                for gi in range(NGA):
                    gen_a = prep_a(gi + 1, ctxa[gi + 1]) if gi + 1 < NGA else None
                    xnT_t, xnT_k = ctxa[gi]["xnT"]
                    for ti in range(4):
                        r0 = gi * 512 + ti * 128
                        tok_t, tok_k = tok.next()
                        for cg in range(5):
                            ps_t, ps_k = psA.next()

                            def mm(e):
                                for k in range(8):
                                    ins = e.matmul(ps_t[:], xnT_t[:, k, ti * 128:(ti + 1) * 128],
                                                   win_t[:, k, 1024 + cg * 512:1024 + (cg + 1) * 512],
                                                   start=(k == 0), stop=(k == 7))
                                return ins
                            p.op('pe', mm, reads=[xnT_k, win_k], writes=[ps_k])
                            ev += 1
                            if ev % 2:
                                p.op('act', lambda e: e.copy(tok_t[:, cg * 512:(cg + 1) * 512], ps_t[:]),
                                     reads=[ps_k], writes=[tok_k])
                            else:
                                p.op('dve', lambda e: e.tensor_copy(tok_t[:, cg * 512:(cg + 1) * 512], ps_t[:]),
                                     reads=[ps_k], writes=[tok_k])
                        p.dma('pool', projT[r0:r0 + 128, :], tok_t[:], reads=[tok_k], writes=['projT'], sem=tok_k, lane='st', nowaw=True)
                        adv_a(gen_a, 1)
                    qk_t, qk_k = qkst.next()
                    for i2 in range(8):
                        adv_a(gen_a, 1)
                        ps_t, ps_k = psA.next()

                        def mm2(e):
                            for k in range(8):
                                ins = e.matmul(ps_t[:], win_t[:, k, i2 * 128:(i2 + 1) * 128], xnT_t[:, k, :],
                                               start=(k == 0), stop=(k == 7))
                            return ins
                        p.op('pe', mm2, reads=[xnT_k, win_k], writes=[ps_k])
                        if i2 < 4:
                            p.op('act', lambda e: e.mul(qk_t[:, i2, :], ps_t[:], 0.125), reads=[ps_k], writes=[qk_k])
                        else:
                            p.op('act', lambda e: e.copy(qk_t[:, i2, :], ps_t[:]), reads=[ps_k], writes=[qk_k])
                    p.dma('pool', qkT[:, :, gi * 512:(gi + 1) * 512].rearrange("(i2 two) d t -> (two d) i2 t", two=2), qk_t[:],
                          reads=[qk_k], writes=['qkT'], sem=qk_k, lane='st', nowaw=True)
                    if gi == 1 and "0" in stages:
                        for blk in range(4):
                            rows = slice(blk * 4096, (blk + 1) * 4096)
                            p.dma('pool', uvtab[rows, 0:1024], peer_u[rows, :], writes=['uvtab'], sem=('uvt', blk), lane=0, nowaw=True)
                            p.dma('pool', uvtab[rows, 1024:2048], peer_v[rows, :], writes=['uvtab'], sem=('uvt', blk), lane=1, nowaw=True)
                    adv_a(gen_a, 100)

        p.barrier()
        if "B" in stages:
            with ExitStack() as ph:
                qTa = Tiles(p, ph, "qTa", [76, 2048], BF16, bufs=2)
                kTa = Tiles(p, ph, "kTa", [76, 2048], BF16, bufs=2)
                vaug = Tiles(p, ph, "vaug", [128, 16, 65], BF16, bufs=2)
                kmb = Tiles(p, ph, "kmb", [64, 8], BF16, bufs=2)
                kmf = Tiles(p, ph, "kmf", [64, 8], F32, bufs=2)
                kjunk = Tiles(p, ph, "kjunk", [64, 256], BF16, bufs=2)
                zer = Tiles(p, ph, "zer", [64, 256], BF16)
                zer_t, zer_k = zer.next()
                p.op('pool', lambda e: e.memset(zer_t[:], 0.0), writes=[zer_k])
                elig = Tiles(p, ph, "elig", [128, 3, 16, 8], F32)
                tri = Tiles(p, ph, "tri", [128, 128], BF16)
                gm = Tiles(p, ph, "gm", [128, 16, 8], F32, bufs=2)
                m8 = Tiles(p, ph, "m8", [128, 16, 8], F32, bufs=2)
                selt = Tiles(p, ph, "selt", [128, 16, 8], F32, bufs=2)
                biast = Tiles(p, ph, "biast", [128, 16, 8], BF16, bufs=2)
                bT = Tiles(p, ph, "bT", [8, 2048], BF16, bufs=2)
                pT = Tiles(p, ph, "pT", [128, 16, 512], BF16, bufs=2)
                osb = Tiles(p, ph, "osb", [128, 4, 64], BF16, bufs=3)
                rc = Tiles(p, ph, "rc", [128, 4], F32, bufs=3)
                psS = Tiles(p, ph, "psS", [128, 512], F32, bufs=3, psum=True)
                psO = Tiles(p, ph, "psO", [128, 512], F32, bufs=2, psum=True)
                psG = Tiles(p, ph, "psG", [128, 16, 8], F32, bufs=1, psum=True)
                psB = Tiles(p, ph, "psB", [128, 2048], BF16, bufs=1, psum=True)
                el_t, el_k = elig.next()
                p.dma('sp', el_t[:].rearrange("p a b c -> p (a b c)"), cst["c_elig"], writes=[el_k])
                tri_t, tri_k = tri.next()
                p.dma('pool', tri_t[:], cst["c_tri"], writes=[tri_k])
                for b in range(2):
                    va_t, va_k = vaug.next()
                    p.op('pool', lambda e: e.memset(va_t[:, :, 64:65], 1.0), writes=[va_k])
                def advance_b(gen, n):
                    if gen is None:
                        return
                    for _ in range(n):
                        try:
                            next(gen)
                        except StopIteration:
                            return

                def prologue(s, h, ctx):
                    t0 = s * 2048
                    if True:
                        q_t, q_k = qTa.next()
                        k_t, k_k = kTa.next()
                        va_t, va_k = vaug.next()
                        p.dma('sp', q_t[0:64, :], qkT[h, :, t0:t0 + 2048], reads=['qkT'], writes=[q_k])
                        p.dma('sp', k_t[0:64, :], qkT[8 + h, :, t0:t0 + 2048], reads=['qkT'], writes=[k_k])
                        p.dma('pool', k_t[64:76, :], cst["c_kaug"][h], writes=[k_k], lane=1)
                        p.dma('pool', q_t[72:76, :], cst["c_qaug"][h], writes=[q_k], lane=1)
                        p.dma('pool', va_t[:, :, 0:64],
                              projT[t0:t0 + 2048, h * 64:(h + 1) * 64].rearrange("(kt p) c -> p kt c", p=128),
                              reads=['projT'], writes=[va_k])
                        yield
                        km_t, km_k = kmb.next()
                        kf_t, kf_k = kmf.next()
                        for n in range(8):
                            kj_t, kj_k = kjunk.next()
                            p.op('dve', lambda e: e.scalar_tensor_tensor(kj_t[:], k_t[0:64, n * 256:(n + 1) * 256], 1.0 / 256, zer_t[:],
                                                                         ALU.mult, ALU.add, accum_out=kf_t[:, n:n + 1]),
                                 reads=[k_k, zer_k], writes=[(kf_k, n)])
                        p.op('dve', lambda e: e.tensor_copy(km_t[:], kf_t[:]), reads=[(kf_k, n_) for n_ in range(8)], writes=[km_k])
                        pg_t, pg_k = psG.next()

                        def gmm(e):
                            for qt in range(16):
                                ins = e.matmul(pg_t[:, qt, :], q_t[0:64, qt * 128:(qt + 1) * 128], km_t[:], start=True, stop=True)
                            return ins
                        p.op('pe', gmm, reads=[q_k, km_k], writes=[pg_k])
                        yield
                        gm_t, gm_k = gm.next()
                        m8_t, m8_k = m8.next()
                        se_t, se_k = selt.next()
                        bi_t, bi_k = biast.next()
                        p.op('dve', lambda e: e.tensor_tensor(gm_t[:], pg_t[:], el_t[:, 0], ALU.add), reads=[pg_k, el_k], writes=[gm_k])
                        for qt in range(16):
                            p.op('dve', lambda e: e.max(m8_t[:, qt, :], gm_t[:, qt, :]), reads=[gm_k], writes=[m8_k])
                        yield
                        p.op('dve', lambda e: e.tensor_tensor(se_t[:], gm_t[:], m8_t[:, :, 2:3].to_broadcast([128, 16, 8]), ALU.is_ge),
                             reads=[gm_k, m8_k], writes=[se_k])
                        p.op('dve', lambda e: e.tensor_tensor(se_t[:], se_t[:], el_t[:, 1], ALU.mult), reads=[se_k, el_k], writes=[se_k])
                        p.op('dve', lambda e: e.scalar_tensor_tensor(bi_t[:], se_t[:], -1.0, el_t[:, 2], ALU.add, ALU.add),
                             reads=[se_k, el_k], writes=[bi_k])
                        yield
                        pb_t, pb_k = psB.next()

                        def btr(e):
                            for qt in range(16):
                                ins = e.transpose(pb_t[0:8, qt * 128:(qt + 1) * 128], bi_t[:, qt, :], idb_t[:])
                            return ins
                        p.op('pe', btr, reads=[bi_k, idb_k], writes=[pb_k])
                        bT_t, bT_k = bT.next()
                        p.op('act', lambda e: e.copy(bT_t[:], pb_t[0:8, :]), reads=[pb_k], writes=[bT_k])
                        p.dma('sp', q_t[64:72, :], bT_t[:], reads=[bT_k], writes=[q_k], lane=2)
                        ctx.update(q=(q_t, q_k), k=(k_t, k_k), va=(va_t, va_k), t0=t0, h=h)
                    yield

                def attention(ctx, gen):
                    q_t, q_k = ctx["q"]; k_t, k_k = ctx["k"]; va_t, va_k = ctx["va"]; t0 = ctx["t0"]; h = ctx["h"]
                    if True:
                        for g in range(4):
                            pT_t, pT_k = pT.next()
                            for kt in range(4 * g + 4):
                                c0 = max(0, kt * 128 - g * 512)
                                ps_t, ps_k = psS.next()
                                diag = kt >= 4 * g

                                def smm(e):
                                    ins = e.matmul(ps_t[:, c0:512], k_t[0:76, kt * 128:(kt + 1) * 128],
                                                   q_t[0:76, g * 512 + c0:(g + 1) * 512], start=True, stop=not diag)
                                    if diag:
                                        ins = e.matmul(ps_t[:, c0:c0 + 128], idb_t[:], tri_t[:], start=False, stop=True)
                                    return ins
                                p.op('pe', smm, reads=[k_k, q_k, idb_k, tri_k], writes=[ps_k])
                                p.op('act', lambda e: e.activation(pT_t[:, kt, c0:512], ps_t[:, c0:512], AF.Exp),
                                     reads=[ps_k], writes=[pT_k])
                                if kt % 4 == 3:
                                    advance_b(gen, 1)
                            po_t, po_k = psO.next()
                            po_v = po_t[:, 0:260].rearrange("p (a b) -> p a b", b=65)

                            def pv(e):
                                for qs in range(4):
                                    qt = 4 * g + qs
                                    for kt in range(qt + 1):
                                        ins = e.matmul(po_v[:, qs, :], pT_t[:, kt, qs * 128:(qs + 1) * 128], va_t[:, kt, :],
                                                       start=(kt == 0), stop=(kt == qt))
                                return ins
                            p.op('pe', pv, reads=[pT_k, va_k], writes=[po_k])
                            rc_t, rc_k = rc.next()
                            os_t, os_k = osb.next()
                            p.op('dve', lambda e: e.reciprocal(rc_t[:], po_v[:, :, 64]), reads=[po_k], writes=[rc_k])
                            p.op('dve', lambda e: e.tensor_tensor(os_t[:], po_v[:, :, 0:64],
                                                                  rc_t[:].unsqueeze(2).to_broadcast([128, 4, 64]), ALU.mult),
                                 reads=[po_k, rc_k], writes=[os_k])
                            r0 = t0 + g * 512
                            p.dma('sp', mix[r0:r0 + 512, h * 64:(h + 1) * 64].rearrange("(qs p) c -> p qs c", p=128), os_t[:],
                                  reads=[os_k], writes=['mix'], sem=os_k, lane='st', nowaw=True)

                units = [(s_, h_) for s_ in range(nseq) for h_ in range(8)]
                ctxb = [dict() for _ in units]
                g0 = prologue(units[0][0], units[0][1], ctxb[0])
                advance_b(g0, 100)
                for ui in range(len(units)):
                    gen = prologue(units[ui + 1][0], units[ui + 1][1], ctxb[ui + 1]) if ui + 1 < len(units) else None
                    attention(ctxb[ui], gen)
                    advance_b(gen, 100)

        p.barrier()
        if "C" in stages:
            with ExitStack() as ph:
                amat = Tiles(p, ph, "amat", [128, 128], F32)
                bsel = Tiles(p, ph, "bsel", [128, 4], F32)
                cmask = Tiles(p, ph, "cmask", [128, 128], I32)
                lbb = Tiles(p, ph, "lbb", [128, 4, 512], F32)
                am_t, am_k = amat.next()
                bs_t, bs_k = bsel.next()
                cm_t, cm_k = cmask.next()
                lb_t, lb_k = lbb.next()
                p.dma('sp', am_t[:], cst["c_amat"], writes=[am_k])
                p.dma('sp', bs_t[:], cst["c_bsel"], writes=[bs_k])
                p.dma('sp', cm_t[:], cst["c_cmask"], writes=[cm_k])
                p.dma('sp', lb_t[:, 0, :], rec_lb[0:1, :].partition_broadcast(128), writes=[lb_k])
                p.dma('sp', lb_t[:, 1, :], rec_lb[1:2, :].partition_broadcast(128), writes=[lb_k], lane=1)
                p.dma('sp', lb_t[:, 3, :], rec_norm_g.partition_broadcast(128), writes=[lb_k], lane=2)
                p.op('dve', lambda e: e.tensor_tensor(lb_t[:, 0, :], lb_t[:, 0, :], lb_t[:, 1, :], ALU.subtract), reads=[lb_k], writes=[lb_k])
                p.op('act', lambda e: e.activation(lb_t[:, 1, :], lb_t[:, 0, :], AF.Sigmoid), reads=[lb_k], writes=[lb_k])
                p.op('dve', lambda e: e.tensor_scalar(lb_t[:, 2, :], lb_t[:, 1, :], -1.0, 1.0, ALU.mult, ALU.add), reads=[lb_k], writes=[lb_k])
                rec = Tiles(p, ph, "rec", [128, 2048], F32, bufs=2)
                fT = Tiles(p, ph, "fT", [128, 512], F32, bufs=2)
                lf = Tiles(p, ph, "lf", [128, 512], F32, bufs=2)
                kkT = Tiles(p, ph, "kkT", [128, 512], F32, bufs=2)
                qfT = Tiles(p, ph, "qfT", [128, 512], F32, bufs=2)
                eD = Tiles(p, ph, "eD", [128, 2, 512], F32, bufs=2)
                qtl = Tiles(p, ph, "qtl", [128, 512], BF16, bufs=2)
                ktl = Tiles(p, ph, "ktl", [128, 512], BF16, bufs=2)
                vbf = Tiles(p, ph, "vbf", [128, 512], BF16, bufs=2)
                qtT = Tiles(p, ph, "qtT", [128, 4, 128], BF16, bufs=2)
                ktT = Tiles(p, ph, "ktT", [128, 4, 128], BF16, bufs=2)
                ecs = Tiles(p, ph, "ecs", [128, 4, 4], F32, bufs=2)
                dl = Tiles(p, ph, "dl", [128, 4, 2], F32, bufs=2)
                pcs = Tiles(p, ph, "pcs", [128, 4, 4], F32, bufs=2)
                edl = Tiles(p, ph, "edl", [128, 4, 2], F32, bufs=2)
                AT = Tiles(p, ph, "AT", [128, 4, 128], BF16, bufs=2)
                Sst = Tiles(p, ph, "Sst", [128, 4, 128], F32, bufs=nseq)
                Sb = Tiles(p, ph, "Sb", [128, 128], BF16, bufs=4)
                tmpS = Tiles(p, ph, "tmpS", [128, 128], F32, bufs=3)
                ssC = Tiles(p, ph, "ssC", [128, 4, 4], F32, bufs=2)
                junkC = Tiles(p, ph, "junkC", [128, 128], BF16, bufs=2)
                sg = Tiles(p, ph, "sg", [128, 512], F32, bufs=2)
                o1 = Tiles(p, ph, "o1", [128, 4, 128], F32, bufs=2)
                recb = Tiles(p, ph, "recb", [128, 512], BF16, bufs=2)
                psD = Tiles(p, ph, "psD", [128, 512], F32, bufs=2, psum=True)
                psOo = Tiles(p, ph, "psOo", [128, 4, 128], F32, bufs=2, psum=True)
                psTq = Tiles(p, ph, "psTq", [128, 4, 128], BF16, bufs=1, psum=True)
                psC = Tiles(p, ph, "psC", [128, 4, 4], F32, bufs=1, psum=True)
                psSc = Tiles(p, ph, "psSc", [128, 128], F32, bufs=1, psum=True)
                psKV = Tiles(p, ph, "psKV", [128, 128], F32, bufs=1, psum=True)
                for b in range(2):
                    at_t, at_k = AT.next()
                    p.op('pool', lambda e: e.memset(at_t[:], 0.0), writes=[at_k])
                for s in range(nseq):
                    S_t, S_k = Sst.next()
                    p.op('pool', lambda e: e.memset(S_t[:], 0.0), writes=[S_k])
                def tile_c(s, ti):
                    if True:
                        S_t, S_k = Sst.t[s], ("Sst", s)
                        r0 = s * 2048 + ti * 128
                        rec_t, rec_k = rec.next()
                        p.dma('sp', rec_t[:], projT[r0:r0 + 128, 512:2560], reads=['projT'], writes=[rec_k])
                        f_t, f_k = fT.next()
                        lf_t, lf_k = lf.next()
                        kk_t, kk_k = kkT.next()
                        qf_t, qf_k = qfT.next()
                        p.op('act', lambda e: e.activation(f_t[:], rec_t[:, 512:1024], AF.Sigmoid), reads=[rec_k], writes=[f_k])
                        yield
                        p.op('act', lambda e: e.activation(qf_t[:], rec_t[:, 0:512], AF.Silu), reads=[rec_k], writes=[qf_k])
                        sg_t, sg_k = sg.next()
                        p.op('act', lambda e: e.activation(sg_t[:], rec_t[:, 1536:2048], AF.Silu), reads=[rec_k], writes=[sg_k])
                        p.op('dve', lambda e: e.tensor_tensor(f_t[:], f_t[:], lb_t[:, 2, :], ALU.mult), reads=[f_k, lb_k], writes=[f_k])
                        p.op('dve', lambda e: e.tensor_tensor(f_t[:], f_t[:], lb_t[:, 1, :], ALU.add), reads=[f_k, lb_k], writes=[f_k])
                        yield
                        p.op('act', lambda e: e.activation(lf_t[:], f_t[:], AF.Ln), reads=[f_k], writes=[lf_k])
                        p.op('dve', lambda e: e.tensor_scalar(kk_t[:], f_t[:], -1.0, 1.0, ALU.mult, ALU.add), reads=[f_k], writes=[kk_k])
                        yield
                        pd_t, pd_k = psD.next()
                        p.op('pe', lambda e: e.matmul(pd_t[:], am_t[:], lf_t[:], start=True, stop=True), reads=[am_k, lf_k], writes=[pd_k])
                        pc_t, pc_k = psC.next()

                        def cmm(e):
                            for h in range(4):
                                ins = e.matmul(pc_t[:, h, :], lf_t[:, h * 128:(h + 1) * 128], bs_t[:], start=True, stop=True)
                            return ins
                        p.op('pe', cmm, reads=[lf_k, bs_k], writes=[pc_k])
                        pcs_t, pcs_k = pcs.next()
                        p.op('act', lambda e: e.copy(pcs_t[:], pc_t[:]), reads=[pc_k], writes=[pcs_k])
                        yield
                        eD_t, eD_k = eD.next()
                        p.op('act', lambda e: e.activation(eD_t[:, 0, :], pd_t[:], AF.Exp), reads=[pd_k], writes=[eD_k])
                        p.op('act', lambda e: e.activation(eD_t[:, 1, :], pd_t[:], AF.Exp, scale=-1.0), reads=[pd_k], writes=[eD_k])
                        ec_t, ec_k = ecs.next()
                        dl_t, dl_k = dl.next()
                        ed_t, ed_k = edl.next()
                        pc_v = pcs_t[:].rearrange("p h (c two) -> p h c two", two=2)
                        p.op('dve', lambda e: e.tensor_tensor(dl_t[:], pc_v[:, :, :, 1], pc_v[:, :, :, 0], ALU.subtract), reads=[pcs_k], writes=[dl_k])
                        p.op('act', lambda e: e.activation(ec_t[:], pcs_t[:], AF.Exp), reads=[pcs_k], writes=[ec_k])
                        p.op('act', lambda e: e.activation(ed_t[:], dl_t[:], AF.Exp), reads=[dl_k], writes=[ed_k])
                        yield
                        qt_t, qt_k = qtl.next()
                        kt_t, kt_k = ktl.next()
                        vb_t, vb_k = vbf.next()
                        p.op('dve', lambda e: e.tensor_tensor(qt_t[:], qf_t[:], eD_t[:, 0, :], ALU.mult), reads=[qf_k, eD_k], writes=[qt_k])
                        p.op('dve', lambda e: e.tensor_tensor(kt_t[:], kk_t[:], eD_t[:, 1, :], ALU.mult), reads=[kk_k, eD_k], writes=[kt_k])
                        p.op('act', lambda e: e.copy(vb_t[:], rec_t[:, 1024:1536]), reads=[rec_k], writes=[vb_k])
                        yield
                        pq_t, pq_k = psTq.next()

                        def trq(e):
                            for h in range(4):
                                ins = e.transpose(pq_t[:, h, :], qt_t[:, h * 128:(h + 1) * 128], idb_t[:])
                            return ins
                        p.op('pe', trq, reads=[qt_k, idb_k], writes=[pq_k])
                        qT_t, qT_k = qtT.next()
                        p.op('act', lambda e: e.copy(qT_t[:], pq_t[:]), reads=[pq_k], writes=[qT_k])
                        yield
                        pk_t, pk_k = psTq.next()

                        def trk(e):
                            for h in range(4):
                                ins = e.transpose(pk_t[:, h, :], kt_t[:, h * 128:(h + 1) * 128], idb_t[:])
                            return ins
                        p.op('pe', trk, reads=[kt_k, idb_k], writes=[pk_k])
                        kT_t, kT_k = ktT.next()
                        p.op('dve', lambda e: e.tensor_copy(kT_t[:], pk_t[:]), reads=[pk_k], writes=[kT_k])
                        yield
                        at_t, at_k = AT.next()
                        po_t, po_k = psOo.next()
                        for h in range(4):
                            yield
                            hs = slice(h * 128, (h + 1) * 128)
                            psc_t, psc_k = psSc.next()
                            p.op('pe', lambda e: e.matmul(psc_t[:], kT_t[:, h, :], qT_t[:, h, :], start=True, stop=True),
                                 reads=[kT_k, qT_k], writes=[psc_k])
                            p.op('dve', lambda e: e.copy_predicated(at_t[:, h, :], cm_t[:], psc_t[:]), reads=[psc_k, cm_k], writes=[at_k])
                            sbs = []
                            for c in range(2):
                                yield
                                sb_t, sb_k = Sb.next()
                                p.op('dve', lambda e: e.tensor_scalar(sb_t[:], S_t[:, h, :], ec_t[:, h, 2 * c:2 * c + 1], None, ALU.mult),
                                     reads=[S_k, ec_k], writes=[sb_k])
                                sbs.append((sb_t, sb_k))
                                pkv_t, pkv_k = psKV.next()
                                cs = slice(c * 64, (c + 1) * 64)
                                p.op('pe', lambda e: e.matmul(pkv_t[:], kt_t[cs, hs], vb_t[cs, hs], start=True, stop=True),
                                     reads=[kt_k, vb_k], writes=[pkv_k])
                                tm_t, tm_k = tmpS.next()
                                p.op('dve', lambda e: e.tensor_scalar(tm_t[:], pkv_t[:], ed_t[:, h, c:c + 1], None, ALU.mult),
                                     reads=[pkv_k, ed_k], writes=[tm_k])
                                p.op('dve', lambda e: e.scalar_tensor_tensor(S_t[:, h, :], S_t[:, h, :], ec_t[:, h, 2 * c + 1:2 * c + 2],
                                                                             tm_t[:], ALU.mult, ALU.add),
                                     reads=[S_k, ec_k, tm_k], writes=[S_k])

                            def omm(e):
                                e.matmul(po_t[:, h, :], at_t[:, h, :], vb_t[:, hs], start=True, stop=False)
                                e.matmul(po_t[0:64, h, :], qT_t[:, h, 0:64], sbs[0][0][:], start=False, stop=True)
                                ins = e.matmul(po_t[64:128, h, :], qT_t[:, h, 64:128], sbs[1][0][:], start=False, stop=True)
                                return ins
                            p.op('pe', omm, reads=[at_k, vb_k, qT_k, sbs[0][1], sbs[1][1]], writes=[po_k])
                        yield
                        ss_t, ss_k = ssC.next()
                        for h in range(4):
                            j_t, j_k = junkC.next()
                            p.op('act', lambda e: e.activation(j_t[:], po_t[:, h, :], AF.Square, accum_out=ss_t[:, 0, h:h + 1]),
                                 reads=[po_k], writes=[j_k, ss_k])
                        p.op('dve', lambda e: e.tensor_scalar(ss_t[:, 1, :], ss_t[:, 0, :], 1.0 / 128, 1e-6, ALU.mult, ALU.add), reads=[ss_k], writes=[ss_k])
                        p.op('act', lambda e: e.activation(ss_t[:, 2, :], ss_t[:, 1, :], AF.Sqrt), reads=[ss_k], writes=[ss_k])
                        p.op('dve', lambda e: e.reciprocal(ss_t[:, 3, :], ss_t[:, 2, :]), reads=[ss_k], writes=[ss_k])
                        yield
                        p.op('dve', lambda e: e.tensor_tensor(sg_t[:], sg_t[:], lb_t[:, 3, :], ALU.mult), reads=[sg_k, lb_k], writes=[sg_k])
                        yield
                        o1_t, o1_k = o1.next()
                        p.op('dve', lambda e: e.tensor_tensor(o1_t[:], po_t[:], ss_t[:, 3, :].unsqueeze(2).to_broadcast([128, 4, 128]), ALU.mult),
                             reads=[po_k, ss_k], writes=[o1_k])
                        rb_t, rb_k = recb.next()
                        p.op('dve', lambda e: e.tensor_tensor(rb_t[:], o1_t[:].rearrange("p a b -> p (a b)"), sg_t[:], ALU.mult),
                             reads=[o1_k, sg_k], writes=[rb_k])
                        p.dma('pool', mix[r0:r0 + 128, 512:1024], rb_t[:], reads=[rb_k], writes=['mix'], sem=rb_k, lane='st', nowaw=True)

                def run_lockstep(gens):
                    live = list(gens)
                    while live:
                        nxt = []
                        for g_ in live:
                            try:
                                next(g_)
                                nxt.append(g_)
                            except StopIteration:
                                pass
                        live = nxt
                for ti in range(16):
                    for s0 in range(0, nseq, 2):
                        run_lockstep([tile_c(s_, ti) for s_ in range(s0, min(s0 + 2, nseq))])

        p.barrier()
        if "D" in stages:
            build_phase_d(nc, p, st, locals())
        p.barrier()
        p.fence('sp', reads=['out', 'mix', 'projT', 'qkT'])
        print("instructions:", p.ninst, "semaphores:", len(p.sems))
    return nc


def build_phase_d(nc, p, st, env):
    x = env["x"]; out = env["out"]; mix = env["mix"]; uvtab = env["uvtab"]; cst = env["cst"]
    w_out = env["w_out"]; norm2_g = env["norm2_g"]; normf_g = env["normf_g"]
    peer_wq = env["peer_wq"]; peer_sk = env["peer_sk"]
    idb_t, idb_k = env["idb_t"], env["idb_k"]
    NT = env["NT"]
    SG = 4
    NBUF = 5
    with ExitStack() as ph:
        wo = Tiles(p, ph, "wo", [128, 8, 1024], BF16)
        wq = Tiles(p, ph, "wq", [128, 8, 2048], BF16)
        skT = Tiles(p, ph, "skT", [128, 16, 128], BF16)
        g2b = Tiles(p, ph, "g2b", [128, 2, 1024], F32)
        io16 = Tiles(p, ph, "io16", [128, 16], F32)
        wo_t, wo_k = wo.next()
        wq_t, wq_k = wq.next()
        skT_t, skT_k = skT.next()
        g2_t, g2_k = g2b.next()
        io_t, io_k = io16.next()
        for k in range(8):
            p.dma('pool', wo_t[:, k, :], w_out[k * 128:(k + 1) * 128, :], writes=[wo_k], lane=k % 4)
            p.dma('pool', wq_t[:, k, :], peer_wq[k * 128:(k + 1) * 128, :], writes=[wq_k], lane=k % 4)
        p.dma('sp', g2_t[:, 0, :], norm2_g.partition_broadcast(128), writes=[g2_k])
        p.dma('sp', g2_t[:, 1, :], normf_g.partition_broadcast(128), writes=[g2_k], lane=1)
        p.dma('sp', io_t[:], cst["c_iota16"], writes=[io_k])
        psT = Tiles(p, ph, "psTd", [128, 8, 128], BF16, bufs=1, psum=True)
        psY = Tiles(p, ph, "psY", [128, 512], F32, bufs=1, psum=True)
        psQ = Tiles(p, ph, "psQ", [128, 512], F32, bufs=2, psum=True)
        psP = Tiles(p, ph, "psP", [128, 1024], F32, bufs=2, psum=True)
        scr = Tiles(p, ph, "scr", [128, 2048], F32, bufs=1)
        scr_t, scr_k = scr.next()
        skn_v = scr_t[:, 0:1024].bitcast(BF16).rearrange("p (g n) -> p g n", n=128)
        p.dma('pool', skn_v, peer_sk.rearrange("g n d -> n g d"), writes=[scr_k])
        for half in range(2):
            pt_t, pt_k = psT.next()

            def trs(e):
                for j in range(8):
                    ins = e.transpose(pt_t[:, j, :], skn_v[:, half * 8 + j, :], idb_t[:])
                return ins
            p.op('pe', trs, reads=[scr_k, idb_k], writes=[pt_k])
            p.op('act', lambda e: e.copy(skT_t[:, half * 8:(half + 1) * 8, :], pt_t[:]), reads=[pt_k], writes=[skT_k])

        mx = Tiles(p, ph, "mx", [128, 1024], BF16, bufs=2)
        mxT = Tiles(p, ph, "mxT", [128, 8, 128], BF16, bufs=1)
        xt = Tiles(p, ph, "xtd", [128, 1024], F32, bufs=3)
        junk = Tiles(p, ph, "junkD", [128, 1024], BF16, bufs=2)
        junk2 = Tiles(p, ph, "junkD2", [128, 1024], BF16, bufs=1)
        ssD = Tiles(p, ph, "ssD", [128, 8], F32, bufs=3)
        mhalf = Tiles(p, ph, "mhalf", [128, 1], F32)
        mh_t, mh_k = mhalf.next()
        p.op('pool', lambda e: e.memset(mh_t[:], -0.5), writes=[mh_k])
        hnb = Tiles(p, ph, "hnb", [128, 1024], BF16, bufs=2)
        hnT = Tiles(p, ph, "hnT", [128, 8, 128], BF16, bufs=1)
        qpT = Tiles(p, ph, "qpT", [128, 16, 128], BF16, bufs=1)
        ss2 = Tiles(p, ph, "ss2", [128, 128], F32, bufs=4)
        v12 = Tiles(p, ph, "v12", [128, 16, 16], F32, bufs=1)
        i12 = Tiles(p, ph, "i12", [128, 16, 16], U32, bufs=1)
        i12f = Tiles(p, ph, "i12f", [128, 16, 16], F32, bufs=1)
        cand2 = Tiles(p, ph, "cand2", [128, 256], F32, bufs=4)
        sc = Tiles(p, ph, "sc", [128, 8, 16], F32, bufs=1)
        pidx = Tiles(p, ph, "pidx", [128, 8, 16], U32, bufs=1)
        pij = Tiles(p, ph, "pij", [128, 2, 128], U32, bufs=1)
        pijf = Tiles(p, ph, "pijf", [128, 2, 128], F32, bufs=1)
        abf = Tiles(p, ph, "abf", [128, 2, 128], F32, bufs=1)
        eidf = Tiles(p, ph, "eidf", [128, 128], F32, bufs=1)
        eid = Tiles(p, ph, "eid", [128, 128], U32, bufs=2)
        gts = Tiles(p, ph, "gts", [128, 8, 16], F32, bufs=2)
        zz = Tiles(p, ph, "zz", [128, 2, 8], F32, bufs=1)
        uvb = Tiles(p, ph, "uvb", [128, SG, 2048], BF16, bufs=NBUF)
        apre = Tiles(p, ph, "apre", [128, SG], F32, bufs=NBUF)
        wgt = Tiles(p, ph, "wgt", [128, SG], F32, bufs=NBUF)
        dg = Tiles(p, ph, "dg", [128, 128], BF16, bufs=6)
        ot = Tiles(p, ph, "ot", [128, 1024], F32, bufs=1)
        OFFLOAD = False
        prod = Tiles(p, ph, "prod", [128, 1024], F32, bufs=1) if OFFLOAD else None

        def stage1(ti, ctx):
            r0 = ti * 128
            mx_t, mx_k = mx.next()
            p.dma('sp', mx_t[:], mix[r0:r0 + 128, :], reads=['mix'], writes=[mx_k])
            xt_t, xt_k = xt.next()
            p.dma('sp', xt_t[:], x[r0:r0 + 128, :], writes=[xt_k])
            pt_t, pt_k = psT.next()

            def tr1(e):
                for k in range(8):
                    ins = e.transpose(pt_t[:, k, :], mx_t[:, k * 128:(k + 1) * 128], idb_t[:])
                return ins
            p.op('pe', tr1, reads=[mx_k, idb_k], writes=[pt_k])
            mxT_t, mxT_k = mxT.next()
            p.op('act', lambda e: e.copy(mxT_t[:], pt_t[:]), reads=[pt_k], writes=[mxT_k])
            yield
            h_t, h_k = xt_t, xt_k
            for hf in range(2):
                py_t, py_k = psY.next()

                def mmy(e):
                    for k in range(8):
                        ins = e.matmul(py_t[:], mxT_t[:, k, :], wo_t[:, k, hf * 512:(hf + 1) * 512], start=(k == 0), stop=(k == 7))
                    return ins
                p.op('pe', mmy, reads=[mxT_k, wo_k], writes=[py_k])
                yield
                p.op('dve', lambda e: e.tensor_tensor(h_t[:, hf * 512:(hf + 1) * 512], py_t[:], xt_t[:, hf * 512:(hf + 1) * 512], ALU.add),
                     reads=[py_k, xt_k], writes=[h_k])
            j_t, j_k = junk2.next()
            ss_t, ss_k = ssD.next()
            yield
            p.op('act', lambda e: e.activation(j_t[:], h_t[:], AF.Square, accum_out=ss_t[:, 0:1]), reads=[h_k], writes=[j_k, ss_k])
            yield
            p.op('dve', lambda e: e.tensor_scalar(ss_t[:, 1:2], ss_t[:, 0:1], 1.0 / 1024, 1e-6, ALU.mult, ALU.add), reads=[ss_k], writes=[ss_k])
            p.op('act', lambda e: e.activation(ss_t[:, 2:3], ss_t[:, 1:2], AF.Sqrt), reads=[ss_k], writes=[ss_k])
            yield
            p.op('dve', lambda e: e.reciprocal(ss_t[:, 3:4], ss_t[:, 2:3]), reads=[ss_k], writes=[ss_k])
            hn_t, hn_k = hnb.next()
            p.op('dve', lambda e: e.scalar_tensor_tensor(hn_t[:], h_t[:], ss_t[:, 3:4], g2_t[:, 0, :], ALU.mult, ALU.mult),
                 reads=[h_k, ss_k, g2_k], writes=[hn_k])
            yield
            pt_t, pt_k = psT.next()

            def tr2(e):
                for k in range(8):
                    ins = e.transpose(pt_t[:, k, :], hn_t[:, k * 128:(k + 1) * 128], idb_t[:])
                return ins
            p.op('pe', tr2, reads=[hn_k, idb_k], writes=[pt_k])
            hnT_t, hnT_k = hnT.next()
            p.op('act', lambda e: e.copy(hnT_t[:], pt_t[:]), reads=[pt_k], writes=[hnT_k])
            yield
            qp_t, qp_k = qpT.next()
            for gq in range(4):
                pq_t, pq_k = psQ.next()

                def mmq(e):
                    for gg in range(4):
                        g = gq * 4 + gg
                        for k in range(8):
                            ins = e.matmul(pq_t[:, gg * 128:(gg + 1) * 128], wq_t[:, k, g * 128:(g + 1) * 128], hnT_t[:, k, :],
                                           start=(k == 0), stop=(k == 7))
                    return ins
                p.op('pe', mmq, reads=[wq_k, hnT_k], writes=[pq_k])
                p.op('act', lambda e: e.copy(qp_t[:, gq * 4:(gq + 1) * 4, :], pq_t[:].rearrange("p (a b) -> p a b", b=128)),
                     reads=[pq_k], writes=[qp_k])
            yield
            s_v = scr_t[:].rearrange("p (g n) -> p g n", n=128)
            for gq in range(4):
                pq_t, pq_k = psQ.next()

                def mms(e):
                    for gg in range(4):
                        g = gq * 4 + gg
                        ins = e.matmul(pq_t[:, gg * 128:(gg + 1) * 128], qp_t[:, g, :], skT_t[:, g, :], start=True, stop=True)
                    return ins
                p.op('pe', mms, reads=[qp_k, skT_k], writes=[pq_k])
                p.op('act', lambda e: e.copy(s_v[:, gq * 4:(gq + 1) * 4, :], pq_t[:].rearrange("p (a b) -> p a b", b=128)),
                     reads=[pq_k], writes=[scr_k])
            yield
            yield
            v_t, v_k = v12.next()
            i_t, i_k = i12.next()
            for g0 in range(0, 16, 4):
                gs = [g0, g0 + 1, g0 + 2, g0 + 3]
                s2s = [ss2.next() for _ in gs]
                for g in gs:
                    p.op('dve', lambda e: e.max(v_t[:, g, 0:8], s_v[:, g, :]), reads=[scr_k], writes=[(v_k, g)])
                for g in gs:
                    p.op('dve', lambda e: e.max_index(i_t[:, g, 0:8], v_t[:, g, 0:8], s_v[:, g, :]), reads=[scr_k, (v_k, g)], writes=[(i_k, g)])
                for g, (s2_t, s2_k) in zip(gs, s2s):
                    p.op('dve', lambda e: e.match_replace(s2_t[:], v_t[:, g, 0:8], s_v[:, g, :], NEG), reads=[scr_k, (v_k, g)], writes=[s2_k])
                for g, (s2_t, s2_k) in zip(gs, s2s):
                    p.op('dve', lambda e: e.max(v_t[:, g, 8:16], s2_t[:]), reads=[s2_k], writes=[(v_k, g, 1)])
                for g, (s2_t, s2_k) in zip(gs, s2s):
                    p.op('dve', lambda e: e.max_index(i_t[:, g, 8:16], v_t[:, g, 8:16], s2_t[:]), reads=[s2_k, (v_k, g, 1)], writes=[(i_k, g, 1)])
                yield
            vall = [(v_k, g) for g in range(16)] + [(v_k, g, 1) for g in range(16)]
            iall = [(i_k, g) for g in range(16)] + [(i_k, g, 1) for g in range(16)]
            if_t, if_k = i12f.next()
            p.op('dve', lambda e: e.tensor_copy(if_t[:], i_t[:]), reads=iall, writes=[if_k])
            ca_v = scr_t[:].rearrange("p (h c) -> p h c", c=256)
            v_v = v_t[:].rearrange("p (h c) k -> p h c k", c=2)
            p.op('dve', lambda e: e.tensor_tensor(ca_v.rearrange("p h (i j) -> p h i j", j=16),
                                                  v_v[:, :, 0, :].unsqueeze(3).to_broadcast([128, 8, 16, 16]),
                                                  v_v[:, :, 1, :].unsqueeze(2).to_broadcast([128, 8, 16, 16]), ALU.add),
                 reads=vall, writes=[scr_k])
            yield
            sc_t, sc_k = sc.next()
            pi_t, pi_k = pidx.next()
            for h0 in range(0, 8, 4):
                hs = [h0, h0 + 1, h0 + 2, h0 + 3]
                c2s = [cand2.next() for _ in hs]
                for h in hs:
                    p.op('dve', lambda e: e.max(sc_t[:, h, 0:8], ca_v[:, h, :]), reads=[scr_k], writes=[(sc_k, h)])
                for h in hs:
                    p.op('dve', lambda e: e.max_index(pi_t[:, h, 0:8], sc_t[:, h, 0:8], ca_v[:, h, :]), reads=[scr_k, (sc_k, h)], writes=[(pi_k, h)])
                for h, (c2_t, c2_k) in zip(hs, c2s):
                    p.op('dve', lambda e: e.match_replace(c2_t[:], sc_t[:, h, 0:8], ca_v[:, h, :], NEG), reads=[scr_k, (sc_k, h)], writes=[c2_k])
                for h, (c2_t, c2_k) in zip(hs, c2s):
                    p.op('dve', lambda e: e.max(sc_t[:, h, 8:16], c2_t[:]), reads=[c2_k], writes=[(sc_k, h, 1)])
                for h, (c2_t, c2_k) in zip(hs, c2s):
                    p.op('dve', lambda e: e.max_index(pi_t[:, h, 8:16], sc_t[:, h, 8:16], c2_t[:]), reads=[c2_k, (sc_k, h, 1)], writes=[(pi_k, h, 1)])
                yield
            scall = [(sc_k, h) for h in range(8)] + [(sc_k, h, 1) for h in range(8)]
            piall = [(pi_k, h) for h in range(8)] + [(pi_k, h, 1) for h in range(8)]
            g_t, g_k = gts.next()
            z_t, z_k = zz.next()
            p.op('dve', lambda e: e.tensor_tensor(g_t[:], sc_t[:], sc_t[:, :, 0:1].to_broadcast([128, 8, 16]), ALU.subtract),
                 reads=scall, writes=[g_k])
            yield
            for h in range(8):
                p.op('act', lambda e: e.activation(g_t[:, h, :], g_t[:, h, :], AF.Exp, accum_out=z_t[:, 0, h:h + 1]),
                     reads=[g_k], writes=[g_k, z_k])
            yield
            GATES_TAIL = True
            pij_t, pij_k = pij.next()
            pf_t, pf_k = pijf.next()
            pi_flat = pi_t[:].rearrange("p h k -> p (h k)")
            p.op('dve', lambda e: e.tensor_single_scalar(pij_t[:, 0, :], pi_flat, 4, ALU.logical_shift_right), reads=piall, writes=[pij_k])
            p.op('dve', lambda e: e.tensor_single_scalar(pij_t[:, 1, :], pi_flat, 15, ALU.bitwise_and), reads=piall, writes=[pij_k])
            p.op('dve', lambda e: e.tensor_copy(pf_t[:], pij_t[:]), reads=[pij_k], writes=[pf_k])
            ab_t, ab_k = abf.next()
            if_v = if_t[:].rearrange("p (h c) k -> p h c k", c=2)
            oh_v3 = scr_t[:].rearrange("p (s i) -> p s i", i=16)
            for c in range(2):
                p.op('dve', lambda e: e.tensor_tensor(oh_v3, io_t[:].unsqueeze(1).to_broadcast([128, 128, 16]),
                                                      pf_t[:, c, :].unsqueeze(2).to_broadcast([128, 128, 16]), ALU.is_equal),
                     reads=[io_k, pf_k], writes=[scr_k])
                oh_v = oh_v3.rearrange("p (h k) i -> p h k i", h=8)
                p.op('dve', lambda e: e.tensor_tensor(oh_v, oh_v, if_v[:, :, c, :].unsqueeze(2).to_broadcast([128, 8, 16, 16]), ALU.mult),
                     reads=[scr_k, if_k], writes=[scr_k])
                yield
                p.op('dve', lambda e: e.tensor_tensor(oh_v3[:, :, 0:8], oh_v3[:, :, 0:8], oh_v3[:, :, 8:16], ALU.add), reads=[scr_k], writes=[scr_k])
                p.op('dve', lambda e: e.tensor_tensor(oh_v3[:, :, 0:4], oh_v3[:, :, 0:4], oh_v3[:, :, 4:8], ALU.add), reads=[scr_k], writes=[scr_k])
                p.op('dve', lambda e: e.tensor_tensor(oh_v3[:, :, 0:2], oh_v3[:, :, 0:2], oh_v3[:, :, 2:4], ALU.add), reads=[scr_k], writes=[scr_k])
                p.op('dve', lambda e: e.tensor_tensor(ab_t[:, c, :], oh_v3[:, :, 0], oh_v3[:, :, 1], ALU.add), reads=[scr_k], writes=[ab_k])
                yield
            p.op('dve', lambda e: e.reciprocal(z_t[:, 1, :], z_t[:, 0, :]), reads=[z_k], writes=[z_k])
            p.op('dve', lambda e: e.tensor_tensor(g_t[:], g_t[:], z_t[:, 1, :].unsqueeze(2).to_broadcast([128, 8, 16]), ALU.mult),
                 reads=[g_k, z_k], writes=[g_k])
            ef_t, ef_k = eidf.next()
            p.op('dve', lambda e: e.scalar_tensor_tensor(ef_t[:], ab_t[:, 0, :], 128.0, ab_t[:, 1, :], ALU.mult, ALU.add),
                 reads=[ab_k], writes=[ef_k])
            ei_t, ei_k = eid.next()
            p.op('dve', lambda e: e.tensor_copy(ei_t[:], ef_t[:]), reads=[ef_k], writes=[ei_k])
            ctx.update(h=(h_t, h_k), hn=(hn_t, hn_k), ss=(ss_t, ss_k), ei=(ei_t, ei_k), g=(g_t, g_k), r0=r0)

        def advance(gen, n):
            if gen is None:
                return
            for _ in range(n):
                try:
                    next(gen)
                except StopIteration:
                    return

        def stage2(ctx, gen, prev_epi):
            h_t, h_k = ctx["h"]; hn_t, hn_k = ctx["hn"]; ss_t, ss_k = ctx["ss"]
            ei_t, ei_k = ctx["ei"]; g_t, g_k = ctx["g"]; r0 = ctx["r0"]
            pp_t, pp_k = psP.next()
            g_flat = g_t[:].rearrange("p h k -> p (h k)")
            NGRP = 128 // SG
            issued = {}

            def issue(grp):
                uv_t, uv_k = uvb.next()
                for sl in range(SG):
                    slot = grp * SG + sl
                    p.dma('pool', None, None, reads=[ei_k, 'uvtab'], writes=[uv_k], lane=sl,
                          fn=lambda e: e.indirect_dma_start(out=uv_t[:, sl, :], out_offset=None, in_=uvtab,
                                                            in_offset=bass.IndirectOffsetOnAxis(ap=ei_t[:, slot:slot + 1], axis=0)))
                issued[grp] = (uv_t, uv_k)
            PD = NBUF - 2
            for grp in range(min(PD, NGRP)):
                issue(grp)

            def post_b(grp, uv_t, uv_k, w_t, w_k):
                p.op('dve', lambda e: e.tensor_tensor(w_t[:], w_t[:], g_flat[:, grp * SG:(grp + 1) * SG], ALU.mult), reads=[w_k, g_k], writes=[w_k])
                for sl in range(SG):
                    slot = grp * SG + sl
                    dg_t, dg_k = dg.next()
                    p.op('act', lambda e: e.activation(dg_t[:], idb_t[:], AF.Copy, scale=w_t[:, sl:sl + 1]), reads=[idb_k, w_k], writes=[dg_k])

                    def mmv(e):
                        e.matmul(pp_t[:, 0:512], dg_t[:], uv_t[:, sl, 1024:1536], start=(slot == 0), stop=(slot == 127))
                        return e.matmul(pp_t[:, 512:1024], dg_t[:], uv_t[:, sl, 1536:2048], start=(slot == 0), stop=(slot == 127))
                    p.op('pe', mmv, reads=[dg_k, uv_k], writes=[pp_k])
            pend = None
            for grp in range(NGRP):
                if grp + PD < NGRP:
                    issue(grp + PD)
                uv_t, uv_k = issued.pop(grp)
                ap_t, ap_k = apre.next()
                w_t, w_k = wgt.next()
                for sl in range(SG):
                    if sl == SG - 1 and OFFLOAD:
                        pr_t, pr_k = prod.next()
                        p.op('pool', lambda e: e.tensor_tensor(pr_t[:], uv_t[:, sl, 0:1024], hn_t[:], ALU.mult), reads=[uv_k, hn_k], writes=[pr_k])
                        jj_t, jj_k = junk2.next()
                        p.op('act', lambda e: e.activation(jj_t[:], pr_t[:], AF.Copy, accum_out=ap_t[:, sl:sl + 1]),
                             reads=[pr_k], writes=[jj_k, (ap_k, 1)])
                        continue
                    jj_t, jj_k = junk.next()
                    p.op('dve', lambda e: e.scalar_tensor_tensor(jj_t[:], uv_t[:, sl, 0:1024], 1.0, hn_t[:], ALU.mult, ALU.mult,
                                                                 accum_out=ap_t[:, sl:sl + 1]),
                         reads=[uv_k, hn_k], writes=[(ap_k, 'c', sl)])
                p.op('act', lambda e: e.activation(w_t[:], ap_t[:], AF.Gelu), reads=[(ap_k, 'c', sl_) for sl_ in range(SG)], writes=[w_k])
                if pend is not None:
                    post_b(*pend)
                pend = (grp, uv_t, uv_k, w_t, w_k)
                if grp == 3 and prev_epi is not None:
                    prev_epi()
                advance(gen, 1)
            post_b(*pend)

            def epilogue():
                p.op('dve', lambda e: e.tensor_tensor(h_t[:], pp_t[:], h_t[:], ALU.add), reads=[pp_k, h_k], writes=[h_k])
                j_t, j_k = junk2.next()
                p.op('act', lambda e: e.activation(j_t[:], h_t[:], AF.Square, accum_out=ss_t[:, 4:5]), reads=[h_k], writes=[j_k, ss_k])
                p.op('dve', lambda e: e.tensor_scalar(ss_t[:, 5:6], ss_t[:, 4:5], 1.0 / 1024, 1e-6, ALU.mult, ALU.add), reads=[ss_k], writes=[ss_k])
                p.op('act', lambda e: e.activation(ss_t[:, 6:7], ss_t[:, 5:6], AF.Sqrt), reads=[ss_k], writes=[ss_k])
                p.op('dve', lambda e: e.reciprocal(ss_t[:, 7:8], ss_t[:, 6:7]), reads=[ss_k], writes=[ss_k])
                o_t, o_k = ot.next()
                p.op('dve', lambda e: e.scalar_tensor_tensor(o_t[:], h_t[:], ss_t[:, 7:8], g2_t[:, 1, :], ALU.mult, ALU.mult),
                     reads=[h_k, ss_k, g2_k], writes=[o_k])
                p.dma('sp', out[r0:r0 + 128, :], o_t[:], reads=[o_k], writes=['out'], sem=o_k, lane='st', nowaw=True)
            return epilogue

        ctxs = [dict() for _ in range(NT)]
        g0 = stage1(0, ctxs[0])
        advance(g0, 10 ** 6)
        epi = None
        for ti in range(NT):
            gen = stage1(ti + 1, ctxs[ti + 1]) if ti + 1 < NT else None
            epi = stage2(ctxs[ti], gen, epi)
            advance(gen, 10 ** 6)
        epi()


_CONSTS = None


def kernel(x, norm1_g, w_in, rec_lb_logits, rec_norm_g, w_out, norm2_g, peer_wq, peer_subkeys, peer_u, peer_v, normf_g):
    global _CONSTS
    if _CONSTS is None:
        _CONSTS = make_consts()
    n = 8
    x = np.asarray(x, np.float32)
    B = x.shape[0]
    per = B // n
    shared = {
        "norm1_g": np.ascontiguousarray(np.asarray(norm1_g, np.float32).reshape(1, 1024)),
        "w_in": np.ascontiguousarray(np.asarray(w_in, np.float32).reshape(1024, 3584)),
        "rec_lb_logits": np.ascontiguousarray(np.asarray(rec_lb_logits, np.float32).reshape(2, 512)),
        "rec_norm_g": np.ascontiguousarray(np.asarray(rec_norm_g, np.float32).reshape(1, 512)),
        "w_out": np.ascontiguousarray(np.asarray(w_out, np.float32).reshape(1024, 1024)),
        "norm2_g": np.ascontiguousarray(np.asarray(norm2_g, np.float32).reshape(1, 1024)),
        "peer_wq": np.ascontiguousarray(np.asarray(peer_wq, np.float32).reshape(1024, 2048)),
        "peer_subkeys": np.ascontiguousarray(np.asarray(peer_subkeys, np.float32).reshape(16, 128, 128)),
        "peer_u": np.ascontiguousarray(np.asarray(peer_u, np.float32).reshape(16384, 1024)),
        "peer_v": np.ascontiguousarray(np.asarray(peer_v, np.float32).reshape(16384, 1024)),
        "normf_g": np.ascontiguousarray(np.asarray(normf_g, np.float32).reshape(1, 1024)),
    }
    shared.update(_CONSTS)
    nc = build(nseq=per)
    in_maps = []
    for i in range(n):
        m = dict(shared)
        m["x"] = np.ascontiguousarray(x[i * per:(i + 1) * per].reshape(per * 2048, 1024))
        in_maps.append(m)
    res = run_bass_kernel_spmd(nc, in_maps, core_ids=list(range(n)))
    outs = [np.asarray(r["out"], np.float32).reshape(per, 2048, 1024) for r in res.results]
    return np.concatenate(outs, axis=0)
```

```python
import numpy as np
from contextlib import ExitStack
import concourse.bass as bass
import concourse.mybir as mybir
from concourse.bass_utils import run_bass_kernel_spmd

F32 = mybir.dt.float32
BF16 = mybir.dt.bfloat16
U32 = mybir.dt.uint32
I32 = mybir.dt.int32
AF = mybir.ActivationFunctionType
ALU = mybir.AluOpType
AX = mybir.AxisListType

SEM_LIMIT = 30000
NEG = -1.0e30
MASKV = 32768.0


class Prog:
    ENG = ('pe', 'dve', 'act', 'pool', 'sp')

    def __init__(self, nc, stack):
        self.nc = nc
        self.stack = stack
        self.eng = {'pe': nc.tensor, 'dve': nc.vector, 'act': nc.scalar, 'pool': nc.gpsimd, 'sp': nc.sync}
        self.semcnt = {}
        self.sems = {}
        self.seen = {e: {} for e in self.ENG}
        self.res = {}
        self.ninst = 0

    def _sem(self, key):
        if key not in self.sems:
            self.sems[key] = self.stack.enter_context(self.nc.semaphore(f"s{len(self.sems)}"))
        return self.sems[key]

    def _bump(self, name, inc):
        ep, val = self.semcnt.get(name, (0, 0))
        if val + inc > SEM_LIMIT:
            ep += 1
            val = 0
        val += inc
        self.semcnt[name] = (ep, val)
        return ((name, ep), val)

    @staticmethod
    def _sibling(semname, w):
        return isinstance(semname, tuple) and len(semname) >= 2 and semname[0] == 'd' and semname[1] == w

    def _deps(self, reads, writes, myname, group, nowaw=False):
        d = {}

        def add(k, v):
            if d.get(k, 0) < v:
                d[k] = v
        for r in reads:
            st = self.res.get(r)
            if st:
                for (k, v) in st[0].values():
                    add(k, v)
        self._saved = {}
        for w in writes:
            st = self.res.get(w)
            sv = {}
            if st:
                sib = group and len(st[0]) > 0 and all(self._sibling(n, w) for n in st[0]) and self._sibling(myname, w)
                if sib or nowaw:
                    sv = dict(st[2])
                else:
                    for (k, v) in st[0].values():
                        sv[k] = max(sv.get(k, 0), v)
                for k, v in st[1].items():
                    sv[k] = max(sv.get(k, 0), v)
                for k, v in sv.items():
                    add(k, v)
            self._saved[w] = sv
        return d

    def _waits(self, eng, d):
        e = self.eng[eng]
        for k, v in d.items():
            if eng == 'pe' and k[0] == 'pe':
                continue
            if self.seen[eng].get(k, 0) >= v:
                continue
            self.seen[eng][k] = v
            e.wait_ge(self._sem(k), v)

    def _update(self, reads, writes, myname, mykey, myval, group, nowaw=False):
        for r in reads:
            st = self.res.setdefault(r, [{}, {}, {}])
            st[1][mykey] = myval
        for w in writes:
            st = self.res.get(w)
            if st and (nowaw or (group and len(st[0]) > 0 and all(self._sibling(n, w) for n in st[0]) and self._sibling(myname, w))):
                wr = dict(st[0])
            else:
                wr = {}
            wr[myname] = (mykey, myval)
            self.res[w] = [wr, {}, self._saved.get(w, {})]

    def op(self, eng, fn, reads=(), writes=()):
        mykey, myval = self._bump(eng, 1)
        d = self._deps(reads, writes, eng, False)
        self._waits(eng, d)
        ins = fn(self.eng[eng])
        ins.then_inc(self._sem(mykey), 1)
        self._update(reads, writes, eng, mykey, myval, False)
        self.ninst += 1
        return ins

    def dma(self, qeng, out, in_, reads=(), writes=(), sem=None, lane=0, group=True, fn=None, nowaw=False, **kw):
        semname = ('d', sem if sem is not None else writes[0], lane)
        prev = self.semcnt.get(semname)
        mykey, myval = self._bump(semname, 16)
        d = self._deps(reads, writes, semname, group, nowaw)
        if prev is not None and prev[1] > 0:
            pk = (semname, prev[0])
            d[pk] = max(d.get(pk, 0), prev[1])
        self._waits(qeng, d)
        e = self.eng[qeng]
        if fn is not None:
            ins = fn(e)
        else:
            ins = e.dma_start(out=out, in_=in_, **kw)
        ins.then_inc(self._sem(mykey), 16)
        self._update(reads, writes, semname, mykey, myval, group, nowaw)
        self.ninst += 1
        return ins

    def barrier(self):
        snap = {(name, ep): val for name, (ep, val) in self.semcnt.items()}
        for eng in self.ENG:
            self._waits(eng, snap)

    def fence(self, eng, reads=(), writes=()):
        d = self._deps(reads, writes, None, False)
        self._waits(eng, d)


class Tiles:
    def __init__(self, p, stack, name, shape, dtype, bufs=1, psum=False):
        alloc = p.nc.psum_tensor if psum else p.nc.sbuf_tensor
        self.t = [stack.enter_context(alloc(f"{name}_{i}", shape, dtype)) for i in range(bufs)]
        self.name = name
        self.bufs = bufs
        self.i = -1

    def next(self):
        self.i += 1
        return self.cur()

    def cur(self):
        b = self.i % self.bufs
        return self.t[b], (self.name, b)


class Views:
    def __init__(self, items):
        self.items = items
        self.i = -1

    def next(self):
        self.i += 1
        return self.items[self.i % len(self.items)]


def make_consts():
    c = {}
    c["c_ident"] = np.eye(128, dtype=np.float32)
    kq = np.arange(128)
    c["c_tri"] = np.where(kq[:, None] > kq[None, :], -MASKV, 0.0).astype(np.float32)
    pos = np.arange(2048)
    blk = pos // 256
    r = pos % 256
    kaug = np.zeros((8, 12, 2048), np.float32)
    qaug = np.zeros((8, 4, 2048), np.float32)
    for h in range(8):
        sl = 2.0 ** (-(h + 1))
        for n in range(8):
            kaug[h, n] = MASKV * (blk == n)
        kaug[h, 8] = sl * r
        kaug[h, 9] = 1.0
        kaug[h, 10] = sl * 256.0 * blk
        kaug[h, 11] = 1.0
        qaug[h, 0] = 1.0
        qaug[h, 1] = -sl * r
        qaug[h, 2] = 1.0
        qaug[h, 3] = -sl * 256.0 * blk
    c["c_kaug"] = kaug
    c["c_qaug"] = qaug
    el = np.zeros((3, 16, 8), np.float32)
    for qt in range(16):
        j = qt // 2
        for n in range(8):
            el[0, qt, n] = 0.0 if n < j else NEG
            el[1, qt, n] = 1.0 if n < j else 0.0
            el[2, qt, n] = 1.0 if n == j else 0.0
    c["c_elig"] = np.broadcast_to(el.reshape(1, 3 * 16 * 8), (128, 384)).copy()
    s = np.arange(128)
    ch = s // 64
    mid = ch * 64 + 31
    same = ch[:, None] == ch[None, :]
    amat = (same & (s[:, None] <= s[None, :])).astype(np.float32) - (same & (s[:, None] <= mid[None, :])).astype(np.float32)
    c["c_amat"] = amat.astype(np.float32)
    bsel = np.zeros((128, 4), np.float32)
    bsel[:, 0] = (s <= 31)
    bsel[:, 1] = (s <= 63)
    bsel[:, 2] = (s >= 64) & (s <= 95)
    bsel[:, 3] = (s >= 64)
    c["c_bsel"] = bsel
    c["c_cmask"] = (same & (s[:, None] <= s[None, :])).astype(np.int32)
    c["c_iota16"] = np.broadcast_to(np.arange(16, dtype=np.float32)[None, :], (128, 16)).copy()
    return c


CONST_SHAPES = {"c_ident": ([128, 128], F32), "c_tri": ([128, 128], F32), "c_kaug": ([8, 12, 2048], F32),
                "c_qaug": ([8, 4, 2048], F32), "c_elig": ([128, 384], F32), "c_amat": ([128, 128], F32),
                "c_bsel": ([128, 4], F32), "c_cmask": ([128, 128], I32), "c_iota16": ([128, 16], F32)}


def build(nseq=4, dbg=False, stages="0ABCD"):
    T = nseq * 2048
    NT = T // 128
    NG = T // 512
    nc = bass.Bass("TRN2", target_bir_lowering=False)

    def din(name, shape, dt=F32):
        return nc.dram_tensor(name, shape, dt, kind="ExternalInput").ap()

    x = din("x", [T, 1024])
    norm1_g = din("norm1_g", [1, 1024])
    w_in = din("w_in", [1024, 3584])
    rec_lb = din("rec_lb_logits", [2, 512])
    rec_norm_g = din("rec_norm_g", [1, 512])
    w_out = din("w_out", [1024, 1024])
    norm2_g = din("norm2_g", [1, 1024])
    peer_wq = din("peer_wq", [1024, 2048])
    peer_sk = din("peer_subkeys", [16, 128, 128])
    peer_u = din("peer_u", [16384, 1024])
    peer_v = din("peer_v", [16384, 1024])
    normf_g = din("normf_g", [1, 1024])
    cst = {k: din(k, sh, dt) for k, (sh, dt) in CONST_SHAPES.items()}
    out = nc.dram_tensor("out", [T, 1024], F32, kind="ExternalOutput").ap()
    skind = "ExternalOutput" if dbg else "Internal"
    projT = nc.dram_tensor("projT", [T, 2560], F32, kind=skind).ap()
    qkT = nc.dram_tensor("qkT", [16, 64, T], BF16, kind=skind).ap()
    mix = nc.dram_tensor("mix", [T, 1024], BF16, kind=skind).ap()
    uvtab = nc.dram_tensor("uvtab", [16384, 2048], BF16, kind="Internal").ap()

    with ExitStack() as st:
        p = Prog(nc, st)
        identf = Tiles(p, st, "identf", [128, 128], F32)
        identb = Tiles(p, st, "identb", [128, 128], BF16)
        idf_t, idf_k = identf.next()
        idb_t, idb_k = identb.next()
        p.dma('sp', idf_t[:], cst["c_ident"], writes=[idf_k])
        p.op('dve', lambda e: e.tensor_copy(idb_t[:], idf_t[:]), reads=[idf_k], writes=[idb_k])
        ksum = Tiles(p, st, "ksum", [64, 8, NG * 2], F32)
        ksum_t, ksum_k = ksum.next()

        p.barrier()
        if "A" in stages:
            with ExitStack() as ph:
                win = Tiles(p, ph, "win", [128, 8, 3584], BF16)
                win_t, win_k = win.next()
                for k in range(8):
                    for hf in range(2):
                        p.dma('pool', win_t[:, k, hf * 1792:(hf + 1) * 1792],
                              w_in[k * 128:(k + 1) * 128, hf * 1792:(hf + 1) * 1792], writes=[win_k], lane=(2 * k + hf) % 4)
                g1b = Tiles(p, ph, "g1b", [128, 1024], F32)
                g1_t, g1_k = g1b.next()
                p.dma('sp', g1_t[:], norm1_g.partition_broadcast(128), writes=[g1_k])
                xt = Tiles(p, ph, "xt", [128, 1024], F32, bufs=3)
                junk = Tiles(p, ph, "junkA", [128, 1024], BF16, bufs=2)
                ssA = Tiles(p, ph, "ssA", [128, 4], F32, bufs=4)
                xn = Tiles(p, ph, "xn", [128, 1024], BF16, bufs=2)
                xnT = Tiles(p, ph, "xnT", [128, 8, 512], BF16, bufs=2)
                tok = Tiles(p, ph, "tok", [128, 2560], F32, bufs=2)
                qkst = Tiles(p, ph, "qkst", [128, 8, 512], BF16, bufs=2)
                psT = Tiles(p, ph, "psT", [128, 8, 128], BF16, bufs=1, psum=True)
                psA = Tiles(p, ph, "psA", [128, 512], F32, bufs=6, psum=True)
                ev = 0
                def prep_a(gi, ctx):
                    xnT_t, xnT_k = xnT.next()
                    for ti in range(4):
                        r0 = gi * 512 + ti * 128
                        xt_t, xt_k = xt.next()
                        p.dma('sp', xt_t[:], x[r0:r0 + 128, :], writes=[xt_k])
                        j_t, j_k = junk.next()
                        ss_t, ss_k = ssA.next()
                        p.op('act', lambda e: e.activation(j_t[:], xt_t[:], AF.Square, accum_out=ss_t[:, 0:1]),
                             reads=[xt_k], writes=[j_k, ss_k])
                        p.op('dve', lambda e: e.tensor_scalar(ss_t[:, 1:2], ss_t[:, 0:1], 1.0 / 1024, 1e-6, ALU.mult, ALU.add),
                             reads=[ss_k], writes=[ss_k])
                        p.op('act', lambda e: e.activation(ss_t[:, 2:3], ss_t[:, 1:2], AF.Sqrt), reads=[ss_k], writes=[ss_k])
                        p.op('dve', lambda e: e.reciprocal(ss_t[:, 3:4], ss_t[:, 2:3]), reads=[ss_k], writes=[ss_k])
                        xn_t, xn_k = xn.next()
                        p.op('dve', lambda e: e.scalar_tensor_tensor(xn_t[:], xt_t[:], ss_t[:, 3:4], g1_t[:], ALU.mult, ALU.mult),
                             reads=[xt_k, ss_k, g1_k], writes=[xn_k])
                        yield
                        pst_t, pst_k = psT.next()

                        def tr(e):
                            for k in range(8):
                                ins = e.transpose(pst_t[:, k, :], xn_t[:, k * 128:(k + 1) * 128], idb_t[:])
                            return ins
                        p.op('pe', tr, reads=[xn_k, idb_k], writes=[pst_k])
                        p.op('act', lambda e: e.copy(xnT_t[:, :, ti * 128:(ti + 1) * 128], pst_t[:]),
                             reads=[pst_k], writes=[xnT_k])
                        yield
                    ctx["xnT"] = (xnT_t, xnT_k)
                    yield

                def adv_a(gen, n):
                    if gen is None:
                        return
                    for _ in range(n):
                        try:
                            next(gen)
                        except StopIteration:
                            return
                NGA = NG
                ctxa = [dict() for _ in range(NGA + 1)]
                gen_a = prep_a(0, ctxa[0])
                adv_a(gen_a, 100)
                for gi in range(NGA):
                    gen_a = prep_a(gi + 1, ctxa[gi + 1]) if gi + 1 < NGA else None
                    xnT_t, xnT_k = ctxa[gi]["xnT"]
                    for ti in range(4):
                        r0 = gi * 512 + ti * 128
                        tok_t, tok_k = tok.next()
                        for cg in range(5):
                            ps_t, ps_k = psA.next()

                            def mm(e):
                                for k in range(8):
                                    ins = e.matmul(ps_t[:], xnT_t[:, k, ti * 128:(ti + 1) * 128],
                                                   win_t[:, k, 1024 + cg * 512:1024 + (cg + 1) * 512],
                                                   start=(k == 0), stop=(k == 7))
                                return ins
                            p.op('pe', mm, reads=[xnT_k, win_k], writes=[ps_k])
                            ev += 1
                            if ev % 2:
                                p.op('act', lambda e: e.copy(tok_t[:, cg * 512:(cg + 1) * 512], ps_t[:]),
                                     reads=[ps_k], writes=[tok_k])
                            else:
                                p.op('dve', lambda e: e.tensor_copy(tok_t[:, cg * 512:(cg + 1) * 512], ps_t[:]),
                                     reads=[ps_k], writes=[tok_k])
                        p.dma('pool', projT[r0:r0 + 128, :], tok_t[:], reads=[tok_k], writes=['projT'], sem=tok_k, lane='st', nowaw=True)
                        adv_a(gen_a, 1)
                    qk_t, qk_k = qkst.next()
                    for i2 in range(8):
                        adv_a(gen_a, 1)
                        ps_t, ps_k = psA.next()

                        def mm2(e):
                            for k in range(8):
                                ins = e.matmul(ps_t[:], win_t[:, k, i2 * 128:(i2 + 1) * 128], xnT_t[:, k, :],
                                               start=(k == 0), stop=(k == 7))
                            return ins
                        p.op('pe', mm2, reads=[xnT_k, win_k], writes=[ps_k])
                        if i2 < 4:
                            p.op('act', lambda e: e.mul(qk_t[:, i2, :], ps_t[:], 0.125), reads=[ps_k], writes=[qk_k])
                        else:
                            p.op('act', lambda e: e.copy(qk_t[:, i2, :], ps_t[:]), reads=[ps_k], writes=[qk_k])
                    p.dma('pool', qkT[:, :, gi * 512:(gi + 1) * 512].rearrange("(i2 two) d t -> (two d) i2 t", two=2), qk_t[:],
                          reads=[qk_k], writes=['qkT'], sem=qk_k, lane='st', nowaw=True)
                    if gi == 1 and "0" in stages:
                        for blk in range(4):
                            rows = slice(blk * 4096, (blk + 1) * 4096)
                            p.dma('pool', uvtab[rows, 0:1024], peer_u[rows, :], writes=['uvtab'], sem=('uvt', blk), lane=0, nowaw=True)
                            p.dma('pool', uvtab[rows, 1024:2048], peer_v[rows, :], writes=['uvtab'], sem=('uvt', blk), lane=1, nowaw=True)
                    adv_a(gen_a, 100)

        p.barrier()
        if "B" in stages:
            with ExitStack() as ph:
                qTa = Tiles(p, ph, "qTa", [76, 2048], BF16, bufs=2)
                kTa = Tiles(p, ph, "kTa", [76, 2048], BF16, bufs=2)
                vaug = Tiles(p, ph, "vaug", [128, 16, 65], BF16, bufs=2)
                kmb = Tiles(p, ph, "kmb", [64, 8], BF16, bufs=2)
                kmf = Tiles(p, ph, "kmf", [64, 8], F32, bufs=2)
                kjunk = Tiles(p, ph, "kjunk", [64, 256], BF16, bufs=2)
                zer = Tiles(p, ph, "zer", [64, 256], BF16)
                zer_t, zer_k = zer.next()
                p.op('pool', lambda e: e.memset(zer_t[:], 0.0), writes=[zer_k])
                elig = Tiles(p, ph, "elig", [128, 3, 16, 8], F32)
                tri = Tiles(p, ph, "tri", [128, 128], BF16)
                gm = Tiles(p, ph, "gm", [128, 16, 8], F32, bufs=2)
                m8 = Tiles(p, ph, "m8", [128, 16, 8], F32, bufs=2)
                selt = Tiles(p, ph, "selt", [128, 16, 8], F32, bufs=2)
                biast = Tiles(p, ph, "biast", [128, 16, 8], BF16, bufs=2)
                bT = Tiles(p, ph, "bT", [8, 2048], BF16, bufs=2)
                pT = Tiles(p, ph, "pT", [128, 16, 512], BF16, bufs=2)
                osb = Tiles(p, ph, "osb", [128, 4, 64], BF16, bufs=3)
                rc = Tiles(p, ph, "rc", [128, 4], F32, bufs=3)
                psS = Tiles(p, ph, "psS", [128, 512], F32, bufs=3, psum=True)
                psO = Tiles(p, ph, "psO", [128, 512], F32, bufs=2, psum=True)
                psG = Tiles(p, ph, "psG", [128, 16, 8], F32, bufs=1, psum=True)
                psB = Tiles(p, ph, "psB", [128, 2048], BF16, bufs=1, psum=True)
                el_t, el_k = elig.next()
                p.dma('sp', el_t[:].rearrange("p a b c -> p (a b c)"), cst["c_elig"], writes=[el_k])
                tri_t, tri_k = tri.next()
                p.dma('pool', tri_t[:], cst["c_tri"], writes=[tri_k])
                for b in range(2):
                    va_t, va_k = vaug.next()
                    p.op('pool', lambda e: e.memset(va_t[:, :, 64:65], 1.0), writes=[va_k])
                def advance_b(gen, n):
                    if gen is None:
                        return
                    for _ in range(n):
                        try:
                            next(gen)
                        except StopIteration:
                            return

                def prologue(s, h, ctx):
                    t0 = s * 2048
                    if True:
                        q_t, q_k = qTa.next()
                        k_t, k_k = kTa.next()
                        va_t, va_k = vaug.next()
                        p.dma('sp', q_t[0:64, :], qkT[h, :, t0:t0 + 2048], reads=['qkT'], writes=[q_k])
                        p.dma('sp', k_t[0:64, :], qkT[8 + h, :, t0:t0 + 2048], reads=['qkT'], writes=[k_k])
                        p.dma('pool', k_t[64:76, :], cst["c_kaug"][h], writes=[k_k], lane=1)
                        p.dma('pool', q_t[72:76, :], cst["c_qaug"][h], writes=[q_k], lane=1)
                        p.dma('pool', va_t[:, :, 0:64],
                              projT[t0:t0 + 2048, h * 64:(h + 1) * 64].rearrange("(kt p) c -> p kt c", p=128),
                              reads=['projT'], writes=[va_k])
                        yield
                        km_t, km_k = kmb.next()
                        kf_t, kf_k = kmf.next()
                        for n in range(8):
                            kj_t, kj_k = kjunk.next()
                            p.op('dve', lambda e: e.scalar_tensor_tensor(kj_t[:], k_t[0:64, n * 256:(n + 1) * 256], 1.0 / 256, zer_t[:],
                                                                         ALU.mult, ALU.add, accum_out=kf_t[:, n:n + 1]),
                                 reads=[k_k, zer_k], writes=[(kf_k, n)])
                        p.op('dve', lambda e: e.tensor_copy(km_t[:], kf_t[:]), reads=[(kf_k, n_) for n_ in range(8)], writes=[km_k])
                        pg_t, pg_k = psG.next()

                        def gmm(e):
                            for qt in range(16):
                                ins = e.matmul(pg_t[:, qt, :], q_t[0:64, qt * 128:(qt + 1) * 128], km_t[:], start=True, stop=True)
                            return ins
                        p.op('pe', gmm, reads=[q_k, km_k], writes=[pg_k])
                        yield
                        gm_t, gm_k = gm.next()
                        m8_t, m8_k = m8.next()
                        se_t, se_k = selt.next()
                        bi_t, bi_k = biast.next()
                        p.op('dve', lambda e: e.tensor_tensor(gm_t[:], pg_t[:], el_t[:, 0], ALU.add), reads=[pg_k, el_k], writes=[gm_k])
                        for qt in range(16):
                            p.op('dve', lambda e: e.max(m8_t[:, qt, :], gm_t[:, qt, :]), reads=[gm_k], writes=[m8_k])
                        yield
                        p.op('dve', lambda e: e.tensor_tensor(se_t[:], gm_t[:], m8_t[:, :, 2:3].to_broadcast([128, 16, 8]), ALU.is_ge),
                             reads=[gm_k, m8_k], writes=[se_k])
                        p.op('dve', lambda e: e.tensor_tensor(se_t[:], se_t[:], el_t[:, 1], ALU.mult), reads=[se_k, el_k], writes=[se_k])
                        p.op('dve', lambda e: e.scalar_tensor_tensor(bi_t[:], se_t[:], -1.0, el_t[:, 2], ALU.add, ALU.add),
                             reads=[se_k, el_k], writes=[bi_k])
                        yield
                        pb_t, pb_k = psB.next()

                        def btr(e):
                            for qt in range(16):
                                ins = e.transpose(pb_t[0:8, qt * 128:(qt + 1) * 128], bi_t[:, qt, :], idb_t[:])
                            return ins
                        p.op('pe', btr, reads=[bi_k, idb_k], writes=[pb_k])
                        bT_t, bT_k = bT.next()
                        p.op('act', lambda e: e.copy(bT_t[:], pb_t[0:8, :]), reads=[pb_k], writes=[bT_k])
                        p.dma('sp', q_t[64:72, :], bT_t[:], reads=[bT_k], writes=[q_k], lane=2)
                        ctx.update(q=(q_t, q_k), k=(k_t, k_k), va=(va_t, va_k), t0=t0, h=h)
                    yield

                def attention(ctx, gen):
                    q_t, q_k = ctx["q"]; k_t, k_k = ctx["k"]; va_t, va_k = ctx["va"]; t0 = ctx["t0"]; h = ctx["h"]
                    if True:
                        for g in range(4):
                            pT_t, pT_k = pT.next()
                            for kt in range(4 * g + 4):
                                c0 = max(0, kt * 128 - g * 512)
                                ps_t, ps_k = psS.next()
                                diag = kt >= 4 * g

                                def smm(e):
                                    ins = e.matmul(ps_t[:, c0:512], k_t[0:76, kt * 128:(kt + 1) * 128],
                                                   q_t[0:76, g * 512 + c0:(g + 1) * 512], start=True, stop=not diag)
                                    if diag:
                                        ins = e.matmul(ps_t[:, c0:c0 + 128], idb_t[:], tri_t[:], start=False, stop=True)
                                    return ins
                                p.op('pe', smm, reads=[k_k, q_k, idb_k, tri_k], writes=[ps_k])
                                p.op('act', lambda e: e.activation(pT_t[:, kt, c0:512], ps_t[:, c0:512], AF.Exp),
                                     reads=[ps_k], writes=[pT_k])
                                if kt % 4 == 3:
                                    advance_b(gen, 1)
                            po_t, po_k = psO.next()
                            po_v = po_t[:, 0:260].rearrange("p (a b) -> p a b", b=65)

                            def pv(e):
                                for qs in range(4):
                                    qt = 4 * g + qs
                                    for kt in range(qt + 1):
                                        ins = e.matmul(po_v[:, qs, :], pT_t[:, kt, qs * 128:(qs + 1) * 128], va_t[:, kt, :],
                                                       start=(kt == 0), stop=(kt == qt))
                                return ins
                            p.op('pe', pv, reads=[pT_k, va_k], writes=[po_k])
                            rc_t, rc_k = rc.next()
                            os_t, os_k = osb.next()
                            p.op('dve', lambda e: e.reciprocal(rc_t[:], po_v[:, :, 64]), reads=[po_k], writes=[rc_k])
                            p.op('dve', lambda e: e.tensor_tensor(os_t[:], po_v[:, :, 0:64],
                                                                  rc_t[:].unsqueeze(2).to_broadcast([128, 4, 64]), ALU.mult),
                                 reads=[po_k, rc_k], writes=[os_k])
                            r0 = t0 + g * 512
                            p.dma('sp', mix[r0:r0 + 512, h * 64:(h + 1) * 64].rearrange("(qs p) c -> p qs c", p=128), os_t[:],
                                  reads=[os_k], writes=['mix'], sem=os_k, lane='st', nowaw=True)

                units = [(s_, h_) for s_ in range(nseq) for h_ in range(8)]
                ctxb = [dict() for _ in units]
                g0 = prologue(units[0][0], units[0][1], ctxb[0])
                advance_b(g0, 100)
                for ui in range(len(units)):
                    gen = prologue(units[ui + 1][0], units[ui + 1][1], ctxb[ui + 1]) if ui + 1 < len(units) else None
                    attention(ctxb[ui], gen)
                    advance_b(gen, 100)

        p.barrier()
        pre_d = None
        if "D" in stages:
            wo_pre = Tiles(p, st, "wo", [128, 8, 1024], BF16)
            wq_pre = Tiles(p, st, "wq", [128, 8, 2048], BF16)
            g2_pre = Tiles(p, st, "g2b", [128, 2, 1024], F32)
            io_pre = Tiles(p, st, "io16", [128, 16], F32)
            pre_d = dict(wo=wo_pre.next(), wq=wq_pre.next(), g2=g2_pre.next(), io=io_pre.next())
            for k in range(8):
                p.dma('pool', pre_d["wo"][0][:, k, :], w_out[k * 128:(k + 1) * 128, :], writes=[pre_d["wo"][1]], lane=k % 4)
                p.dma('pool', pre_d["wq"][0][:, k, :], peer_wq[k * 128:(k + 1) * 128, :], writes=[pre_d["wq"][1]], lane=k % 4)
            p.dma('sp', pre_d["g2"][0][:, 0, :], norm2_g.partition_broadcast(128), writes=[pre_d["g2"][1]])
            p.dma('sp', pre_d["g2"][0][:, 1, :], normf_g.partition_broadcast(128), writes=[pre_d["g2"][1]], lane=1)
            p.dma('sp', pre_d["io"][0][:], cst["c_iota16"], writes=[pre_d["io"][1]])
        if "C" in stages:
            with ExitStack() as ph:
                amat = Tiles(p, ph, "amat", [128, 128], F32)
                bsel = Tiles(p, ph, "bsel", [128, 4], F32)
                cmask = Tiles(p, ph, "cmask", [128, 128], I32)
                lbb = Tiles(p, ph, "lbb", [128, 4, 512], F32)
                am_t, am_k = amat.next()
                bs_t, bs_k = bsel.next()
                cm_t, cm_k = cmask.next()
                lb_t, lb_k = lbb.next()
                p.dma('sp', am_t[:], cst["c_amat"], writes=[am_k])
                p.dma('sp', bs_t[:], cst["c_bsel"], writes=[bs_k])
                p.dma('sp', cm_t[:], cst["c_cmask"], writes=[cm_k])
                p.dma('sp', lb_t[:, 0, :], rec_lb[0:1, :].partition_broadcast(128), writes=[lb_k])
                p.dma('sp', lb_t[:, 1, :], rec_lb[1:2, :].partition_broadcast(128), writes=[lb_k], lane=1)
                p.dma('sp', lb_t[:, 3, :], rec_norm_g.partition_broadcast(128), writes=[lb_k], lane=2)
                p.op('dve', lambda e: e.tensor_tensor(lb_t[:, 0, :], lb_t[:, 0, :], lb_t[:, 1, :], ALU.subtract), reads=[lb_k], writes=[lb_k])
                p.op('act', lambda e: e.activation(lb_t[:, 1, :], lb_t[:, 0, :], AF.Sigmoid), reads=[lb_k], writes=[lb_k])
                p.op('dve', lambda e: e.tensor_scalar(lb_t[:, 2, :], lb_t[:, 1, :], -1.0, 1.0, ALU.mult, ALU.add), reads=[lb_k], writes=[lb_k])
                rec = Tiles(p, ph, "rec", [128, 2048], F32, bufs=2)
                fT = Tiles(p, ph, "fT", [128, 512], F32, bufs=2)
                lf = Tiles(p, ph, "lf", [128, 512], F32, bufs=2)
                kkT = Tiles(p, ph, "kkT", [128, 512], F32, bufs=2)
                qfT = Tiles(p, ph, "qfT", [128, 512], F32, bufs=2)
                eD = Tiles(p, ph, "eD", [128, 2, 512], F32, bufs=2)
                qtl = Tiles(p, ph, "qtl", [128, 512], BF16, bufs=2)
                ktl = Tiles(p, ph, "ktl", [128, 512], BF16, bufs=2)
                vbf = Tiles(p, ph, "vbf", [128, 512], BF16, bufs=2)
                qtT = Tiles(p, ph, "qtT", [128, 4, 128], BF16, bufs=2)
                ktT = Tiles(p, ph, "ktT", [128, 4, 128], BF16, bufs=2)
                ecs = Tiles(p, ph, "ecs", [128, 4, 4], F32, bufs=2)
                dl = Tiles(p, ph, "dl", [128, 4, 2], F32, bufs=2)
                pcs = Tiles(p, ph, "pcs", [128, 4, 4], F32, bufs=2)
                edl = Tiles(p, ph, "edl", [128, 4, 2], F32, bufs=2)
                AT = Tiles(p, ph, "AT", [128, 4, 128], BF16, bufs=2)
                Sst = Tiles(p, ph, "Sst", [128, 4, 128], F32, bufs=nseq)
                Sb = Tiles(p, ph, "Sb", [128, 128], BF16, bufs=4)
                tmpS = Tiles(p, ph, "tmpS", [128, 128], F32, bufs=3)
                ssC = Tiles(p, ph, "ssC", [128, 4, 4], F32, bufs=2)
                junkC = Tiles(p, ph, "junkC", [128, 128], BF16, bufs=2)
                sg = Tiles(p, ph, "sg", [128, 512], F32, bufs=2)
                o1 = Tiles(p, ph, "o1", [128, 4, 128], F32, bufs=2)
                recb = Tiles(p, ph, "recb", [128, 512], BF16, bufs=2)
                psD = Tiles(p, ph, "psD", [128, 512], F32, bufs=2, psum=True)
                psOo = Tiles(p, ph, "psOo", [128, 4, 128], F32, bufs=2, psum=True)
                psTq = Tiles(p, ph, "psTq", [128, 4, 128], BF16, bufs=1, psum=True)
                psC = Tiles(p, ph, "psC", [128, 4, 4], F32, bufs=1, psum=True)
                psSc = Tiles(p, ph, "psSc", [128, 128], F32, bufs=1, psum=True)
                psKV = Tiles(p, ph, "psKV", [128, 128], F32, bufs=1, psum=True)
                for b in range(2):
                    at_t, at_k = AT.next()
                    p.op('pool', lambda e: e.memset(at_t[:], 0.0), writes=[at_k])
                for s in range(nseq):
                    S_t, S_k = Sst.next()
                    p.op('pool', lambda e: e.memset(S_t[:], 0.0), writes=[S_k])
                def tile_c(s, ti):
                    if True:
                        S_t, S_k = Sst.t[s], ("Sst", s)
                        r0 = s * 2048 + ti * 128
                        rec_t, rec_k = rec.next()
                        p.dma('sp', rec_t[:], projT[r0:r0 + 128, 512:2560], reads=['projT'], writes=[rec_k])
                        f_t, f_k = fT.next()
                        lf_t, lf_k = lf.next()
                        kk_t, kk_k = kkT.next()
                        qf_t, qf_k = qfT.next()
                        p.op('act', lambda e: e.activation(f_t[:], rec_t[:, 512:1024], AF.Sigmoid), reads=[rec_k], writes=[f_k])
                        yield
                        p.op('act', lambda e: e.activation(qf_t[:], rec_t[:, 0:512], AF.Silu), reads=[rec_k], writes=[qf_k])
                        sg_t, sg_k = sg.next()
                        p.op('act', lambda e: e.activation(sg_t[:], rec_t[:, 1536:2048], AF.Silu), reads=[rec_k], writes=[sg_k])
                        p.op('dve', lambda e: e.tensor_tensor(f_t[:], f_t[:], lb_t[:, 2, :], ALU.mult), reads=[f_k, lb_k], writes=[f_k])
                        p.op('dve', lambda e: e.tensor_tensor(f_t[:], f_t[:], lb_t[:, 1, :], ALU.add), reads=[f_k, lb_k], writes=[f_k])
                        yield
                        p.op('act', lambda e: e.activation(lf_t[:], f_t[:], AF.Ln), reads=[f_k], writes=[lf_k])
                        p.op('dve', lambda e: e.tensor_scalar(kk_t[:], f_t[:], -1.0, 1.0, ALU.mult, ALU.add), reads=[f_k], writes=[kk_k])
                        yield
                        pd_t, pd_k = psD.next()
                        p.op('pe', lambda e: e.matmul(pd_t[:], am_t[:], lf_t[:], start=True, stop=True), reads=[am_k, lf_k], writes=[pd_k])
                        pc_t, pc_k = psC.next()

                        def cmm(e):
                            for h in range(4):
                                ins = e.matmul(pc_t[:, h, :], lf_t[:, h * 128:(h + 1) * 128], bs_t[:], start=True, stop=True)
                            return ins
                        p.op('pe', cmm, reads=[lf_k, bs_k], writes=[pc_k])
                        pcs_t, pcs_k = pcs.next()
                        p.op('act', lambda e: e.copy(pcs_t[:], pc_t[:]), reads=[pc_k], writes=[pcs_k])
                        yield
                        eD_t, eD_k = eD.next()
                        p.op('act', lambda e: e.activation(eD_t[:, 0, :], pd_t[:], AF.Exp), reads=[pd_k], writes=[eD_k])
                        p.op('act', lambda e: e.activation(eD_t[:, 1, :], pd_t[:], AF.Exp, scale=-1.0), reads=[pd_k], writes=[eD_k])
                        ec_t, ec_k = ecs.next()
                        dl_t, dl_k = dl.next()
                        ed_t, ed_k = edl.next()
                        pc_v = pcs_t[:].rearrange("p h (c two) -> p h c two", two=2)
                        p.op('dve', lambda e: e.tensor_tensor(dl_t[:], pc_v[:, :, :, 1], pc_v[:, :, :, 0], ALU.subtract), reads=[pcs_k], writes=[dl_k])
                        p.op('act', lambda e: e.activation(ec_t[:], pcs_t[:], AF.Exp), reads=[pcs_k], writes=[ec_k])
                        p.op('act', lambda e: e.activation(ed_t[:], dl_t[:], AF.Exp), reads=[dl_k], writes=[ed_k])
                        yield
                        qt_t, qt_k = qtl.next()
                        kt_t, kt_k = ktl.next()
                        vb_t, vb_k = vbf.next()
                        p.op('dve', lambda e: e.tensor_tensor(qt_t[:], qf_t[:], eD_t[:, 0, :], ALU.mult), reads=[qf_k, eD_k], writes=[qt_k])
                        p.op('dve', lambda e: e.tensor_tensor(kt_t[:], kk_t[:], eD_t[:, 1, :], ALU.mult), reads=[kk_k, eD_k], writes=[kt_k])
                        p.op('act', lambda e: e.copy(vb_t[:], rec_t[:, 1024:1536]), reads=[rec_k], writes=[vb_k])
                        yield
                        pq_t, pq_k = psTq.next()

                        def trq(e):
                            for h in range(4):
                                ins = e.transpose(pq_t[:, h, :], qt_t[:, h * 128:(h + 1) * 128], idb_t[:])
                            return ins
                        p.op('pe', trq, reads=[qt_k, idb_k], writes=[pq_k])
                        qT_t, qT_k = qtT.next()
                        p.op('act', lambda e: e.copy(qT_t[:], pq_t[:]), reads=[pq_k], writes=[qT_k])
                        yield
                        pk_t, pk_k = psTq.next()

                        def trk(e):
                            for h in range(4):
                                ins = e.transpose(pk_t[:, h, :], kt_t[:, h * 128:(h + 1) * 128], idb_t[:])
                            return ins
                        p.op('pe', trk, reads=[kt_k, idb_k], writes=[pk_k])
                        kT_t, kT_k = ktT.next()
                        p.op('dve', lambda e: e.tensor_copy(kT_t[:], pk_t[:]), reads=[pk_k], writes=[kT_k])
                        yield
                        at_t, at_k = AT.next()
                        po_t, po_k = psOo.next()
                        for h in range(4):
                            yield
                            hs = slice(h * 128, (h + 1) * 128)
                            psc_t, psc_k = psSc.next()
                            p.op('pe', lambda e: e.matmul(psc_t[:], kT_t[:, h, :], qT_t[:, h, :], start=True, stop=True),
                                 reads=[kT_k, qT_k], writes=[psc_k])
                            p.op('dve', lambda e: e.copy_predicated(at_t[:, h, :], cm_t[:], psc_t[:]), reads=[psc_k, cm_k], writes=[at_k])
                            sbs = []
                            for c in range(2):
                                yield
                                sb_t, sb_k = Sb.next()
                                p.op('dve', lambda e: e.tensor_scalar(sb_t[:], S_t[:, h, :], ec_t[:, h, 2 * c:2 * c + 1], None, ALU.mult),
                                     reads=[S_k, ec_k], writes=[sb_k])
                                sbs.append((sb_t, sb_k))
                                pkv_t, pkv_k = psKV.next()
                                cs = slice(c * 64, (c + 1) * 64)
                                p.op('pe', lambda e: e.matmul(pkv_t[:], kt_t[cs, hs], vb_t[cs, hs], start=True, stop=True),
                                     reads=[kt_k, vb_k], writes=[pkv_k])
                                tm_t, tm_k = tmpS.next()
                                p.op('dve', lambda e: e.tensor_scalar(tm_t[:], pkv_t[:], ed_t[:, h, c:c + 1], None, ALU.mult),
                                     reads=[pkv_k, ed_k], writes=[tm_k])
                                p.op('dve', lambda e: e.scalar_tensor_tensor(S_t[:, h, :], S_t[:, h, :], ec_t[:, h, 2 * c + 1:2 * c + 2],
                                                                             tm_t[:], ALU.mult, ALU.add),
                                     reads=[S_k, ec_k, tm_k], writes=[S_k])

                            def omm(e):
                                e.matmul(po_t[:, h, :], at_t[:, h, :], vb_t[:, hs], start=True, stop=False)
                                e.matmul(po_t[0:64, h, :], qT_t[:, h, 0:64], sbs[0][0][:], start=False, stop=True)
                                ins = e.matmul(po_t[64:128, h, :], qT_t[:, h, 64:128], sbs[1][0][:], start=False, stop=True)
                                return ins
                            p.op('pe', omm, reads=[at_k, vb_k, qT_k, sbs[0][1], sbs[1][1]], writes=[po_k])
                        yield
                        ss_t, ss_k = ssC.next()
                        for h in range(4):
                            j_t, j_k = junkC.next()
                            p.op('act', lambda e: e.activation(j_t[:], po_t[:, h, :], AF.Square, accum_out=ss_t[:, 0, h:h + 1]),
                                 reads=[po_k], writes=[j_k, ss_k])
                        p.op('dve', lambda e: e.tensor_scalar(ss_t[:, 1, :], ss_t[:, 0, :], 1.0 / 128, 1e-6, ALU.mult, ALU.add), reads=[ss_k], writes=[ss_k])
                        p.op('act', lambda e: e.activation(ss_t[:, 2, :], ss_t[:, 1, :], AF.Sqrt), reads=[ss_k], writes=[ss_k])
                        p.op('dve', lambda e: e.reciprocal(ss_t[:, 3, :], ss_t[:, 2, :]), reads=[ss_k], writes=[ss_k])
                        yield
                        p.op('dve', lambda e: e.tensor_tensor(sg_t[:], sg_t[:], lb_t[:, 3, :], ALU.mult), reads=[sg_k, lb_k], writes=[sg_k])
                        yield
                        o1_t, o1_k = o1.next()
                        p.op('dve', lambda e: e.tensor_tensor(o1_t[:], po_t[:], ss_t[:, 3, :].unsqueeze(2).to_broadcast([128, 4, 128]), ALU.mult),
                             reads=[po_k, ss_k], writes=[o1_k])
                        rb_t, rb_k = recb.next()
                        p.op('dve', lambda e: e.tensor_tensor(rb_t[:], o1_t[:].rearrange("p a b -> p (a b)"), sg_t[:], ALU.mult),
                             reads=[o1_k, sg_k], writes=[rb_k])
                        p.dma('pool', mix[r0:r0 + 128, 512:1024], rb_t[:], reads=[rb_k], writes=['mix'], sem=rb_k, lane='st', nowaw=True)

                def run_lockstep(gens):
                    live = list(gens)
                    while live:
                        nxt = []
                        for g_ in live:
                            try:
                                next(g_)
                                nxt.append(g_)
                            except StopIteration:
                                pass
                        live = nxt
                for ti in range(16):
                    for s0 in range(0, nseq, 2):
                        run_lockstep([tile_c(s_, ti) for s_ in range(s0, min(s0 + 2, nseq))])

        p.barrier()
        if "D" in stages:
            build_phase_d(nc, p, st, locals())
        p.barrier()
        p.fence('sp', reads=['out', 'mix', 'projT', 'qkT'])
        print("instructions:", p.ninst, "semaphores:", len(p.sems))
    return nc


def build_phase_d(nc, p, st, env):
    x = env["x"]; out = env["out"]; mix = env["mix"]; uvtab = env["uvtab"]; cst = env["cst"]
    w_out = env["w_out"]; norm2_g = env["norm2_g"]; normf_g = env["normf_g"]
    peer_wq = env["peer_wq"]; peer_sk = env["peer_sk"]
    idb_t, idb_k = env["idb_t"], env["idb_k"]
    NT = env["NT"]
    SG = 4
    NBUF = 5
    with ExitStack() as ph:
        skT = Tiles(p, ph, "skT", [128, 16, 128], BF16)
        skT_t, skT_k = skT.next()
        pre = env["pre_d"]
        wo_t, wo_k = pre["wo"]
        wq_t, wq_k = pre["wq"]
        g2_t, g2_k = pre["g2"]
        io_t, io_k = pre["io"]
        psT = Tiles(p, ph, "psTd", [128, 8, 128], BF16, bufs=1, psum=True)
        psY = Tiles(p, ph, "psY", [128, 512], F32, bufs=1, psum=True)
        psQ = Tiles(p, ph, "psQ", [128, 512], F32, bufs=2, psum=True)
        psP = Tiles(p, ph, "psP", [128, 1024], F32, bufs=2, psum=True)
        scr = Tiles(p, ph, "scr", [128, 2048], F32, bufs=1)
        scr_t, scr_k = scr.next()
        skn_v = scr_t[:, 0:1024].bitcast(BF16).rearrange("p (g n) -> p g n", n=128)
        p.dma('pool', skn_v, peer_sk.rearrange("g n d -> n g d"), writes=[scr_k])
        for half in range(2):
            pt_t, pt_k = psT.next()

            def trs(e):
                for j in range(8):
                    ins = e.transpose(pt_t[:, j, :], skn_v[:, half * 8 + j, :], idb_t[:])
                return ins
            p.op('pe', trs, reads=[scr_k, idb_k], writes=[pt_k])
            p.op('act', lambda e: e.copy(skT_t[:, half * 8:(half + 1) * 8, :], pt_t[:]), reads=[pt_k], writes=[skT_k])

        mx = Tiles(p, ph, "mx", [128, 1024], BF16, bufs=2)
        mxT = Tiles(p, ph, "mxT", [128, 8, 128], BF16, bufs=1)
        xt = Tiles(p, ph, "xtd", [128, 1024], F32, bufs=3)
        junk = Tiles(p, ph, "junkD", [128, 1024], BF16, bufs=2)
        junk2 = Tiles(p, ph, "junkD2", [128, 1024], BF16, bufs=1)
        ssD = Tiles(p, ph, "ssD", [128, 8], F32, bufs=3)
        mhalf = Tiles(p, ph, "mhalf", [128, 1], F32)
        mh_t, mh_k = mhalf.next()
        p.op('pool', lambda e: e.memset(mh_t[:], -0.5), writes=[mh_k])
        hnb = Tiles(p, ph, "hnb", [128, 1024], BF16, bufs=2)
        hnT = Tiles(p, ph, "hnT", [128, 8, 128], BF16, bufs=1)
        qpT = Tiles(p, ph, "qpT", [128, 16, 128], BF16, bufs=1)
        ss2 = Tiles(p, ph, "ss2", [128, 128], F32, bufs=4)
        v12 = Tiles(p, ph, "v12", [128, 16, 16], F32, bufs=1)
        i12 = Tiles(p, ph, "i12", [128, 16, 16], U32, bufs=1)
        i12f = Tiles(p, ph, "i12f", [128, 16, 16], F32, bufs=1)
        cand2 = Tiles(p, ph, "cand2", [128, 256], F32, bufs=4)
        sc = Tiles(p, ph, "sc", [128, 8, 16], F32, bufs=1)
        pidx = Tiles(p, ph, "pidx", [128, 8, 16], U32, bufs=1)
        pij = Tiles(p, ph, "pij", [128, 2, 128], U32, bufs=1)
        pijf = Tiles(p, ph, "pijf", [128, 2, 128], F32, bufs=1)
        abf = Tiles(p, ph, "abf", [128, 2, 128], F32, bufs=1)
        eidf = Tiles(p, ph, "eidf", [128, 128], F32, bufs=1)
        eid = Tiles(p, ph, "eid", [128, 128], U32, bufs=2)
        gts = Tiles(p, ph, "gts", [128, 8, 16], F32, bufs=2)
        zz = Tiles(p, ph, "zz", [128, 2, 8], F32, bufs=1)
        uvb = Tiles(p, ph, "uvb", [128, SG, 2048], BF16, bufs=NBUF)
        apre = Tiles(p, ph, "apre", [128, SG], F32, bufs=NBUF)
        wgt = Tiles(p, ph, "wgt", [128, SG], F32, bufs=NBUF)
        dg = Tiles(p, ph, "dg", [128, 128], BF16, bufs=6)
        ot = Tiles(p, ph, "ot", [128, 1024], F32, bufs=1)
        OFFLOAD = False
        prod = Tiles(p, ph, "prod", [128, 1024], F32, bufs=1) if OFFLOAD else None

        def stage1(ti, ctx):
            r0 = ti * 128
            mx_t, mx_k = mx.next()
            p.dma('sp', mx_t[:], mix[r0:r0 + 128, :], reads=['mix'], writes=[mx_k])
            xt_t, xt_k = xt.next()
            p.dma('sp', xt_t[:], x[r0:r0 + 128, :], writes=[xt_k])
            pt_t, pt_k = psT.next()

            def tr1(e):
                for k in range(8):
                    ins = e.transpose(pt_t[:, k, :], mx_t[:, k * 128:(k + 1) * 128], idb_t[:])
                return ins
            p.op('pe', tr1, reads=[mx_k, idb_k], writes=[pt_k])
            mxT_t, mxT_k = mxT.next()
            p.op('act', lambda e: e.copy(mxT_t[:], pt_t[:]), reads=[pt_k], writes=[mxT_k])
            yield
            h_t, h_k = xt_t, xt_k
            for hf in range(2):
                py_t, py_k = psY.next()

                def mmy(e):
                    for k in range(8):
                        ins = e.matmul(py_t[:], mxT_t[:, k, :], wo_t[:, k, hf * 512:(hf + 1) * 512], start=(k == 0), stop=(k == 7))
                    return ins
                p.op('pe', mmy, reads=[mxT_k, wo_k], writes=[py_k])
                yield
                p.op('dve', lambda e: e.tensor_tensor(h_t[:, hf * 512:(hf + 1) * 512], py_t[:], xt_t[:, hf * 512:(hf + 1) * 512], ALU.add),
                     reads=[py_k, xt_k], writes=[h_k])
            j_t, j_k = junk2.next()
            ss_t, ss_k = ssD.next()
            yield
            p.op('act', lambda e: e.activation(j_t[:], h_t[:], AF.Square, accum_out=ss_t[:, 0:1]), reads=[h_k], writes=[j_k, ss_k])
            yield
            p.op('dve', lambda e: e.tensor_scalar(ss_t[:, 1:2], ss_t[:, 0:1], 1.0 / 1024, 1e-6, ALU.mult, ALU.add), reads=[ss_k], writes=[ss_k])
            p.op('act', lambda e: e.activation(ss_t[:, 2:3], ss_t[:, 1:2], AF.Sqrt), reads=[ss_k], writes=[ss_k])
            yield
            p.op('dve', lambda e: e.reciprocal(ss_t[:, 3:4], ss_t[:, 2:3]), reads=[ss_k], writes=[ss_k])
            hn_t, hn_k = hnb.next()
            p.op('dve', lambda e: e.scalar_tensor_tensor(hn_t[:], h_t[:], ss_t[:, 3:4], g2_t[:, 0, :], ALU.mult, ALU.mult),
                 reads=[h_k, ss_k, g2_k], writes=[hn_k])
            yield
            pt_t, pt_k = psT.next()

            def tr2(e):
                for k in range(8):
                    ins = e.transpose(pt_t[:, k, :], hn_t[:, k * 128:(k + 1) * 128], idb_t[:])
                return ins
            p.op('pe', tr2, reads=[hn_k, idb_k], writes=[pt_k])
            hnT_t, hnT_k = hnT.next()
            p.op('act', lambda e: e.copy(hnT_t[:], pt_t[:]), reads=[pt_k], writes=[hnT_k])
            yield
            qp_t, qp_k = qpT.next()
            for gq in range(4):
                pq_t, pq_k = psQ.next()

                def mmq(e):
                    for gg in range(4):
                        g = gq * 4 + gg
                        for k in range(8):
                            ins = e.matmul(pq_t[:, gg * 128:(gg + 1) * 128], wq_t[:, k, g * 128:(g + 1) * 128], hnT_t[:, k, :],
                                           start=(k == 0), stop=(k == 7))
                    return ins
                p.op('pe', mmq, reads=[wq_k, hnT_k], writes=[pq_k])
                p.op('act', lambda e: e.copy(qp_t[:, gq * 4:(gq + 1) * 4, :], pq_t[:].rearrange("p (a b) -> p a b", b=128)),
                     reads=[pq_k], writes=[qp_k])
            yield
            s_v = scr_t[:].rearrange("p (g n) -> p g n", n=128)
            for gq in range(4):
                pq_t, pq_k = psQ.next()

                def mms(e):
                    for gg in range(4):
                        g = gq * 4 + gg
                        ins = e.matmul(pq_t[:, gg * 128:(gg + 1) * 128], qp_t[:, g, :], skT_t[:, g, :], start=True, stop=True)
                    return ins
                p.op('pe', mms, reads=[qp_k, skT_k], writes=[pq_k])
                p.op('act', lambda e: e.copy(s_v[:, gq * 4:(gq + 1) * 4, :], pq_t[:].rearrange("p (a b) -> p a b", b=128)),
                     reads=[pq_k], writes=[scr_k])
            yield
            yield
            v_t, v_k = v12.next()
            i_t, i_k = i12.next()
            for g0 in range(0, 16, 4):
                gs = [g0, g0 + 1, g0 + 2, g0 + 3]
                s2s = [ss2.next() for _ in gs]
                for g in gs:
                    p.op('dve', lambda e: e.max(v_t[:, g, 0:8], s_v[:, g, :]), reads=[scr_k], writes=[(v_k, g)])
                for g in gs:
                    p.op('dve', lambda e: e.max_index(i_t[:, g, 0:8], v_t[:, g, 0:8], s_v[:, g, :]), reads=[scr_k, (v_k, g)], writes=[(i_k, g)])
                for g, (s2_t, s2_k) in zip(gs, s2s):
                    p.op('dve', lambda e: e.match_replace(s2_t[:], v_t[:, g, 0:8], s_v[:, g, :], NEG), reads=[scr_k, (v_k, g)], writes=[s2_k])
                for g, (s2_t, s2_k) in zip(gs, s2s):
                    p.op('dve', lambda e: e.max(v_t[:, g, 8:16], s2_t[:]), reads=[s2_k], writes=[(v_k, g, 1)])
                for g, (s2_t, s2_k) in zip(gs, s2s):
                    p.op('dve', lambda e: e.max_index(i_t[:, g, 8:16], v_t[:, g, 8:16], s2_t[:]), reads=[s2_k, (v_k, g, 1)], writes=[(i_k, g, 1)])
                yield
            vall = [(v_k, g) for g in range(16)] + [(v_k, g, 1) for g in range(16)]
            iall = [(i_k, g) for g in range(16)] + [(i_k, g, 1) for g in range(16)]
            if_t, if_k = i12f.next()
            p.op('dve', lambda e: e.tensor_copy(if_t[:], i_t[:]), reads=iall, writes=[if_k])
            ca_v = scr_t[:].rearrange("p (h c) -> p h c", c=256)
            v_v = v_t[:].rearrange("p (h c) k -> p h c k", c=2)
            p.op('dve', lambda e: e.tensor_tensor(ca_v.rearrange("p h (i j) -> p h i j", j=16),
                                                  v_v[:, :, 0, :].unsqueeze(3).to_broadcast([128, 8, 16, 16]),
                                                  v_v[:, :, 1, :].unsqueeze(2).to_broadcast([128, 8, 16, 16]), ALU.add),
                 reads=vall, writes=[scr_k])
            yield
            sc_t, sc_k = sc.next()
            pi_t, pi_k = pidx.next()
            for h0 in range(0, 8, 4):
                hs = [h0, h0 + 1, h0 + 2, h0 + 3]
                c2s = [cand2.next() for _ in hs]
                for h in hs:
                    p.op('dve', lambda e: e.max(sc_t[:, h, 0:8], ca_v[:, h, :]), reads=[scr_k], writes=[(sc_k, h)])
                for h in hs:
                    p.op('dve', lambda e: e.max_index(pi_t[:, h, 0:8], sc_t[:, h, 0:8], ca_v[:, h, :]), reads=[scr_k, (sc_k, h)], writes=[(pi_k, h)])
                for h, (c2_t, c2_k) in zip(hs, c2s):
                    p.op('dve', lambda e: e.match_replace(c2_t[:], sc_t[:, h, 0:8], ca_v[:, h, :], NEG), reads=[scr_k, (sc_k, h)], writes=[c2_k])
                for h, (c2_t, c2_k) in zip(hs, c2s):
                    p.op('dve', lambda e: e.max(sc_t[:, h, 8:16], c2_t[:]), reads=[c2_k], writes=[(sc_k, h, 1)])
                for h, (c2_t, c2_k) in zip(hs, c2s):
                    p.op('dve', lambda e: e.max_index(pi_t[:, h, 8:16], sc_t[:, h, 8:16], c2_t[:]), reads=[c2_k, (sc_k, h, 1)], writes=[(pi_k, h, 1)])
                yield
            scall = [(sc_k, h) for h in range(8)] + [(sc_k, h, 1) for h in range(8)]
            piall = [(pi_k, h) for h in range(8)] + [(pi_k, h, 1) for h in range(8)]
            g_t, g_k = gts.next()
            z_t, z_k = zz.next()
            p.op('dve', lambda e: e.tensor_tensor(g_t[:], sc_t[:], sc_t[:, :, 0:1].to_broadcast([128, 8, 16]), ALU.subtract),
                 reads=scall, writes=[g_k])
            yield
            for h in range(8):
                p.op('act', lambda e: e.activation(g_t[:, h, :], g_t[:, h, :], AF.Exp, accum_out=z_t[:, 0, h:h + 1]),
                     reads=[g_k], writes=[g_k, z_k])
            yield
            GATES_TAIL = True
            pij_t, pij_k = pij.next()
            pf_t, pf_k = pijf.next()
            pi_flat = pi_t[:].rearrange("p h k -> p (h k)")
            p.op('dve', lambda e: e.tensor_single_scalar(pij_t[:, 0, :], pi_flat, 4, ALU.logical_shift_right), reads=piall, writes=[pij_k])
            p.op('dve', lambda e: e.tensor_single_scalar(pij_t[:, 1, :], pi_flat, 15, ALU.bitwise_and), reads=piall, writes=[pij_k])
            p.op('dve', lambda e: e.tensor_copy(pf_t[:], pij_t[:]), reads=[pij_k], writes=[pf_k])
            ab_t, ab_k = abf.next()
            if_v = if_t[:].rearrange("p (h c) k -> p h c k", c=2)
            oh_v3 = scr_t[:].rearrange("p (s i) -> p s i", i=16)
            for c in range(2):
                p.op('dve', lambda e: e.tensor_tensor(oh_v3, io_t[:].unsqueeze(1).to_broadcast([128, 128, 16]),
                                                      pf_t[:, c, :].unsqueeze(2).to_broadcast([128, 128, 16]), ALU.is_equal),
                     reads=[io_k, pf_k], writes=[scr_k])
                oh_v = oh_v3.rearrange("p (h k) i -> p h k i", h=8)
                p.op('dve', lambda e: e.tensor_tensor(oh_v, oh_v, if_v[:, :, c, :].unsqueeze(2).to_broadcast([128, 8, 16, 16]), ALU.mult),
                     reads=[scr_k, if_k], writes=[scr_k])
                yield
                p.op('dve', lambda e: e.tensor_tensor(oh_v3[:, :, 0:8], oh_v3[:, :, 0:8], oh_v3[:, :, 8:16], ALU.add), reads=[scr_k], writes=[scr_k])
                p.op('dve', lambda e: e.tensor_tensor(oh_v3[:, :, 0:4], oh_v3[:, :, 0:4], oh_v3[:, :, 4:8], ALU.add), reads=[scr_k], writes=[scr_k])
                p.op('dve', lambda e: e.tensor_tensor(oh_v3[:, :, 0:2], oh_v3[:, :, 0:2], oh_v3[:, :, 2:4], ALU.add), reads=[scr_k], writes=[scr_k])
                p.op('dve', lambda e: e.tensor_tensor(ab_t[:, c, :], oh_v3[:, :, 0], oh_v3[:, :, 1], ALU.add), reads=[scr_k], writes=[ab_k])
                yield
            p.op('dve', lambda e: e.reciprocal(z_t[:, 1, :], z_t[:, 0, :]), reads=[z_k], writes=[z_k])
            p.op('dve', lambda e: e.tensor_tensor(g_t[:], g_t[:], z_t[:, 1, :].unsqueeze(2).to_broadcast([128, 8, 16]), ALU.mult),
                 reads=[g_k, z_k], writes=[g_k])
            ef_t, ef_k = eidf.next()
            p.op('dve', lambda e: e.scalar_tensor_tensor(ef_t[:], ab_t[:, 0, :], 128.0, ab_t[:, 1, :], ALU.mult, ALU.add),
                 reads=[ab_k], writes=[ef_k])
            ei_t, ei_k = eid.next()
            p.op('dve', lambda e: e.tensor_copy(ei_t[:], ef_t[:]), reads=[ef_k], writes=[ei_k])
            ctx.update(h=(h_t, h_k), hn=(hn_t, hn_k), ss=(ss_t, ss_k), ei=(ei_t, ei_k), g=(g_t, g_k), r0=r0)

        def advance(gen, n):
            if gen is None:
                return
            for _ in range(n):
                try:
                    next(gen)
                except StopIteration:
                    return

        def stage2(ctx, gen, prev_epi):
            h_t, h_k = ctx["h"]; hn_t, hn_k = ctx["hn"]; ss_t, ss_k = ctx["ss"]
            ei_t, ei_k = ctx["ei"]; g_t, g_k = ctx["g"]; r0 = ctx["r0"]
            pp_t, pp_k = psP.next()
            g_flat = g_t[:].rearrange("p h k -> p (h k)")
            NGRP = 128 // SG
            issued = {}

            def issue(grp):
                uv_t, uv_k = uvb.next()
                for sl in range(SG):
                    slot = grp * SG + sl
                    p.dma('pool', None, None, reads=[ei_k, 'uvtab'], writes=[uv_k], lane=sl,
                          fn=lambda e: e.indirect_dma_start(out=uv_t[:, sl, :], out_offset=None, in_=uvtab,
                                                            in_offset=bass.IndirectOffsetOnAxis(ap=ei_t[:, slot:slot + 1], axis=0)))
                issued[grp] = (uv_t, uv_k)
            PD = NBUF - 2
            for grp in range(min(PD, NGRP)):
                issue(grp)

            def post_b(grp, uv_t, uv_k, w_t, w_k):
                p.op('dve', lambda e: e.tensor_tensor(w_t[:], w_t[:], g_flat[:, grp * SG:(grp + 1) * SG], ALU.mult), reads=[w_k, g_k], writes=[w_k])
                for sl in range(SG):
                    slot = grp * SG + sl
                    dg_t, dg_k = dg.next()
                    p.op('act', lambda e: e.activation(dg_t[:], idb_t[:], AF.Copy, scale=w_t[:, sl:sl + 1]), reads=[idb_k, w_k], writes=[dg_k])

                    def mmv(e):
                        e.matmul(pp_t[:, 0:512], dg_t[:], uv_t[:, sl, 1024:1536], start=(slot == 0), stop=(slot == 127))
                        return e.matmul(pp_t[:, 512:1024], dg_t[:], uv_t[:, sl, 1536:2048], start=(slot == 0), stop=(slot == 127))
                    p.op('pe', mmv, reads=[dg_k, uv_k], writes=[pp_k])
            pend = None
            for grp in range(NGRP):
                if grp + PD < NGRP:
                    issue(grp + PD)
                uv_t, uv_k = issued.pop(grp)
                ap_t, ap_k = apre.next()
                w_t, w_k = wgt.next()
                for sl in range(SG):
                    if sl == SG - 1 and OFFLOAD:
                        pr_t, pr_k = prod.next()
                        p.op('pool', lambda e: e.tensor_tensor(pr_t[:], uv_t[:, sl, 0:1024], hn_t[:], ALU.mult), reads=[uv_k, hn_k], writes=[pr_k])
                        jj_t, jj_k = junk2.next()
                        p.op('act', lambda e: e.activation(jj_t[:], pr_t[:], AF.Copy, accum_out=ap_t[:, sl:sl + 1]),
                             reads=[pr_k], writes=[jj_k, (ap_k, 1)])
                        continue
                    jj_t, jj_k = junk.next()
                    p.op('dve', lambda e: e.scalar_tensor_tensor(jj_t[:], uv_t[:, sl, 0:1024], 1.0, hn_t[:], ALU.mult, ALU.mult,
                                                                 accum_out=ap_t[:, sl:sl + 1]),
                         reads=[uv_k, hn_k], writes=[(ap_k, 'c', sl)])
                p.op('act', lambda e: e.activation(w_t[:], ap_t[:], AF.Gelu), reads=[(ap_k, 'c', sl_) for sl_ in range(SG)], writes=[w_k])
                if pend is not None:
                    post_b(*pend)
                pend = (grp, uv_t, uv_k, w_t, w_k)
                if grp == 3 and prev_epi is not None:
                    prev_epi()
                advance(gen, 1)
            post_b(*pend)

            def epilogue():
                p.op('dve', lambda e: e.tensor_tensor(h_t[:], pp_t[:], h_t[:], ALU.add), reads=[pp_k, h_k], writes=[h_k])
                j_t, j_k = junk2.next()
                p.op('act', lambda e: e.activation(j_t[:], h_t[:], AF.Square, accum_out=ss_t[:, 4:5]), reads=[h_k], writes=[j_k, ss_k])
                p.op('dve', lambda e: e.tensor_scalar(ss_t[:, 5:6], ss_t[:, 4:5], 1.0 / 1024, 1e-6, ALU.mult, ALU.add), reads=[ss_k], writes=[ss_k])
                p.op('act', lambda e: e.activation(ss_t[:, 6:7], ss_t[:, 5:6], AF.Sqrt), reads=[ss_k], writes=[ss_k])
                p.op('dve', lambda e: e.reciprocal(ss_t[:, 7:8], ss_t[:, 6:7]), reads=[ss_k], writes=[ss_k])
                o_t, o_k = ot.next()
                p.op('dve', lambda e: e.scalar_tensor_tensor(o_t[:], h_t[:], ss_t[:, 7:8], g2_t[:, 1, :], ALU.mult, ALU.mult),
                     reads=[h_k, ss_k, g2_k], writes=[o_k])
                p.dma('sp', out[r0:r0 + 128, :], o_t[:], reads=[o_k], writes=['out'], sem=o_k, lane='st', nowaw=True)
            return epilogue

        ctxs = [dict() for _ in range(NT)]
        g0 = stage1(0, ctxs[0])
        advance(g0, 10 ** 6)
        epi = None
        for ti in range(NT):
            gen = stage1(ti + 1, ctxs[ti + 1]) if ti + 1 < NT else None
            epi = stage2(ctxs[ti], gen, epi)
            advance(gen, 10 ** 6)
        epi()


_CONSTS = None


def kernel(x, norm1_g, w_in, rec_lb_logits, rec_norm_g, w_out, norm2_g, peer_wq, peer_subkeys, peer_u, peer_v, normf_g):
    global _CONSTS
    if _CONSTS is None:
        _CONSTS = make_consts()
    n = 8
    x = np.asarray(x, np.float32)
    B = x.shape[0]
    per = B // n
    shared = {
        "norm1_g": np.ascontiguousarray(np.asarray(norm1_g, np.float32).reshape(1, 1024)),
        "w_in": np.ascontiguousarray(np.asarray(w_in, np.float32).reshape(1024, 3584)),
        "rec_lb_logits": np.ascontiguousarray(np.asarray(rec_lb_logits, np.float32).reshape(2, 512)),
        "rec_norm_g": np.ascontiguousarray(np.asarray(rec_norm_g, np.float32).reshape(1, 512)),
        "w_out": np.ascontiguousarray(np.asarray(w_out, np.float32).reshape(1024, 1024)),
        "norm2_g": np.ascontiguousarray(np.asarray(norm2_g, np.float32).reshape(1, 1024)),
        "peer_wq": np.ascontiguousarray(np.asarray(peer_wq, np.float32).reshape(1024, 2048)),
        "peer_subkeys": np.ascontiguousarray(np.asarray(peer_subkeys, np.float32).reshape(16, 128, 128)),
        "peer_u": np.ascontiguousarray(np.asarray(peer_u, np.float32).reshape(16384, 1024)),
        "peer_v": np.ascontiguousarray(np.asarray(peer_v, np.float32).reshape(16384, 1024)),
        "normf_g": np.ascontiguousarray(np.asarray(normf_g, np.float32).reshape(1, 1024)),
    }
    shared.update(_CONSTS)
    nc = build(nseq=per)
    in_maps = []
    for i in range(n):
        m = dict(shared)
        m["x"] = np.ascontiguousarray(x[i * per:(i + 1) * per].reshape(per * 2048, 1024))
        in_maps.append(m)
    res = run_bass_kernel_spmd(nc, in_maps, core_ids=list(range(n)))
    outs = [np.asarray(r["out"], np.float32).reshape(per, 2048, 1024) for r in res.results]
    return np.concatenate(outs, axis=0)
```

```python
import numpy as np
from contextlib import ExitStack
import concourse.bass as bass
import concourse.mybir as mybir
from concourse.bass_utils import run_bass_kernel_spmd

F32 = mybir.dt.float32
BF16 = mybir.dt.bfloat16
U32 = mybir.dt.uint32
I32 = mybir.dt.int32
AF = mybir.ActivationFunctionType
ALU = mybir.AluOpType
AX = mybir.AxisListType

SEM_LIMIT = 30000
NEG = -1.0e30
MASKV = 32768.0


class Prog:
    ENG = ('pe', 'dve', 'act', 'pool', 'sp')

    def __init__(self, nc, stack):
        self.nc = nc
        self.stack = stack
        self.eng = {'pe': nc.tensor, 'dve': nc.vector, 'act': nc.scalar, 'pool': nc.gpsimd, 'sp': nc.sync}
        self.semcnt = {}
        self.sems = {}
        self.seen = {e: {} for e in self.ENG}
        self.res = {}
        self.ninst = 0

    def _sem(self, key):
        if key not in self.sems:
            self.sems[key] = self.stack.enter_context(self.nc.semaphore(f"s{len(self.sems)}"))
        return self.sems[key]

    def _bump(self, name, inc):
        ep, val = self.semcnt.get(name, (0, 0))
        if val + inc > SEM_LIMIT:
            ep += 1
            val = 0
        val += inc
        self.semcnt[name] = (ep, val)
        return ((name, ep), val)

    @staticmethod
    def _sibling(semname, w):
        return isinstance(semname, tuple) and len(semname) >= 2 and semname[0] == 'd' and semname[1] == w

    def _deps(self, reads, writes, myname, group, nowaw=False):
        d = {}

        def add(k, v):
            if d.get(k, 0) < v:
                d[k] = v
        for r in reads:
            st = self.res.get(r)
            if st:
                for (k, v) in st[0].values():
                    add(k, v)
        self._saved = {}
        for w in writes:
            st = self.res.get(w)
            sv = {}
            if st:
                sib = group and len(st[0]) > 0 and all(self._sibling(n, w) for n in st[0]) and self._sibling(myname, w)
                if sib or nowaw:
                    sv = dict(st[2])
                else:
                    for (k, v) in st[0].values():
                        sv[k] = max(sv.get(k, 0), v)
                for k, v in st[1].items():
                    sv[k] = max(sv.get(k, 0), v)
                for k, v in sv.items():
                    add(k, v)
            self._saved[w] = sv
        return d

    def _waits(self, eng, d):
        e = self.eng[eng]
        for k, v in d.items():
            if eng == 'pe' and k[0] == 'pe':
                continue
            if self.seen[eng].get(k, 0) >= v:
                continue
            self.seen[eng][k] = v
            e.wait_ge(self._sem(k), v)

    def _update(self, reads, writes, myname, mykey, myval, group, nowaw=False):
        for r in reads:
            st = self.res.setdefault(r, [{}, {}, {}])
            st[1][mykey] = myval
        for w in writes:
            st = self.res.get(w)
            if st and (nowaw or (group and len(st[0]) > 0 and all(self._sibling(n, w) for n in st[0]) and self._sibling(myname, w))):
                wr = dict(st[0])
            else:
                wr = {}
            wr[myname] = (mykey, myval)
            self.res[w] = [wr, {}, self._saved.get(w, {})]

    def op(self, eng, fn, reads=(), writes=()):
        mykey, myval = self._bump(eng, 1)
        d = self._deps(reads, writes, eng, False)
        self._waits(eng, d)
        ins = fn(self.eng[eng])
        ins.then_inc(self._sem(mykey), 1)
        self._update(reads, writes, eng, mykey, myval, False)
        self.ninst += 1
        return ins

    def dma(self, qeng, out, in_, reads=(), writes=(), sem=None, lane=0, group=True, fn=None, nowaw=False, **kw):
        semname = ('d', sem if sem is not None else writes[0], lane)
        prev = self.semcnt.get(semname)
        mykey, myval = self._bump(semname, 16)
        d = self._deps(reads, writes, semname, group, nowaw)
        if prev is not None and prev[1] > 0:
            pk = (semname, prev[0])
            d[pk] = max(d.get(pk, 0), prev[1])
        self._waits(qeng, d)
        e = self.eng[qeng]
        if fn is not None:
            ins = fn(e)
        else:
            ins = e.dma_start(out=out, in_=in_, **kw)
        ins.then_inc(self._sem(mykey), 16)
        self._update(reads, writes, semname, mykey, myval, group, nowaw)
        self.ninst += 1
        return ins

    def barrier(self):
        snap = {(name, ep): val for name, (ep, val) in self.semcnt.items()}
        for eng in self.ENG:
            self._waits(eng, snap)

    def fence(self, eng, reads=(), writes=()):
        d = self._deps(reads, writes, None, False)
        self._waits(eng, d)


class Tiles:
    def __init__(self, p, stack, name, shape, dtype, bufs=1, psum=False):
        alloc = p.nc.psum_tensor if psum else p.nc.sbuf_tensor
        self.t = [stack.enter_context(alloc(f"{name}_{i}", shape, dtype)) for i in range(bufs)]
        self.name = name
        self.bufs = bufs
        self.i = -1

    def next(self):
        self.i += 1
        return self.cur()

    def cur(self):
        b = self.i % self.bufs
        return self.t[b], (self.name, b)


class Views:
    def __init__(self, items):
        self.items = items
        self.i = -1

    def next(self):
        self.i += 1
        return self.items[self.i % len(self.items)]


def make_consts():
    c = {}
    c["c_ident"] = np.eye(128, dtype=np.float32)
    kq = np.arange(128)
    c["c_tri"] = np.where(kq[:, None] > kq[None, :], -MASKV, 0.0).astype(np.float32)
    pos = np.arange(2048)
    blk = pos // 256
    r = pos % 256
    kaug = np.zeros((8, 12, 2048), np.float32)
    qaug = np.zeros((8, 4, 2048), np.float32)
    for h in range(8):
        sl = 2.0 ** (-(h + 1))
        for n in range(8):
            kaug[h, n] = MASKV * (blk == n)
        kaug[h, 8] = sl * r
        kaug[h, 9] = 1.0
        kaug[h, 10] = sl * 256.0 * blk
        kaug[h, 11] = 1.0
        qaug[h, 0] = 1.0
        qaug[h, 1] = -sl * r
        qaug[h, 2] = 1.0
        qaug[h, 3] = -sl * 256.0 * blk
    c["c_kaug"] = kaug
    c["c_qaug"] = qaug
    el = np.zeros((3, 16, 8), np.float32)
    for qt in range(16):
        j = qt // 2
        for n in range(8):
            el[0, qt, n] = 0.0 if n < j else NEG
            el[1, qt, n] = 1.0 if n < j else 0.0
            el[2, qt, n] = 1.0 if n == j else 0.0
    c["c_elig"] = np.broadcast_to(el.reshape(1, 3 * 16 * 8), (128, 384)).copy()
    s = np.arange(128)
    ch = s // 64
    mid = ch * 64 + 31
    same = ch[:, None] == ch[None, :]
    amat = (same & (s[:, None] <= s[None, :])).astype(np.float32) - (same & (s[:, None] <= mid[None, :])).astype(np.float32)
    c["c_amat"] = amat.astype(np.float32)
    bsel = np.zeros((128, 4), np.float32)
    bsel[:, 0] = (s <= 31)
    bsel[:, 1] = (s <= 63)
    bsel[:, 2] = (s >= 64) & (s <= 95)
    bsel[:, 3] = (s >= 64)
    c["c_bsel"] = bsel
    c["c_cmask"] = (same & (s[:, None] <= s[None, :])).astype(np.int32)
    c["c_iota16"] = np.broadcast_to(np.arange(16, dtype=np.float32)[None, :], (128, 16)).copy()
    return c


CONST_SHAPES = {"c_ident": ([128, 128], F32), "c_tri": ([128, 128], F32), "c_kaug": ([8, 12, 2048], F32),
                "c_qaug": ([8, 4, 2048], F32), "c_elig": ([128, 384], F32), "c_amat": ([128, 128], F32),
                "c_bsel": ([128, 4], F32), "c_cmask": ([128, 128], I32), "c_iota16": ([128, 16], F32)}


def build(nseq=4, dbg=False, stages="0ABCD"):
    T = nseq * 2048
    NT = T // 128
    NG = T // 512
    nc = bass.Bass("TRN2", target_bir_lowering=False)

    def din(name, shape, dt=F32):
        return nc.dram_tensor(name, shape, dt, kind="ExternalInput").ap()

    x = din("x", [T, 1024])
    norm1_g = din("norm1_g", [1, 1024])
    w_in = din("w_in", [1024, 3584])
    rec_lb = din("rec_lb_logits", [2, 512])
    rec_norm_g = din("rec_norm_g", [1, 512])
    w_out = din("w_out", [1024, 1024])
    norm2_g = din("norm2_g", [1, 1024])
    peer_wq = din("peer_wq", [1024, 2048])
    peer_sk = din("peer_subkeys", [16, 128, 128])
    peer_u = din("peer_u", [16384, 1024])
    peer_v = din("peer_v", [16384, 1024])
    normf_g = din("normf_g", [1, 1024])
    cst = {k: din(k, sh, dt) for k, (sh, dt) in CONST_SHAPES.items()}
    out = nc.dram_tensor("out", [T, 1024], F32, kind="ExternalOutput").ap()
    skind = "ExternalOutput" if dbg else "Internal"
    projT = nc.dram_tensor("projT", [T, 2560], F32, kind=skind).ap()
    qkT = nc.dram_tensor("qkT", [16, 64, T], BF16, kind=skind).ap()
    mix = nc.dram_tensor("mix", [T, 1024], BF16, kind=skind).ap()
    uvtab = nc.dram_tensor("uvtab", [16384, 2048], BF16, kind="Internal").ap()

    with ExitStack() as st:
        p = Prog(nc, st)
        identf = Tiles(p, st, "identf", [128, 128], F32)
        identb = Tiles(p, st, "identb", [128, 128], BF16)
        idf_t, idf_k = identf.next()
        idb_t, idb_k = identb.next()
        p.dma('sp', idf_t[:], cst["c_ident"], writes=[idf_k])
        p.op('dve', lambda e: e.tensor_copy(idb_t[:], idf_t[:]), reads=[idf_k], writes=[idb_k])
        ksum = Tiles(p, st, "ksum", [64, 8, NG * 2], F32)
        ksum_t, ksum_k = ksum.next()

        p.barrier()
        if "A" in stages:
            with ExitStack() as ph:
                win = Tiles(p, ph, "win", [128, 8, 3584], BF16)
                win_t, win_k = win.next()
                for k in range(8):
                    for hf in range(2):
                        p.dma('pool', win_t[:, k, hf * 1792:(hf + 1) * 1792],
                              w_in[k * 128:(k + 1) * 128, hf * 1792:(hf + 1) * 1792], writes=[win_k], lane=(2 * k + hf) % 4)
                g1b = Tiles(p, ph, "g1b", [128, 1024], F32)
                g1_t, g1_k = g1b.next()
                p.dma('sp', g1_t[:], norm1_g.partition_broadcast(128), writes=[g1_k])
                xt = Tiles(p, ph, "xt", [128, 1024], F32, bufs=3)
                junk = Tiles(p, ph, "junkA", [128, 1024], BF16, bufs=2)
                ssA = Tiles(p, ph, "ssA", [128, 4], F32, bufs=4)
                xn = Tiles(p, ph, "xn", [128, 1024], BF16, bufs=2)
                xnT = Tiles(p, ph, "xnT", [128, 8, 512], BF16, bufs=2)
                tok = Tiles(p, ph, "tok", [128, 2560], F32, bufs=2)
                qkst = Tiles(p, ph, "qkst", [128, 8, 512], BF16, bufs=2)
                psT = Tiles(p, ph, "psT", [128, 8, 128], BF16, bufs=1, psum=True)
                psA = Tiles(p, ph, "psA", [128, 512], F32, bufs=6, psum=True)
                ev = 0
                def prep_a(gi, ctx):
                    xnT_t, xnT_k = xnT.next()
                    for ti in range(4):
                        r0 = gi * 512 + ti * 128
                        xt_t, xt_k = xt.next()
                        p.dma('sp', xt_t[:], x[r0:r0 + 128, :], writes=[xt_k])
                        j_t, j_k = junk.next()
                        ss_t, ss_k = ssA.next()
                        p.op('act', lambda e: e.activation(j_t[:], xt_t[:], AF.Square, accum_out=ss_t[:, 0:1]),
                             reads=[xt_k], writes=[j_k, ss_k])
                        p.op('dve', lambda e: e.tensor_scalar(ss_t[:, 1:2], ss_t[:, 0:1], 1.0 / 1024, 1e-6, ALU.mult, ALU.add),
                             reads=[ss_k], writes=[ss_k])
                        p.op('act', lambda e: e.activation(ss_t[:, 2:3], ss_t[:, 1:2], AF.Sqrt), reads=[ss_k], writes=[ss_k])
                        p.op('dve', lambda e: e.reciprocal(ss_t[:, 3:4], ss_t[:, 2:3]), reads=[ss_k], writes=[ss_k])
                        xn_t, xn_k = xn.next()
                        p.op('dve', lambda e: e.scalar_tensor_tensor(xn_t[:], xt_t[:], ss_t[:, 3:4], g1_t[:], ALU.mult, ALU.mult),
                             reads=[xt_k, ss_k, g1_k], writes=[xn_k])
                        yield
                        pst_t, pst_k = psT.next()

                        def tr(e):
                            for k in range(8):
                                ins = e.transpose(pst_t[:, k, :], xn_t[:, k * 128:(k + 1) * 128], idb_t[:])
                            return ins
                        p.op('pe', tr, reads=[xn_k, idb_k], writes=[pst_k])
                        p.op('act', lambda e: e.copy(xnT_t[:, :, ti * 128:(ti + 1) * 128], pst_t[:]),
                             reads=[pst_k], writes=[xnT_k])
                        yield
                    ctx["xnT"] = (xnT_t, xnT_k)
                    yield

                def adv_a(gen, n):
                    if gen is None:
                        return
                    for _ in range(n):
                        try:
                            next(gen)
                        except StopIteration:
                            return
                NGA = NG
                ctxa = [dict() for _ in range(NGA + 1)]
                gen_a = prep_a(0, ctxa[0])
                adv_a(gen_a, 100)
                for gi in range(NGA):
                    gen_a = prep_a(gi + 1, ctxa[gi + 1]) if gi + 1 < NGA else None
                    xnT_t, xnT_k = ctxa[gi]["xnT"]
                    for ti in range(4):
                        r0 = gi * 512 + ti * 128
                        tok_t, tok_k = tok.next()
                        for cg in range(5):
                            ps_t, ps_k = psA.next()

                            def mm(e):
                                for k in range(8):
                                    ins = e.matmul(ps_t[:], xnT_t[:, k, ti * 128:(ti + 1) * 128],
                                                   win_t[:, k, 1024 + cg * 512:1024 + (cg + 1) * 512],
                                                   start=(k == 0), stop=(k == 7))
                                return ins
                            p.op('pe', mm, reads=[xnT_k, win_k], writes=[ps_k])
                            ev += 1
                            if ev % 2:
                                p.op('act', lambda e: e.copy(tok_t[:, cg * 512:(cg + 1) * 512], ps_t[:]),
                                     reads=[ps_k], writes=[tok_k])
                            else:
                                p.op('dve', lambda e: e.tensor_copy(tok_t[:, cg * 512:(cg + 1) * 512], ps_t[:]),
                                     reads=[ps_k], writes=[tok_k])
                        p.dma('pool', projT[r0:r0 + 128, :], tok_t[:], reads=[tok_k], writes=['projT'], sem=tok_k, lane='st', nowaw=True)
                        adv_a(gen_a, 1)
                    qk_t, qk_k = qkst.next()
                    for i2 in range(8):
                        adv_a(gen_a, 1)
                        ps_t, ps_k = psA.next()

                        def mm2(e):
                            for k in range(8):
                                ins = e.matmul(ps_t[:], win_t[:, k, i2 * 128:(i2 + 1) * 128], xnT_t[:, k, :],
                                               start=(k == 0), stop=(k == 7))
                            return ins
                        p.op('pe', mm2, reads=[xnT_k, win_k], writes=[ps_k])
                        if i2 < 4:
                            p.op('act', lambda e: e.mul(qk_t[:, i2, :], ps_t[:], 0.125), reads=[ps_k], writes=[qk_k])
                        else:
                            p.op('act', lambda e: e.copy(qk_t[:, i2, :], ps_t[:]), reads=[ps_k], writes=[qk_k])
                    p.dma('pool', qkT[:, :, gi * 512:(gi + 1) * 512].rearrange("(i2 two) d t -> (two d) i2 t", two=2), qk_t[:],
                          reads=[qk_k], writes=['qkT'], sem=qk_k, lane='st', nowaw=True)
                    if gi == 1 and "0" in stages:
                        for blk in range(4):
                            rows = slice(blk * 4096, (blk + 1) * 4096)
                            p.dma('pool', uvtab[rows, 0:1024], peer_u[rows, :], writes=['uvtab'], sem=('uvt', blk), lane=0, nowaw=True)
                            p.dma('pool', uvtab[rows, 1024:2048], peer_v[rows, :], writes=['uvtab'], sem=('uvt', blk), lane=1, nowaw=True)
                    adv_a(gen_a, 100)

        p.barrier()
        if "B" in stages:
            with ExitStack() as ph:
                qTa = Tiles(p, ph, "qTa", [76, 2048], BF16, bufs=2)
                kTa = Tiles(p, ph, "kTa", [76, 2048], BF16, bufs=2)
                vaug = Tiles(p, ph, "vaug", [128, 16, 65], BF16, bufs=2)
                kmb = Tiles(p, ph, "kmb", [64, 8], BF16, bufs=2)
                kmf = Tiles(p, ph, "kmf", [64, 8], F32, bufs=2)
                kjunk = Tiles(p, ph, "kjunk", [64, 256], BF16, bufs=2)
                zer = Tiles(p, ph, "zer", [64, 256], BF16)
                zer_t, zer_k = zer.next()
                p.op('pool', lambda e: e.memset(zer_t[:], 0.0), writes=[zer_k])
                elig = Tiles(p, ph, "elig", [128, 3, 16, 8], F32)
                tri = Tiles(p, ph, "tri", [128, 128], BF16)
                gm = Tiles(p, ph, "gm", [128, 16, 8], F32, bufs=2)
                m8 = Tiles(p, ph, "m8", [128, 16, 8], F32, bufs=2)
                selt = Tiles(p, ph, "selt", [128, 16, 8], F32, bufs=2)
                biast = Tiles(p, ph, "biast", [128, 16, 8], BF16, bufs=2)
                bT = Tiles(p, ph, "bT", [8, 2048], BF16, bufs=2)
                pT = Tiles(p, ph, "pT", [128, 16, 512], BF16, bufs=2)
                osb = Tiles(p, ph, "osb", [128, 4, 64], BF16, bufs=3)
                rc = Tiles(p, ph, "rc", [128, 4], F32, bufs=3)
                psS = Tiles(p, ph, "psS", [128, 512], F32, bufs=3, psum=True)
                psO = Tiles(p, ph, "psO", [128, 512], F32, bufs=2, psum=True)
                psG = Tiles(p, ph, "psG", [128, 16, 8], F32, bufs=1, psum=True)
                psB = Tiles(p, ph, "psB", [128, 2048], BF16, bufs=1, psum=True)
                el_t, el_k = elig.next()
                p.dma('sp', el_t[:].rearrange("p a b c -> p (a b c)"), cst["c_elig"], writes=[el_k])
                tri_t, tri_k = tri.next()
                p.dma('pool', tri_t[:], cst["c_tri"], writes=[tri_k])
                for b in range(2):
                    va_t, va_k = vaug.next()
                    p.op('pool', lambda e: e.memset(va_t[:, :, 64:65], 1.0), writes=[va_k])
                def advance_b(gen, n):
                    if gen is None:
                        return
                    for _ in range(n):
                        try:
                            next(gen)
                        except StopIteration:
                            return

                def prologue(s, h, ctx):
                    t0 = s * 2048
                    if True:
                        q_t, q_k = qTa.next()
                        k_t, k_k = kTa.next()
                        va_t, va_k = vaug.next()
                        p.dma('sp', q_t[0:64, :], qkT[h, :, t0:t0 + 2048], reads=['qkT'], writes=[q_k])
                        p.dma('sp', k_t[0:64, :], qkT[8 + h, :, t0:t0 + 2048], reads=['qkT'], writes=[k_k])
                        p.dma('pool', k_t[64:76, :], cst["c_kaug"][h], writes=[k_k], lane=1)
                        p.dma('pool', q_t[72:76, :], cst["c_qaug"][h], writes=[q_k], lane=1)
                        p.dma('pool', va_t[:, :, 0:64],
                              projT[t0:t0 + 2048, h * 64:(h + 1) * 64].rearrange("(kt p) c -> p kt c", p=128),
                              reads=['projT'], writes=[va_k])
                        yield
                        km_t, km_k = kmb.next()
                        kf_t, kf_k = kmf.next()
                        for n in range(8):
                            kj_t, kj_k = kjunk.next()
                            p.op('dve', lambda e: e.scalar_tensor_tensor(kj_t[:], k_t[0:64, n * 256:(n + 1) * 256], 1.0 / 256, zer_t[:],
                                                                         ALU.mult, ALU.add, accum_out=kf_t[:, n:n + 1]),
                                 reads=[k_k, zer_k], writes=[(kf_k, n)])
                        p.op('dve', lambda e: e.tensor_copy(km_t[:], kf_t[:]), reads=[(kf_k, n_) for n_ in range(8)], writes=[km_k])
                        pg_t, pg_k = psG.next()

                        def gmm(e):
                            for qt in range(16):
                                ins = e.matmul(pg_t[:, qt, :], q_t[0:64, qt * 128:(qt + 1) * 128], km_t[:], start=True, stop=True)
                            return ins
                        p.op('pe', gmm, reads=[q_k, km_k], writes=[pg_k])
                        yield
                        gm_t, gm_k = gm.next()
                        m8_t, m8_k = m8.next()
                        se_t, se_k = selt.next()
                        bi_t, bi_k = biast.next()
                        p.op('dve', lambda e: e.tensor_tensor(gm_t[:], pg_t[:], el_t[:, 0], ALU.add), reads=[pg_k, el_k], writes=[gm_k])
                        for qt in range(16):
                            p.op('dve', lambda e: e.max(m8_t[:, qt, :], gm_t[:, qt, :]), reads=[gm_k], writes=[m8_k])
                        yield
                        p.op('dve', lambda e: e.tensor_tensor(se_t[:], gm_t[:], m8_t[:, :, 2:3].to_broadcast([128, 16, 8]), ALU.is_ge),
                             reads=[gm_k, m8_k], writes=[se_k])
                        p.op('dve', lambda e: e.tensor_tensor(se_t[:], se_t[:], el_t[:, 1], ALU.mult), reads=[se_k, el_k], writes=[se_k])
                        p.op('dve', lambda e: e.scalar_tensor_tensor(bi_t[:], se_t[:], -1.0, el_t[:, 2], ALU.add, ALU.add),
                             reads=[se_k, el_k], writes=[bi_k])
                        yield
                        pb_t, pb_k = psB.next()

                        def btr(e):
                            for qt in range(16):
                                ins = e.transpose(pb_t[0:8, qt * 128:(qt + 1) * 128], bi_t[:, qt, :], idb_t[:])
                            return ins
                        p.op('pe', btr, reads=[bi_k, idb_k], writes=[pb_k])
                        bT_t, bT_k = bT.next()
                        p.op('act', lambda e: e.copy(bT_t[:], pb_t[0:8, :]), reads=[pb_k], writes=[bT_k])
                        p.dma('sp', q_t[64:72, :], bT_t[:], reads=[bT_k], writes=[q_k], lane=2)
                        ctx.update(q=(q_t, q_k), k=(k_t, k_k), va=(va_t, va_k), t0=t0, h=h)
                    yield

                def attention(ctx, gen):
                    q_t, q_k = ctx["q"]; k_t, k_k = ctx["k"]; va_t, va_k = ctx["va"]; t0 = ctx["t0"]; h = ctx["h"]
                    if True:
                        for g in range(4):
                            pT_t, pT_k = pT.next()
                            for kt in range(4 * g + 4):
                                c0 = max(0, kt * 128 - g * 512)
                                ps_t, ps_k = psS.next()
                                diag = kt >= 4 * g

                                def smm(e):
                                    ins = e.matmul(ps_t[:, c0:512], k_t[0:76, kt * 128:(kt + 1) * 128],
                                                   q_t[0:76, g * 512 + c0:(g + 1) * 512], start=True, stop=not diag)
                                    if diag:
                                        ins = e.matmul(ps_t[:, c0:c0 + 128], idb_t[:], tri_t[:], start=False, stop=True)
                                    return ins
                                p.op('pe', smm, reads=[k_k, q_k, idb_k, tri_k], writes=[ps_k])
                                p.op('act', lambda e: e.activation(pT_t[:, kt, c0:512], ps_t[:, c0:512], AF.Exp),
                                     reads=[ps_k], writes=[pT_k])
                                if kt % 4 == 3:
                                    advance_b(gen, 1)
                            po_t, po_k = psO.next()
                            po_v = po_t[:, 0:260].rearrange("p (a b) -> p a b", b=65)

                            def pv(e):
                                for qs in range(4):
                                    qt = 4 * g + qs
                                    for kt in range(qt + 1):
                                        ins = e.matmul(po_v[:, qs, :], pT_t[:, kt, qs * 128:(qs + 1) * 128], va_t[:, kt, :],
                                                       start=(kt == 0), stop=(kt == qt))
                                return ins
                            p.op('pe', pv, reads=[pT_k, va_k], writes=[po_k])
                            rc_t, rc_k = rc.next()
                            os_t, os_k = osb.next()
                            p.op('dve', lambda e: e.reciprocal(rc_t[:], po_v[:, :, 64]), reads=[po_k], writes=[rc_k])
                            p.op('dve', lambda e: e.tensor_tensor(os_t[:], po_v[:, :, 0:64],
                                                                  rc_t[:].unsqueeze(2).to_broadcast([128, 4, 64]), ALU.mult),
                                 reads=[po_k, rc_k], writes=[os_k])
                            r0 = t0 + g * 512
                            p.dma('pool', mix[r0:r0 + 512, h * 64:(h + 1) * 64].rearrange("(qs p) c -> p qs c", p=128), os_t[:],
                                  reads=[os_k], writes=['mix'], sem=os_k, lane='st', nowaw=True)

                units = [(s_, h_) for s_ in range(nseq) for h_ in range(8)]
                ctxb = [dict() for _ in units]
                g0 = prologue(units[0][0], units[0][1], ctxb[0])
                advance_b(g0, 100)
                for ui in range(len(units)):
                    gen = prologue(units[ui + 1][0], units[ui + 1][1], ctxb[ui + 1]) if ui + 1 < len(units) else None
                    attention(ctxb[ui], gen)
                    advance_b(gen, 100)

        p.barrier()
        pre_d = None
        if "D" in stages:
            wo_pre = Tiles(p, st, "wo", [128, 8, 1024], BF16)
            wq_pre = Tiles(p, st, "wq", [128, 8, 2048], BF16)
            g2_pre = Tiles(p, st, "g2b", [128, 2, 1024], F32)
            io_pre = Tiles(p, st, "io16", [128, 16], F32)
            pre_d = dict(wo=wo_pre.next(), wq=wq_pre.next(), g2=g2_pre.next(), io=io_pre.next())
            for k in range(8):
                p.dma('pool', pre_d["wo"][0][:, k, :], w_out[k * 128:(k + 1) * 128, :], writes=[pre_d["wo"][1]], lane=k % 4)
                p.dma('pool', pre_d["wq"][0][:, k, :], peer_wq[k * 128:(k + 1) * 128, :], writes=[pre_d["wq"][1]], lane=k % 4)
            p.dma('sp', pre_d["g2"][0][:, 0, :], norm2_g.partition_broadcast(128), writes=[pre_d["g2"][1]])
            p.dma('sp', pre_d["g2"][0][:, 1, :], normf_g.partition_broadcast(128), writes=[pre_d["g2"][1]], lane=1)
            p.dma('sp', pre_d["io"][0][:], cst["c_iota16"], writes=[pre_d["io"][1]])
        if "C" in stages:
            with ExitStack() as ph:
                amat = Tiles(p, ph, "amat", [128, 128], F32)
                bsel = Tiles(p, ph, "bsel", [128, 4], F32)
                cmask = Tiles(p, ph, "cmask", [128, 128], I32)
                lbb = Tiles(p, ph, "lbb", [128, 4, 512], F32)
                am_t, am_k = amat.next()
                bs_t, bs_k = bsel.next()
                cm_t, cm_k = cmask.next()
                lb_t, lb_k = lbb.next()
                p.dma('sp', am_t[:], cst["c_amat"], writes=[am_k])
                p.dma('sp', bs_t[:], cst["c_bsel"], writes=[bs_k])
                p.dma('sp', cm_t[:], cst["c_cmask"], writes=[cm_k])
                p.dma('sp', lb_t[:, 0, :], rec_lb[0:1, :].partition_broadcast(128), writes=[lb_k])
                p.dma('sp', lb_t[:, 1, :], rec_lb[1:2, :].partition_broadcast(128), writes=[lb_k], lane=1)
                p.dma('sp', lb_t[:, 3, :], rec_norm_g.partition_broadcast(128), writes=[lb_k], lane=2)
                p.op('dve', lambda e: e.tensor_tensor(lb_t[:, 0, :], lb_t[:, 0, :], lb_t[:, 1, :], ALU.subtract), reads=[lb_k], writes=[lb_k])
                p.op('act', lambda e: e.activation(lb_t[:, 1, :], lb_t[:, 0, :], AF.Sigmoid), reads=[lb_k], writes=[lb_k])
                p.op('dve', lambda e: e.tensor_scalar(lb_t[:, 2, :], lb_t[:, 1, :], -1.0, 1.0, ALU.mult, ALU.add), reads=[lb_k], writes=[lb_k])
                rec = Tiles(p, ph, "rec", [128, 2048], F32, bufs=2)
                fT = Tiles(p, ph, "fT", [128, 512], F32, bufs=2)
                lf = Tiles(p, ph, "lf", [128, 512], F32, bufs=2)
                kkT = Tiles(p, ph, "kkT", [128, 512], F32, bufs=2)
                qfT = Tiles(p, ph, "qfT", [128, 512], F32, bufs=2)
                eD = Tiles(p, ph, "eD", [128, 2, 512], F32, bufs=2)
                qtl = Tiles(p, ph, "qtl", [128, 512], BF16, bufs=2)
                ktl = Tiles(p, ph, "ktl", [128, 512], BF16, bufs=2)
                vbf = Tiles(p, ph, "vbf", [128, 512], BF16, bufs=2)
                qtT = Tiles(p, ph, "qtT", [128, 4, 128], BF16, bufs=2)
                ktT = Tiles(p, ph, "ktT", [128, 4, 128], BF16, bufs=2)
                ecs = Tiles(p, ph, "ecs", [128, 4, 4], F32, bufs=2)
                dl = Tiles(p, ph, "dl", [128, 4, 2], F32, bufs=2)
                pcs = Tiles(p, ph, "pcs", [128, 4, 4], F32, bufs=2)
                edl = Tiles(p, ph, "edl", [128, 4, 2], F32, bufs=2)
                AT = Tiles(p, ph, "AT", [128, 4, 128], BF16, bufs=2)
                Sst = Tiles(p, ph, "Sst", [128, 4, 128], F32, bufs=nseq)
                Sb = Tiles(p, ph, "Sb", [128, 128], BF16, bufs=4)
                tmpS = Tiles(p, ph, "tmpS", [128, 128], F32, bufs=3)
                ssC = Tiles(p, ph, "ssC", [128, 4, 4], F32, bufs=2)
                junkC = Tiles(p, ph, "junkC", [128, 128], BF16, bufs=2)
                sg = Tiles(p, ph, "sg", [128, 512], F32, bufs=2)
                o1 = Tiles(p, ph, "o1", [128, 4, 128], F32, bufs=2)
                recb = Tiles(p, ph, "recb", [128, 512], BF16, bufs=2)
                psD = Tiles(p, ph, "psD", [128, 512], F32, bufs=2, psum=True)
                psOo = Tiles(p, ph, "psOo", [128, 4, 128], F32, bufs=2, psum=True)
                psTq = Tiles(p, ph, "psTq", [128, 4, 128], BF16, bufs=1, psum=True)
                psC = Tiles(p, ph, "psC", [128, 4, 4], F32, bufs=1, psum=True)
                psSc = Tiles(p, ph, "psSc", [128, 128], F32, bufs=1, psum=True)
                psKV = Tiles(p, ph, "psKV", [128, 128], F32, bufs=1, psum=True)
                for b in range(2):
                    at_t, at_k = AT.next()
                    p.op('pool', lambda e: e.memset(at_t[:], 0.0), writes=[at_k])
                for s in range(nseq):
                    S_t, S_k = Sst.next()
                    p.op('pool', lambda e: e.memset(S_t[:], 0.0), writes=[S_k])
                def tile_c(s, ti):
                    if True:
                        S_t, S_k = Sst.t[s], ("Sst", s)
                        r0 = s * 2048 + ti * 128
                        rec_t, rec_k = rec.next()
                        p.dma('sp', rec_t[:], projT[r0:r0 + 128, 512:2560], reads=['projT'], writes=[rec_k])
                        f_t, f_k = fT.next()
                        lf_t, lf_k = lf.next()
                        kk_t, kk_k = kkT.next()
                        qf_t, qf_k = qfT.next()
                        p.op('act', lambda e: e.activation(f_t[:], rec_t[:, 512:1024], AF.Sigmoid), reads=[rec_k], writes=[f_k])
                        yield
                        p.op('act', lambda e: e.activation(qf_t[:], rec_t[:, 0:512], AF.Silu), reads=[rec_k], writes=[qf_k])
                        sg_t, sg_k = sg.next()
                        p.op('act', lambda e: e.activation(sg_t[:], rec_t[:, 1536:2048], AF.Silu), reads=[rec_k], writes=[sg_k])
                        p.op('dve', lambda e: e.tensor_tensor(f_t[:], f_t[:], lb_t[:, 2, :], ALU.mult), reads=[f_k, lb_k], writes=[f_k])
                        p.op('dve', lambda e: e.tensor_tensor(f_t[:], f_t[:], lb_t[:, 1, :], ALU.add), reads=[f_k, lb_k], writes=[f_k])
                        yield
                        p.op('act', lambda e: e.activation(lf_t[:], f_t[:], AF.Ln), reads=[f_k], writes=[lf_k])
                        p.op('dve', lambda e: e.tensor_scalar(kk_t[:], f_t[:], -1.0, 1.0, ALU.mult, ALU.add), reads=[f_k], writes=[kk_k])
                        yield
                        pd_t, pd_k = psD.next()
                        p.op('pe', lambda e: e.matmul(pd_t[:], am_t[:], lf_t[:], start=True, stop=True), reads=[am_k, lf_k], writes=[pd_k])
                        pc_t, pc_k = psC.next()

                        def cmm(e):
                            for h in range(4):
                                ins = e.matmul(pc_t[:, h, :], lf_t[:, h * 128:(h + 1) * 128], bs_t[:], start=True, stop=True)
                            return ins
                        p.op('pe', cmm, reads=[lf_k, bs_k], writes=[pc_k])
                        pcs_t, pcs_k = pcs.next()
                        p.op('act', lambda e: e.copy(pcs_t[:], pc_t[:]), reads=[pc_k], writes=[pcs_k])
                        yield
                        eD_t, eD_k = eD.next()
                        p.op('act', lambda e: e.activation(eD_t[:, 0, :], pd_t[:], AF.Exp), reads=[pd_k], writes=[eD_k])
                        p.op('act', lambda e: e.activation(eD_t[:, 1, :], pd_t[:], AF.Exp, scale=-1.0), reads=[pd_k], writes=[eD_k])
                        ec_t, ec_k = ecs.next()
                        dl_t, dl_k = dl.next()
                        ed_t, ed_k = edl.next()
                        pc_v = pcs_t[:].rearrange("p h (c two) -> p h c two", two=2)
                        p.op('dve', lambda e: e.tensor_tensor(dl_t[:], pc_v[:, :, :, 1], pc_v[:, :, :, 0], ALU.subtract), reads=[pcs_k], writes=[dl_k])
                        p.op('act', lambda e: e.activation(ec_t[:], pcs_t[:], AF.Exp), reads=[pcs_k], writes=[ec_k])
                        p.op('act', lambda e: e.activation(ed_t[:], dl_t[:], AF.Exp), reads=[dl_k], writes=[ed_k])
                        yield
                        qt_t, qt_k = qtl.next()
                        kt_t, kt_k = ktl.next()
                        vb_t, vb_k = vbf.next()
                        p.op('dve', lambda e: e.tensor_tensor(qt_t[:], qf_t[:], eD_t[:, 0, :], ALU.mult), reads=[qf_k, eD_k], writes=[qt_k])
                        p.op('dve', lambda e: e.tensor_tensor(kt_t[:], kk_t[:], eD_t[:, 1, :], ALU.mult), reads=[kk_k, eD_k], writes=[kt_k])
                        p.op('act', lambda e: e.copy(vb_t[:], rec_t[:, 1024:1536]), reads=[rec_k], writes=[vb_k])
                        yield
                        pq_t, pq_k = psTq.next()

                        def trq(e):
                            for h in range(4):
                                ins = e.transpose(pq_t[:, h, :], qt_t[:, h * 128:(h + 1) * 128], idb_t[:])
                            return ins
                        p.op('pe', trq, reads=[qt_k, idb_k], writes=[pq_k])
                        qT_t, qT_k = qtT.next()
                        p.op('act', lambda e: e.copy(qT_t[:], pq_t[:]), reads=[pq_k], writes=[qT_k])
                        yield
                        pk_t, pk_k = psTq.next()

                        def trk(e):
                            for h in range(4):
                                ins = e.transpose(pk_t[:, h, :], kt_t[:, h * 128:(h + 1) * 128], idb_t[:])
                            return ins
                        p.op('pe', trk, reads=[kt_k, idb_k], writes=[pk_k])
                        kT_t, kT_k = ktT.next()
                        p.op('dve', lambda e: e.tensor_copy(kT_t[:], pk_t[:]), reads=[pk_k], writes=[kT_k])
                        yield
                        at_t, at_k = AT.next()
                        po_t, po_k = psOo.next()
                        for h in range(4):
                            yield
                            hs = slice(h * 128, (h + 1) * 128)
                            psc_t, psc_k = psSc.next()
                            p.op('pe', lambda e: e.matmul(psc_t[:], kT_t[:, h, :], qT_t[:, h, :], start=True, stop=True),
                                 reads=[kT_k, qT_k], writes=[psc_k])
                            p.op('dve', lambda e: e.copy_predicated(at_t[:, h, :], cm_t[:], psc_t[:]), reads=[psc_k, cm_k], writes=[at_k])
                            sbs = []
                            for c in range(2):
                                yield
                                sb_t, sb_k = Sb.next()
                                p.op('dve', lambda e: e.tensor_scalar(sb_t[:], S_t[:, h, :], ec_t[:, h, 2 * c:2 * c + 1], None, ALU.mult),
                                     reads=[S_k, ec_k], writes=[sb_k])
                                sbs.append((sb_t, sb_k))
                                pkv_t, pkv_k = psKV.next()
                                cs = slice(c * 64, (c + 1) * 64)
                                p.op('pe', lambda e: e.matmul(pkv_t[:], kt_t[cs, hs], vb_t[cs, hs], start=True, stop=True),
                                     reads=[kt_k, vb_k], writes=[pkv_k])
                                tm_t, tm_k = tmpS.next()
                                p.op('dve', lambda e: e.tensor_scalar(tm_t[:], pkv_t[:], ed_t[:, h, c:c + 1], None, ALU.mult),
                                     reads=[pkv_k, ed_k], writes=[tm_k])
                                p.op('dve', lambda e: e.scalar_tensor_tensor(S_t[:, h, :], S_t[:, h, :], ec_t[:, h, 2 * c + 1:2 * c + 2],
                                                                             tm_t[:], ALU.mult, ALU.add),
                                     reads=[S_k, ec_k, tm_k], writes=[S_k])

                            def omm(e):
                                e.matmul(po_t[:, h, :], at_t[:, h, :], vb_t[:, hs], start=True, stop=False)
                                e.matmul(po_t[0:64, h, :], qT_t[:, h, 0:64], sbs[0][0][:], start=False, stop=True)
                                ins = e.matmul(po_t[64:128, h, :], qT_t[:, h, 64:128], sbs[1][0][:], start=False, stop=True)
                                return ins
                            p.op('pe', omm, reads=[at_k, vb_k, qT_k, sbs[0][1], sbs[1][1]], writes=[po_k])
                        yield
                        ss_t, ss_k = ssC.next()
                        for h in range(4):
                            j_t, j_k = junkC.next()
                            p.op('act', lambda e: e.activation(j_t[:], po_t[:, h, :], AF.Square, accum_out=ss_t[:, 0, h:h + 1]),
                                 reads=[po_k], writes=[j_k, ss_k])
                        p.op('dve', lambda e: e.tensor_scalar(ss_t[:, 1, :], ss_t[:, 0, :], 1.0 / 128, 1e-6, ALU.mult, ALU.add), reads=[ss_k], writes=[ss_k])
                        p.op('act', lambda e: e.activation(ss_t[:, 2, :], ss_t[:, 1, :], AF.Sqrt), reads=[ss_k], writes=[ss_k])
                        p.op('dve', lambda e: e.reciprocal(ss_t[:, 3, :], ss_t[:, 2, :]), reads=[ss_k], writes=[ss_k])
                        yield
                        p.op('dve', lambda e: e.tensor_tensor(sg_t[:], sg_t[:], lb_t[:, 3, :], ALU.mult), reads=[sg_k, lb_k], writes=[sg_k])
                        yield
                        o1_t, o1_k = o1.next()
                        p.op('dve', lambda e: e.tensor_tensor(o1_t[:], po_t[:], ss_t[:, 3, :].unsqueeze(2).to_broadcast([128, 4, 128]), ALU.mult),
                             reads=[po_k, ss_k], writes=[o1_k])
                        rb_t, rb_k = recb.next()
                        p.op('dve', lambda e: e.tensor_tensor(rb_t[:], o1_t[:].rearrange("p a b -> p (a b)"), sg_t[:], ALU.mult),
                             reads=[o1_k, sg_k], writes=[rb_k])
                        p.dma('pool', mix[r0:r0 + 128, 512:1024], rb_t[:], reads=[rb_k], writes=['mix'], sem=rb_k, lane='st', nowaw=True)

                def run_lockstep(gens):
                    live = list(gens)
                    while live:
                        nxt = []
                        for g_ in live:
                            try:
                                next(g_)
                                nxt.append(g_)
                            except StopIteration:
                                pass
                        live = nxt
                for ti in range(16):
                    for s0 in range(0, nseq, 2):
                        run_lockstep([tile_c(s_, ti) for s_ in range(s0, min(s0 + 2, nseq))])

        p.barrier()
        if "D" in stages:
            build_phase_d(nc, p, st, locals())
        p.barrier()
        p.fence('sp', reads=['out', 'mix', 'projT', 'qkT'])
        print("instructions:", p.ninst, "semaphores:", len(p.sems))
    return nc


def build_phase_d(nc, p, st, env):
    x = env["x"]; out = env["out"]; mix = env["mix"]; uvtab = env["uvtab"]; cst = env["cst"]
    w_out = env["w_out"]; norm2_g = env["norm2_g"]; normf_g = env["normf_g"]
    peer_wq = env["peer_wq"]; peer_sk = env["peer_sk"]
    idb_t, idb_k = env["idb_t"], env["idb_k"]
    NT = env["NT"]
    SG = 4
    NBUF = 5
    with ExitStack() as ph:
        skT = Tiles(p, ph, "skT", [128, 16, 128], BF16)
        skT_t, skT_k = skT.next()
        pre = env["pre_d"]
        wo_t, wo_k = pre["wo"]
        wq_t, wq_k = pre["wq"]
        g2_t, g2_k = pre["g2"]
        io_t, io_k = pre["io"]
        psT = Tiles(p, ph, "psTd", [128, 8, 128], BF16, bufs=1, psum=True)
        psY = Tiles(p, ph, "psY", [128, 512], F32, bufs=1, psum=True)
        psQ = Tiles(p, ph, "psQ", [128, 512], F32, bufs=2, psum=True)
        psP = Tiles(p, ph, "psP", [128, 1024], F32, bufs=2, psum=True)
        scr = Tiles(p, ph, "scr", [128, 2048], F32, bufs=1)
        scr_t, scr_k = scr.next()
        skn_v = scr_t[:, 0:1024].bitcast(BF16).rearrange("p (g n) -> p g n", n=128)
        p.dma('pool', skn_v, peer_sk.rearrange("g n d -> n g d"), writes=[scr_k])
        for half in range(2):
            pt_t, pt_k = psT.next()

            def trs(e):
                for j in range(8):
                    ins = e.transpose(pt_t[:, j, :], skn_v[:, half * 8 + j, :], idb_t[:])
                return ins
            p.op('pe', trs, reads=[scr_k, idb_k], writes=[pt_k])
            p.op('act', lambda e: e.copy(skT_t[:, half * 8:(half + 1) * 8, :], pt_t[:]), reads=[pt_k], writes=[skT_k])

        mx = Tiles(p, ph, "mx", [128, 1024], BF16, bufs=2)
        mxT = Tiles(p, ph, "mxT", [128, 8, 128], BF16, bufs=1)
        xt = Tiles(p, ph, "xtd", [128, 1024], F32, bufs=3)
        junk = Tiles(p, ph, "junkD", [128, 1024], BF16, bufs=2)
        junk2 = Tiles(p, ph, "junkD2", [128, 1024], BF16, bufs=1)
        ssD = Tiles(p, ph, "ssD", [128, 8], F32, bufs=3)
        mhalf = Tiles(p, ph, "mhalf", [128, 1], F32)
        mh_t, mh_k = mhalf.next()
        p.op('pool', lambda e: e.memset(mh_t[:], -0.5), writes=[mh_k])
        hnb = Tiles(p, ph, "hnb", [128, 1024], BF16, bufs=2)
        hnT = Tiles(p, ph, "hnT", [128, 8, 128], BF16, bufs=1)
        qpT = Tiles(p, ph, "qpT", [128, 16, 128], BF16, bufs=1)
        ss2 = Tiles(p, ph, "ss2", [128, 128], F32, bufs=4)
        v12 = Tiles(p, ph, "v12", [128, 16, 16], F32, bufs=1)
        i12 = Tiles(p, ph, "i12", [128, 16, 16], U32, bufs=1)
        i12f = Tiles(p, ph, "i12f", [128, 16, 16], F32, bufs=1)
        cand2 = Tiles(p, ph, "cand2", [128, 256], F32, bufs=4)
        sc = Tiles(p, ph, "sc", [128, 8, 16], F32, bufs=1)
        pidx = Tiles(p, ph, "pidx", [128, 8, 16], U32, bufs=1)
        pij = Tiles(p, ph, "pij", [128, 2, 128], U32, bufs=1)
        pijf = Tiles(p, ph, "pijf", [128, 2, 128], F32, bufs=1)
        abf = Tiles(p, ph, "abf", [128, 2, 128], F32, bufs=1)
        eidf = Tiles(p, ph, "eidf", [128, 128], F32, bufs=1)
        eid = Tiles(p, ph, "eid", [128, 128], U32, bufs=2)
        gts = Tiles(p, ph, "gts", [128, 8, 16], F32, bufs=2)
        zz = Tiles(p, ph, "zz", [128, 2, 8], F32, bufs=1)
        uvb = Tiles(p, ph, "uvb", [128, SG, 2048], BF16, bufs=NBUF)
        apre = Tiles(p, ph, "apre", [128, SG], F32, bufs=NBUF)
        wgt = Tiles(p, ph, "wgt", [128, SG], F32, bufs=NBUF)
        dg = Tiles(p, ph, "dg", [128, 128], BF16, bufs=6)
        ot = Tiles(p, ph, "ot", [128, 1024], F32, bufs=1)
        OFFLOAD = False
        prod = Tiles(p, ph, "prod", [128, 1024], F32, bufs=1) if OFFLOAD else None

        def stage1(ti, ctx):
            r0 = ti * 128
            mx_t, mx_k = mx.next()
            p.dma('sp', mx_t[:], mix[r0:r0 + 128, :], reads=['mix'], writes=[mx_k])
            xt_t, xt_k = xt.next()
            p.dma('sp', xt_t[:], x[r0:r0 + 128, :], writes=[xt_k])
            pt_t, pt_k = psT.next()

            def tr1(e):
                for k in range(8):
                    ins = e.transpose(pt_t[:, k, :], mx_t[:, k * 128:(k + 1) * 128], idb_t[:])
                return ins
            p.op('pe', tr1, reads=[mx_k, idb_k], writes=[pt_k])
            mxT_t, mxT_k = mxT.next()
            p.op('act', lambda e: e.copy(mxT_t[:], pt_t[:]), reads=[pt_k], writes=[mxT_k])
            yield
            h_t, h_k = xt_t, xt_k
            for hf in range(2):
                py_t, py_k = psY.next()

                def mmy(e):
                    for k in range(8):
                        ins = e.matmul(py_t[:], mxT_t[:, k, :], wo_t[:, k, hf * 512:(hf + 1) * 512], start=(k == 0), stop=(k == 7))
                    return ins
                p.op('pe', mmy, reads=[mxT_k, wo_k], writes=[py_k])
                yield
                p.op('dve', lambda e: e.tensor_tensor(h_t[:, hf * 512:(hf + 1) * 512], py_t[:], xt_t[:, hf * 512:(hf + 1) * 512], ALU.add),
                     reads=[py_k, xt_k], writes=[h_k])
            j_t, j_k = junk2.next()
            ss_t, ss_k = ssD.next()
            yield
            p.op('act', lambda e: e.activation(j_t[:], h_t[:], AF.Square, accum_out=ss_t[:, 0:1]), reads=[h_k], writes=[j_k, ss_k])
            yield
            p.op('dve', lambda e: e.tensor_scalar(ss_t[:, 1:2], ss_t[:, 0:1], 1.0 / 1024, 1e-6, ALU.mult, ALU.add), reads=[ss_k], writes=[ss_k])
            p.op('act', lambda e: e.activation(ss_t[:, 2:3], ss_t[:, 1:2], AF.Sqrt), reads=[ss_k], writes=[ss_k])
            yield
            p.op('dve', lambda e: e.reciprocal(ss_t[:, 3:4], ss_t[:, 2:3]), reads=[ss_k], writes=[ss_k])
            hn_t, hn_k = hnb.next()
            p.op('dve', lambda e: e.scalar_tensor_tensor(hn_t[:], h_t[:], ss_t[:, 3:4], g2_t[:, 0, :], ALU.mult, ALU.mult),
                 reads=[h_k, ss_k, g2_k], writes=[hn_k])
            yield
            pt_t, pt_k = psT.next()

            def tr2(e):
                for k in range(8):
                    ins = e.transpose(pt_t[:, k, :], hn_t[:, k * 128:(k + 1) * 128], idb_t[:])
                return ins
            p.op('pe', tr2, reads=[hn_k, idb_k], writes=[pt_k])
            hnT_t, hnT_k = hnT.next()
            p.op('act', lambda e: e.copy(hnT_t[:], pt_t[:]), reads=[pt_k], writes=[hnT_k])
            yield
            qp_t, qp_k = qpT.next()
            for gq in range(4):
                pq_t, pq_k = psQ.next()

                def mmq(e):
                    for gg in range(4):
                        g = gq * 4 + gg
                        for k in range(8):
                            ins = e.matmul(pq_t[:, gg * 128:(gg + 1) * 128], wq_t[:, k, g * 128:(g + 1) * 128], hnT_t[:, k, :],
                                           start=(k == 0), stop=(k == 7))
                    return ins
                p.op('pe', mmq, reads=[wq_k, hnT_k], writes=[pq_k])
                p.op('act', lambda e: e.copy(qp_t[:, gq * 4:(gq + 1) * 4, :], pq_t[:].rearrange("p (a b) -> p a b", b=128)),
                     reads=[pq_k], writes=[qp_k])
            yield
            s_v = scr_t[:].rearrange("p (g n) -> p g n", n=128)
            for gq in range(4):
                pq_t, pq_k = psQ.next()

                def mms(e):
                    for gg in range(4):
                        g = gq * 4 + gg
                        ins = e.matmul(pq_t[:, gg * 128:(gg + 1) * 128], qp_t[:, g, :], skT_t[:, g, :], start=True, stop=True)
                    return ins
                p.op('pe', mms, reads=[qp_k, skT_k], writes=[pq_k])
                p.op('act', lambda e: e.copy(s_v[:, gq * 4:(gq + 1) * 4, :], pq_t[:].rearrange("p (a b) -> p a b", b=128)),
                     reads=[pq_k], writes=[scr_k])
            yield
            yield
            v_t, v_k = v12.next()
            i_t, i_k = i12.next()
            for g0 in range(0, 16, 4):
                gs = [g0, g0 + 1, g0 + 2, g0 + 3]
                s2s = [ss2.next() for _ in gs]
                for g in gs:
                    p.op('dve', lambda e: e.max(v_t[:, g, 0:8], s_v[:, g, :]), reads=[scr_k], writes=[(v_k, g)])
                for g in gs:
                    p.op('dve', lambda e: e.max_index(i_t[:, g, 0:8], v_t[:, g, 0:8], s_v[:, g, :]), reads=[scr_k, (v_k, g)], writes=[(i_k, g)])
                for g, (s2_t, s2_k) in zip(gs, s2s):
                    p.op('dve', lambda e: e.match_replace(s2_t[:], v_t[:, g, 0:8], s_v[:, g, :], NEG), reads=[scr_k, (v_k, g)], writes=[s2_k])
                for g, (s2_t, s2_k) in zip(gs, s2s):
                    p.op('dve', lambda e: e.max(v_t[:, g, 8:16], s2_t[:]), reads=[s2_k], writes=[(v_k, g, 1)])
                for g, (s2_t, s2_k) in zip(gs, s2s):
                    p.op('dve', lambda e: e.max_index(i_t[:, g, 8:16], v_t[:, g, 8:16], s2_t[:]), reads=[s2_k, (v_k, g, 1)], writes=[(i_k, g, 1)])
                yield
            vall = [(v_k, g) for g in range(16)] + [(v_k, g, 1) for g in range(16)]
            iall = [(i_k, g) for g in range(16)] + [(i_k, g, 1) for g in range(16)]
            if_t, if_k = i12f.next()
            p.op('dve', lambda e: e.tensor_copy(if_t[:], i_t[:]), reads=iall, writes=[if_k])
            ca_v = scr_t[:].rearrange("p (h c) -> p h c", c=256)
            v_v = v_t[:].rearrange("p (h c) k -> p h c k", c=2)
            p.op('dve', lambda e: e.tensor_tensor(ca_v.rearrange("p h (i j) -> p h i j", j=16),
                                                  v_v[:, :, 0, :].unsqueeze(3).to_broadcast([128, 8, 16, 16]),
                                                  v_v[:, :, 1, :].unsqueeze(2).to_broadcast([128, 8, 16, 16]), ALU.add),
                 reads=vall, writes=[scr_k])
            yield
            sc_t, sc_k = sc.next()
            pi_t, pi_k = pidx.next()
            for h0 in range(0, 8, 4):
                hs = [h0, h0 + 1, h0 + 2, h0 + 3]
                c2s = [cand2.next() for _ in hs]
                for h in hs:
                    p.op('dve', lambda e: e.max(sc_t[:, h, 0:8], ca_v[:, h, :]), reads=[scr_k], writes=[(sc_k, h)])
                for h in hs:
                    p.op('dve', lambda e: e.max_index(pi_t[:, h, 0:8], sc_t[:, h, 0:8], ca_v[:, h, :]), reads=[scr_k, (sc_k, h)], writes=[(pi_k, h)])
                for h, (c2_t, c2_k) in zip(hs, c2s):
                    p.op('dve', lambda e: e.match_replace(c2_t[:], sc_t[:, h, 0:8], ca_v[:, h, :], NEG), reads=[scr_k, (sc_k, h)], writes=[c2_k])
                for h, (c2_t, c2_k) in zip(hs, c2s):
                    p.op('dve', lambda e: e.max(sc_t[:, h, 8:16], c2_t[:]), reads=[c2_k], writes=[(sc_k, h, 1)])
                for h, (c2_t, c2_k) in zip(hs, c2s):
                    p.op('dve', lambda e: e.max_index(pi_t[:, h, 8:16], sc_t[:, h, 8:16], c2_t[:]), reads=[c2_k, (sc_k, h, 1)], writes=[(pi_k, h, 1)])
                yield
            scall = [(sc_k, h) for h in range(8)] + [(sc_k, h, 1) for h in range(8)]
            piall = [(pi_k, h) for h in range(8)] + [(pi_k, h, 1) for h in range(8)]
            g_t, g_k = gts.next()
            z_t, z_k = zz.next()
            p.op('dve', lambda e: e.tensor_tensor(g_t[:], sc_t[:], sc_t[:, :, 0:1].to_broadcast([128, 8, 16]), ALU.subtract),
                 reads=scall, writes=[g_k])
            yield
            for h in range(8):
                p.op('act', lambda e: e.activation(g_t[:, h, :], g_t[:, h, :], AF.Exp, accum_out=z_t[:, 0, h:h + 1]),
                     reads=[g_k], writes=[g_k, z_k])
            yield
            GATES_TAIL = True
            pij_t, pij_k = pij.next()
            pf_t, pf_k = pijf.next()
            pi_flat = pi_t[:].rearrange("p h k -> p (h k)")
            p.op('dve', lambda e: e.tensor_single_scalar(pij_t[:, 0, :], pi_flat, 4, ALU.logical_shift_right), reads=piall, writes=[pij_k])
            p.op('dve', lambda e: e.tensor_single_scalar(pij_t[:, 1, :], pi_flat, 15, ALU.bitwise_and), reads=piall, writes=[pij_k])
            p.op('dve', lambda e: e.tensor_copy(pf_t[:], pij_t[:]), reads=[pij_k], writes=[pf_k])
            ab_t, ab_k = abf.next()
            if_v = if_t[:].rearrange("p (h c) k -> p h c k", c=2)
            oh_v3 = scr_t[:].rearrange("p (s i) -> p s i", i=16)
            for c in range(2):
                p.op('dve', lambda e: e.tensor_tensor(oh_v3, io_t[:].unsqueeze(1).to_broadcast([128, 128, 16]),
                                                      pf_t[:, c, :].unsqueeze(2).to_broadcast([128, 128, 16]), ALU.is_equal),
                     reads=[io_k, pf_k], writes=[scr_k])
                oh_v = oh_v3.rearrange("p (h k) i -> p h k i", h=8)
                p.op('dve', lambda e: e.tensor_tensor(oh_v, oh_v, if_v[:, :, c, :].unsqueeze(2).to_broadcast([128, 8, 16, 16]), ALU.mult),
                     reads=[scr_k, if_k], writes=[scr_k])
                yield
                p.op('dve', lambda e: e.tensor_tensor(oh_v3[:, :, 0:8], oh_v3[:, :, 0:8], oh_v3[:, :, 8:16], ALU.add), reads=[scr_k], writes=[scr_k])
                p.op('dve', lambda e: e.tensor_tensor(oh_v3[:, :, 0:4], oh_v3[:, :, 0:4], oh_v3[:, :, 4:8], ALU.add), reads=[scr_k], writes=[scr_k])
                p.op('dve', lambda e: e.tensor_tensor(oh_v3[:, :, 0:2], oh_v3[:, :, 0:2], oh_v3[:, :, 2:4], ALU.add), reads=[scr_k], writes=[scr_k])
                p.op('dve', lambda e: e.tensor_tensor(ab_t[:, c, :], oh_v3[:, :, 0], oh_v3[:, :, 1], ALU.add), reads=[scr_k], writes=[ab_k])
                yield
            p.op('dve', lambda e: e.reciprocal(z_t[:, 1, :], z_t[:, 0, :]), reads=[z_k], writes=[z_k])
            p.op('dve', lambda e: e.tensor_tensor(g_t[:], g_t[:], z_t[:, 1, :].unsqueeze(2).to_broadcast([128, 8, 16]), ALU.mult),
                 reads=[g_k, z_k], writes=[g_k])
            ef_t, ef_k = eidf.next()
            p.op('dve', lambda e: e.scalar_tensor_tensor(ef_t[:], ab_t[:, 0, :], 128.0, ab_t[:, 1, :], ALU.mult, ALU.add),
                 reads=[ab_k], writes=[ef_k])
            ei_t, ei_k = eid.next()
            p.op('dve', lambda e: e.tensor_copy(ei_t[:], ef_t[:]), reads=[ef_k], writes=[ei_k])
            ctx.update(h=(h_t, h_k), hn=(hn_t, hn_k), ss=(ss_t, ss_k), ei=(ei_t, ei_k), g=(g_t, g_k), r0=r0)

        def advance(gen, n):
            if gen is None:
                return
            for _ in range(n):
                try:
                    next(gen)
                except StopIteration:
                    return

        def stage2(ctx, gen, prev_epi):
            h_t, h_k = ctx["h"]; hn_t, hn_k = ctx["hn"]; ss_t, ss_k = ctx["ss"]
            ei_t, ei_k = ctx["ei"]; g_t, g_k = ctx["g"]; r0 = ctx["r0"]
            pp_t, pp_k = psP.next()
            g_flat = g_t[:].rearrange("p h k -> p (h k)")
            NGRP = 128 // SG
            issued = {}

            def issue(grp):
                uv_t, uv_k = uvb.next()
                for sl in range(SG):
                    slot = grp * SG + sl
                    p.dma('pool', None, None, reads=[ei_k, 'uvtab'], writes=[uv_k], lane=sl,
                          fn=lambda e: e.indirect_dma_start(out=uv_t[:, sl, :], out_offset=None, in_=uvtab,
                                                            in_offset=bass.IndirectOffsetOnAxis(ap=ei_t[:, slot:slot + 1], axis=0)))
                issued[grp] = (uv_t, uv_k)
            PD = NBUF - 2
            for grp in range(min(PD, NGRP)):
                issue(grp)

            def post_b(grp, uv_t, uv_k, w_t, w_k):
                p.op('dve', lambda e: e.tensor_tensor(w_t[:], w_t[:], g_flat[:, grp * SG:(grp + 1) * SG], ALU.mult), reads=[w_k, g_k], writes=[w_k])
                for sl in range(SG):
                    slot = grp * SG + sl
                    dg_t, dg_k = dg.next()
                    p.op('act', lambda e: e.activation(dg_t[:], idb_t[:], AF.Copy, scale=w_t[:, sl:sl + 1]), reads=[idb_k, w_k], writes=[dg_k])

                    def mmv(e):
                        e.matmul(pp_t[:, 0:512], dg_t[:], uv_t[:, sl, 1024:1536], start=(slot == 0), stop=(slot == 127))
                        return e.matmul(pp_t[:, 512:1024], dg_t[:], uv_t[:, sl, 1536:2048], start=(slot == 0), stop=(slot == 127))
                    p.op('pe', mmv, reads=[dg_k, uv_k], writes=[pp_k])
            pend = None
            for grp in range(NGRP):
                if grp + PD < NGRP:
                    issue(grp + PD)
                uv_t, uv_k = issued.pop(grp)
                ap_t, ap_k = apre.next()
                w_t, w_k = wgt.next()
                for sl in range(SG):
                    if sl == SG - 1 and OFFLOAD:
                        pr_t, pr_k = prod.next()
                        p.op('pool', lambda e: e.tensor_tensor(pr_t[:], uv_t[:, sl, 0:1024], hn_t[:], ALU.mult), reads=[uv_k, hn_k], writes=[pr_k])
                        jj_t, jj_k = junk2.next()
                        p.op('act', lambda e: e.activation(jj_t[:], pr_t[:], AF.Copy, accum_out=ap_t[:, sl:sl + 1]),
                             reads=[pr_k], writes=[jj_k, (ap_k, 1)])
                        continue
                    jj_t, jj_k = junk.next()
                    p.op('dve', lambda e: e.scalar_tensor_tensor(jj_t[:], uv_t[:, sl, 0:1024], 1.0, hn_t[:], ALU.mult, ALU.mult,
                                                                 accum_out=ap_t[:, sl:sl + 1]),
                         reads=[uv_k, hn_k], writes=[(ap_k, 'c', sl)])
                p.op('act', lambda e: e.activation(w_t[:], ap_t[:], AF.Gelu), reads=[(ap_k, 'c', sl_) for sl_ in range(SG)], writes=[w_k])
                if pend is not None:
                    post_b(*pend)
                pend = (grp, uv_t, uv_k, w_t, w_k)
                if grp == 3 and prev_epi is not None:
                    prev_epi()
                advance(gen, 1)
            post_b(*pend)

            def epilogue():
                p.op('dve', lambda e: e.tensor_tensor(h_t[:], pp_t[:], h_t[:], ALU.add), reads=[pp_k, h_k], writes=[h_k])
                j_t, j_k = junk2.next()
                p.op('act', lambda e: e.activation(j_t[:], h_t[:], AF.Square, accum_out=ss_t[:, 4:5]), reads=[h_k], writes=[j_k, ss_k])
                p.op('dve', lambda e: e.tensor_scalar(ss_t[:, 5:6], ss_t[:, 4:5], 1.0 / 1024, 1e-6, ALU.mult, ALU.add), reads=[ss_k], writes=[ss_k])
                p.op('act', lambda e: e.activation(ss_t[:, 6:7], ss_t[:, 5:6], AF.Sqrt), reads=[ss_k], writes=[ss_k])
                p.op('dve', lambda e: e.reciprocal(ss_t[:, 7:8], ss_t[:, 6:7]), reads=[ss_k], writes=[ss_k])
                o_t, o_k = ot.next()
                p.op('dve', lambda e: e.scalar_tensor_tensor(o_t[:], h_t[:], ss_t[:, 7:8], g2_t[:, 1, :], ALU.mult, ALU.mult),
                     reads=[h_k, ss_k, g2_k], writes=[o_k])
                p.dma('sp', out[r0:r0 + 128, :], o_t[:], reads=[o_k], writes=['out'], sem=o_k, lane='st', nowaw=True)
            return epilogue

        ctxs = [dict() for _ in range(NT)]
        g0 = stage1(0, ctxs[0])
        advance(g0, 10 ** 6)
        epi = None
        for ti in range(NT):
            gen = stage1(ti + 1, ctxs[ti + 1]) if ti + 1 < NT else None
            epi = stage2(ctxs[ti], gen, epi)
            advance(gen, 10 ** 6)
        epi()


_CONSTS = None


def kernel(x, norm1_g, w_in, rec_lb_logits, rec_norm_g, w_out, norm2_g, peer_wq, peer_subkeys, peer_u, peer_v, normf_g):
    global _CONSTS
    if _CONSTS is None:
        _CONSTS = make_consts()
    n = 8
    x = np.asarray(x, np.float32)
    B = x.shape[0]
    per = B // n
    shared = {
        "norm1_g": np.ascontiguousarray(np.asarray(norm1_g, np.float32).reshape(1, 1024)),
        "w_in": np.ascontiguousarray(np.asarray(w_in, np.float32).reshape(1024, 3584)),
        "rec_lb_logits": np.ascontiguousarray(np.asarray(rec_lb_logits, np.float32).reshape(2, 512)),
        "rec_norm_g": np.ascontiguousarray(np.asarray(rec_norm_g, np.float32).reshape(1, 512)),
        "w_out": np.ascontiguousarray(np.asarray(w_out, np.float32).reshape(1024, 1024)),
        "norm2_g": np.ascontiguousarray(np.asarray(norm2_g, np.float32).reshape(1, 1024)),
        "peer_wq": np.ascontiguousarray(np.asarray(peer_wq, np.float32).reshape(1024, 2048)),
        "peer_subkeys": np.ascontiguousarray(np.asarray(peer_subkeys, np.float32).reshape(16, 128, 128)),
        "peer_u": np.ascontiguousarray(np.asarray(peer_u, np.float32).reshape(16384, 1024)),
        "peer_v": np.ascontiguousarray(np.asarray(peer_v, np.float32).reshape(16384, 1024)),
        "normf_g": np.ascontiguousarray(np.asarray(normf_g, np.float32).reshape(1, 1024)),
    }
    shared.update(_CONSTS)
    nc = build(nseq=per)
    in_maps = []
    for i in range(n):
        m = dict(shared)
        m["x"] = np.ascontiguousarray(x[i * per:(i + 1) * per].reshape(per * 2048, 1024))
        in_maps.append(m)
    res = run_bass_kernel_spmd(nc, in_maps, core_ids=list(range(n)))
    outs = [np.asarray(r["out"], np.float32).reshape(per, 2048, 1024) for r in res.results]
    return np.concatenate(outs, axis=0)
```
